# Optimizing a Trainium2 kernel written in Bass

```python
import math
import jax
import jax.numpy as jnp
from jax import lax
import numpy as np

D_MODEL = 1024
BATCH = 16
SEQ = 256
DEPTH = 4
DEC_BATCH = 4
DEC_SEQ = 1024
PAST_LEN = 256

GRID_W = 64
BRANCH_W = D_MODEL // 2
N_BRANCH = 3
DA_HEADS = 4
DA_HEAD_DIM = BRANCH_W // (2 * DA_HEADS)
DA_V_DIM = 2 * DA_HEAD_DIM
ROPE_BASE = 10000.0
Q_BLOCK = 128
RG_WIDTH = BRANCH_W
RG_BLOCKS = 8
RG_BLOCK_W = RG_WIDTH // RG_BLOCKS
RG_CONV_W = 4
RG_C = 8.0
HG_HEADS = 4
HG_KEY = BRANCH_W // HG_HEADS
HG_VAL = BRANCH_W // HG_HEADS
HG_CHUNK = 16
N_EXPERTS = 32
TOP_K = 4
D_EXPERT = D_MODEL
SWIGLU_ALPHA = 1.702
SWIGLU_LIMIT = 7.0
DN_ALPHA = (2 * DEPTH) ** 0.25
DN_BETA = (8 * DEPTH) ** -0.25
NORM_EPS = 1e-5
_IN_SIZES = (BRANCH_W, BRANCH_W, BRANCH_W, RG_WIDTH, RG_WIDTH,
             HG_HEADS * HG_KEY, HG_HEADS * HG_KEY, HG_HEADS * HG_KEY, HG_HEADS * HG_VAL, HG_HEADS * HG_VAL,
             N_BRANCH * D_MODEL)
D_IN = sum(_IN_SIZES)
_IN_SPLITS = tuple(int(s) for s in np.cumsum(_IN_SIZES)[:-1])

kernel_name = 'hybrid_diffusion_step'

F32 = jnp.float32


def _layer_norm(x, g, b):
    xf = x.astype(F32)
    mu = jnp.mean(xf, -1, keepdims=True)
    var = jnp.mean(jnp.square(xf - mu), -1, keepdims=True)
    return ((xf - mu) * lax.rsqrt(var + NORM_EPS)).astype(x.dtype) * g + b


def _rms_norm(x, w):
    xf = x.astype(F32)
    y = xf * lax.rsqrt(jnp.mean(jnp.square(xf), -1, keepdims=True) + NORM_EPS)
    return y.astype(x.dtype) * w


def _adaln(cond, w, b):
    m = (jax.nn.silu(cond) @ w + b).reshape(cond.shape[0], 1, 6, D_MODEL)
    return [m[:, :, j] for j in range(6)]


def _rope_1d(x, pos):
    d = x.shape[-1]
    half = d // 2
    inv = ROPE_BASE ** (-jnp.arange(0, d, 2, dtype=F32) / d)
    ang = pos[:, None] * inv[None, :]
    shape = (1, pos.shape[0]) + (1,) * (x.ndim - 3) + (half,)
    cos = jnp.cos(ang).reshape(shape)
    sin = jnp.sin(ang).reshape(shape)
    x1 = x[..., :half].astype(F32)
    x2 = x[..., half:].astype(F32)
    return jnp.concatenate([x1 * cos - x2 * sin, x2 * cos + x1 * sin], -1).astype(x.dtype)


def _rope_2d(x):
    S = x.shape[1]
    rows = S // GRID_W
    row = jnp.repeat(jnp.arange(rows, dtype=F32), GRID_W)
    col = jnp.tile(jnp.arange(GRID_W, dtype=F32), rows)
    half = x.shape[-1] // 2
    return jnp.concatenate([_rope_1d(x[..., :half], row), _rope_1d(x[..., half:], col)], -1)


def _diff_attention(q, k, v, lam):
    B, Sq = q.shape[0], q.shape[1]
    nb = Sq // Q_BLOCK
    q_blocks = jnp.swapaxes(q.reshape((B, nb, Q_BLOCK) + q.shape[2:]), 0, 1)
    scale = DA_HEAD_DIM ** -0.5

    def block(qb):
        s = jnp.einsum('bqhmd,bkhmd->bhmqk', qb, k).astype(F32) * scale
        a = jax.nn.softmax(s, axis=-1)
        w = a[:, :, 0] - lam * a[:, :, 1]
        return jnp.einsum('bhqk,bkhe->bqhe', w.astype(v.dtype), v)

    out = lax.map(block, q_blocks)
    return jnp.swapaxes(out, 0, 1).reshape(B, Sq, DA_HEADS, DA_V_DIM)


def _depthwise_conv(x, w, b):
    left = RG_CONV_W // 2
    right = RG_CONV_W - 1 - left
    y = lax.conv_general_dilated(x, w[:, None, :].astype(x.dtype), window_strides=(1,),
                                 padding=[(left, right)], dimension_numbers=('NWC', 'WIO', 'NWC'),
                                 feature_group_count=x.shape[-1])
    return y + b


def _linear_scan(a, b, h0, reverse):
    def combine(left, right):
        return left[0] * right[0], right[0] * left[1] + right[1]
    a_cum, b_cum = lax.associative_scan(combine, (a, b), reverse=reverse, axis=1)
    h = a_cum * h0[:, None, :] + b_cum
    return h, (h[:, 0] if reverse else h[:, -1])


def _rglru(x, gate_w, gate_b, lam, h0, reverse):
    B, S, W = x.shape
    xb = x.reshape(B, S, RG_BLOCKS, RG_BLOCK_W)
    g = jnp.einsum('bsnc,gncd->gbsnd', xb, gate_w).reshape(2, B, S, W) + gate_b[:, None, None, :]
    g = g.astype(F32)
    r = jax.nn.sigmoid(g[0])
    i = jax.nn.sigmoid(g[1])
    log_a = -RG_C * jax.nn.softplus(-lam.astype(F32)) * r
    a = jnp.exp(log_a)
    b = jnp.sqrt(-jnp.expm1(2.0 * log_a)) * i * x.astype(F32)
    return _linear_scan(a, b, h0.astype(F32), reverse)


def _hgrn_lower_bounds(hg_lb):
    pr = jax.nn.softmax(hg_lb.astype(F32), axis=0)
    return jnp.cumsum(pr, axis=0) - pr[0]


def _hgrn2_gates(z, lb):
    B, S, _ = z.shape
    zf = z.astype(F32)
    log_f = jnp.log(lb + (1.0 - lb) * jax.nn.sigmoid(zf))
    k = (1.0 - lb) * jax.nn.sigmoid(-zf)
    shape = (B, S, HG_HEADS, HG_KEY)
    return k.reshape(shape), log_f.reshape(shape)


def _hgrn2_scan(q, k, v, log_f, s0, reverse):
    if reverse:
        o, s_fin = _hgrn2_scan(jnp.flip(q, 1), jnp.flip(k, 1), jnp.flip(v, 1), jnp.flip(log_f, 1), s0, False)
        return jnp.flip(o, 1), s_fin
    B, S, H, _ = q.shape
    n = S // HG_CHUNK

    def chunks(t):
        return t.astype(F32).reshape(B, n, HG_CHUNK, H, t.shape[-1]).transpose(0, 1, 3, 2, 4)

    qc, kc, vc, gc = chunks(q), chunks(k), chunks(v), chunks(log_f)
    bc = jnp.cumsum(gc, axis=3)
    causal = jnp.tril(jnp.ones((HG_CHUNK, HG_CHUNK), bool))[:, :, None]
    diff = bc[:, :, :, :, None, :] - bc[:, :, :, None, :, :]
    decay = jnp.exp(jnp.where(causal, diff, -jnp.inf))
    scores = jnp.einsum('bnhtk,bnhsk,bnhtsk->bnhts', qc, kc, decay)
    o_intra = jnp.einsum('bnhts,bnhsv->bnhtv', scores, vc)
    b_last = bc[:, :, :, -1:, :]
    kv = jnp.einsum('bnhck,bnhcv->bnhkv', kc * jnp.exp(b_last - bc), vc)
    chunk_decay = jnp.exp(b_last[:, :, :, 0, :])

    def step(state, inp):
        dec, kv_n = inp
        return dec[..., None] * state + kv_n, state

    s_fin, s_start = lax.scan(step, s0.astype(F32),
                              (jnp.swapaxes(chunk_decay, 0, 1), jnp.swapaxes(kv, 0, 1)))
    o_inter = jnp.einsum('bnhck,bnhkv->bnhcv', qc * jnp.exp(bc), jnp.swapaxes(s_start, 0, 1))
    o = (o_intra + o_inter).transpose(0, 1, 3, 2, 4).reshape(B, S, H, -1)
    return o, s_fin


def _token_mix(u, l, p, lb, ctx):
    B, S, _ = u.shape
    (aq, ak, av, rx, rgate, hq, hz_f, hz_b, hi, hgate, mg) = jnp.split(u @ p['w_in'][l], _IN_SPLITS, axis=-1)

    q = aq.reshape(B, S, DA_HEADS, 2, DA_HEAD_DIM)
    k = ak.reshape(B, S, DA_HEADS, 2, DA_HEAD_DIM)
    v = av.reshape(B, S, DA_HEADS, DA_V_DIM)
    lambda_init = 0.8 - 0.6 * math.exp(-0.3 * l)
    lq1, lk1, lq2, lk2 = p['da_lambda'][l].astype(F32)
    lam = jnp.exp(jnp.sum(lq1 * lk1)) - jnp.exp(jnp.sum(lq2 * lk2)) + lambda_init
    if ctx is None:
        q_att, k_att, v_att = q, k, v
        h0_rg = jnp.zeros((B, 2, RG_WIDTH), F32)
        s0_hg = jnp.zeros((B, 2, HG_HEADS, HG_KEY, HG_VAL), F32)
    else:
        k_ctx, v_ctx, h0_rg, s0_hg = ctx
        q_att = _rope_2d(q)
        k_att = jnp.concatenate([_rope_2d(k), k_ctx.astype(k.dtype)], axis=1)
        v_att = jnp.concatenate([v, v_ctx.astype(v.dtype)], axis=1)
    att = _diff_attention(q_att, k_att, v_att, lam)
    att = (_rms_norm(att, p['da_subln'][l]) * (1.0 - lambda_init)).reshape(B, S, BRANCH_W)

    xr = _depthwise_conv(rx, p['rg_conv_w'][l], p['rg_conv_b'][l])
    h_f, hl_f = _rglru(xr, p['rg_gate_w'][l, 0], p['rg_gate_b'][l, 0], p['rg_lambda'][l, 0], h0_rg[:, 0], False)
    h_b, hl_b = _rglru(xr, p['rg_gate_w'][l, 1], p['rg_gate_b'][l, 1], p['rg_lambda'][l, 1], h0_rg[:, 1], True)
    rg = (h_f + h_b).astype(u.dtype) * jax.nn.gelu(rgate)

    qh = jax.nn.silu(hq).reshape(B, S, HG_HEADS, HG_KEY)
    vh = hi.reshape(B, S, HG_HEADS, HG_VAL)
    kf, gf = _hgrn2_gates(hz_f, lb[0])
    o_f, sl_f = _hgrn2_scan(qh, kf, vh, gf, s0_hg[:, 0], False)
    kb, gb = _hgrn2_gates(hz_b, lb[1])
    o_b, sl_b = _hgrn2_scan(qh, kb, vh, gb, s0_hg[:, 1], True)
    hg = (_rms_norm(o_f + o_b, p['hg_norm'][l]).astype(u.dtype)
          * jax.nn.silu(hgate.reshape(B, S, HG_HEADS, HG_VAL))).reshape(B, S, BRANCH_W)

    branches = jnp.stack([att, rg, hg], axis=2)
    proj = jnp.einsum('bsnc,ncd->bsnd', branches, p['w_branch'][l])
    gates = jax.nn.sigmoid(mg.reshape(B, S, N_BRANCH, D_MODEL))
    out = jnp.sum(gates * proj, axis=2) @ p['w_out'][l]
    if ctx is None:
        return out, (k, v, jnp.stack([hl_f, hl_b], axis=1), jnp.stack([sl_f, sl_b], axis=1))
    return out, None


def _moe(u, l, p):
    B, S, D = u.shape
    t = u.reshape(-1, D)
    logits = (t @ p['router_w'][l] + p['router_b'][l]).astype(F32)
    top_v, top_i = lax.top_k(logits, TOP_K)
    wts = jax.nn.softmax(top_v, axis=-1)
    gate = jnp.sum(jax.nn.one_hot(top_i, N_EXPERTS, dtype=F32) * wts[..., None], axis=1).astype(u.dtype)
    h = jnp.einsum('td,edf->tef', t, p['w1'][l]) + p['b1'][l]
    glu = jnp.minimum(h[..., ::2], SWIGLU_LIMIT)
    lin = jnp.clip(h[..., 1::2], -SWIGLU_LIMIT, SWIGLU_LIMIT)
    act = glu * jax.nn.sigmoid(SWIGLU_ALPHA * glu) * (lin + 1.0)
    y = jnp.einsum('tef,te,efd->td', act, gate, p['w2'][l]) + gate @ p['b2'][l]
    return y.reshape(B, S, D)


def _trunk_layer(x, cond, l, p, lb, ctx):
    sh1, sc1, g1, sh2, sc2, g2 = _adaln(cond, p['w_ada'][l], p['b_ada'][l])
    mix, new_ctx = _token_mix(x * (1.0 + sc1) + sh1, l, p, lb, ctx)
    x = _layer_norm(DN_ALPHA * x + g1 * mix, p['ln1_g'][l], p['ln1_b'][l])
    ffn = _moe(x * (1.0 + sc2) + sh2, l, p)
    x = _layer_norm(DN_ALPHA * x + g2 * ffn, p['ln2_g'][l], p['ln2_b'][l])
    return x, new_ctx


def setup_inputs(seed: int = 0) -> dict:
    key = jax.random.key(seed)
    keys = iter(jax.random.split(key, 40))

    def nrm(shape, scale):
        return scale * jax.random.normal(next(keys), shape, F32)

    a0 = jax.random.uniform(next(keys), (DEPTH, 2, RG_WIDTH), F32, 0.9, 0.999)
    a_root = a0 ** (1.0 / RG_C)
    rg_lambda = jnp.log(a_root) - jnp.log1p(-a_root)
    return {
        'x_prompt': nrm((BATCH, SEQ, D_MODEL), 1.0),
        'x_sample': nrm((DEC_BATCH, DEC_SEQ, D_MODEL), 1.0),
        'cache_attn_k': nrm((DEC_BATCH, DEPTH, PAST_LEN, DA_HEADS, 2, DA_HEAD_DIM), 1.0),
        'cache_attn_v': nrm((DEC_BATCH, DEPTH, PAST_LEN, DA_HEADS, DA_V_DIM), 1.0),
        'state_rglru': nrm((DEC_BATCH, DEPTH, 2, RG_WIDTH), 0.5),
        'state_hgrn': nrm((DEC_BATCH, DEPTH, 2, HG_HEADS, HG_KEY, HG_VAL), 0.3),
        'c': nrm((DEC_BATCH, D_MODEL), 1.0),
        'c_ctx': nrm((D_MODEL,), 1.0),
        'w_ada': nrm((DEPTH, D_MODEL, 6 * D_MODEL), 0.5 * D_MODEL ** -0.5),
        'b_ada': nrm((DEPTH, 6 * D_MODEL), 0.02),
        'w_in': nrm((DEPTH, D_MODEL, D_IN), D_MODEL ** -0.5),
        'da_lambda': nrm((DEPTH, 4, DA_HEAD_DIM), 0.1),
        'da_subln': 1.0 + nrm((DEPTH, DA_V_DIM), 0.01),
        'rg_conv_w': nrm((DEPTH, RG_CONV_W, RG_WIDTH), RG_CONV_W ** -0.5),
        'rg_conv_b': nrm((DEPTH, RG_WIDTH), 0.01),
        'rg_gate_w': nrm((DEPTH, 2, 2, RG_BLOCKS, RG_BLOCK_W, RG_BLOCK_W), RG_BLOCK_W ** -0.5),
        'rg_gate_b': nrm((DEPTH, 2, 2, RG_WIDTH), 0.01),
        'rg_lambda': rg_lambda,
        'hg_lb': 1.0 + nrm((DEPTH, 2, HG_HEADS * HG_KEY), 0.1),
        'hg_norm': 1.0 + nrm((DEPTH, HG_VAL), 0.01),
        'w_branch': nrm((DEPTH, N_BRANCH, BRANCH_W, D_MODEL), BRANCH_W ** -0.5),
        'w_out': nrm((DEPTH, D_MODEL, D_MODEL), DN_BETA * D_MODEL ** -0.5),
        'ln1_g': 1.0 + nrm((DEPTH, D_MODEL), 0.01),
        'ln1_b': nrm((DEPTH, D_MODEL), 0.01),
        'router_w': nrm((DEPTH, D_MODEL, N_EXPERTS), D_MODEL ** -0.5),
        'router_b': nrm((DEPTH, N_EXPERTS), 0.01),
        'w1': nrm((DEPTH, N_EXPERTS, D_MODEL, 2 * D_EXPERT), D_MODEL ** -0.5),
        'b1': nrm((DEPTH, N_EXPERTS, 2 * D_EXPERT), 0.01),
        'w2': nrm((DEPTH, N_EXPERTS, D_EXPERT, D_MODEL), DN_BETA * D_EXPERT ** -0.5),
        'b2': nrm((DEPTH, N_EXPERTS, D_MODEL), 0.01),
        'ln2_g': 1.0 + nrm((DEPTH, D_MODEL), 0.01),
        'ln2_b': nrm((DEPTH, D_MODEL), 0.01),
    }


def reference(x_prompt, x_sample, cache_attn_k, cache_attn_v, state_rglru, state_hgrn, c, c_ctx,
              w_ada, b_ada, w_in, da_lambda, da_subln, rg_conv_w, rg_conv_b, rg_gate_w, rg_gate_b,
              rg_lambda, hg_lb, hg_norm, w_branch, w_out, ln1_g, ln1_b, router_w, router_b,
              w1, b1, w2, b2, ln2_g, ln2_b):
    p = dict(w_ada=w_ada, b_ada=b_ada, w_in=w_in, da_lambda=da_lambda, da_subln=da_subln,
             rg_conv_w=rg_conv_w, rg_conv_b=rg_conv_b, rg_gate_w=rg_gate_w, rg_gate_b=rg_gate_b,
             rg_lambda=rg_lambda, hg_norm=hg_norm, w_branch=w_branch, w_out=w_out,
             ln1_g=ln1_g, ln1_b=ln1_b, router_w=router_w, router_b=router_b,
             w1=w1, b1=b1, w2=w2, b2=b2, ln2_g=ln2_g, ln2_b=ln2_b)
    lbs = _hgrn_lower_bounds(hg_lb)

    y_prompt = x_prompt
    cond_ctx = c_ctx[None, :]
    ks, vs, rgs, hgs = [], [], [], []
    for l in range(DEPTH):
        y_prompt, (k_l, v_l, rg_l, hg_l) = _trunk_layer(y_prompt, cond_ctx, l, p, lbs[l], None)
        ks.append(k_l)
        vs.append(v_l)
        rgs.append(rg_l)
        hgs.append(hg_l)

    y_sample = x_sample
    for l in range(DEPTH):
        ctx = (cache_attn_k[:, l], cache_attn_v[:, l], state_rglru[:, l], state_hgrn[:, l])
        y_sample, _ = _trunk_layer(y_sample, c, l, p, lbs[l], ctx)

    new_attn_k = jnp.stack(ks, axis=1)
    new_attn_v = jnp.stack(vs, axis=1)
    new_state_rglru = jnp.stack(rgs, axis=1)
    new_state_hgrn = jnp.stack(hgs, axis=1)
    return (y_prompt, y_sample, new_attn_k, new_attn_v, new_state_rglru, new_state_hgrn)
```

```python
import math
from contextlib import ExitStack
from concourse.bass_utils import run_bass_kernel_spmd
import numpy as np
import concourse.bass as bass
import concourse.mybir as mybir

F32 = mybir.dt.float32
BF16 = mybir.dt.bfloat16
I32 = mybir.dt.int32
AF = mybir.ActivationFunctionType
ALU = mybir.AluOpType
AX = mybir.AxisListType


class Buf:
    def __init__(self, t, name=""):
        self.t = t
        self.name = name
        self.last_w = None
        self.readers = []

    def __getitem__(self, idx):
        return View(self, self.t[idx])

    def ap(self, a):
        return View(self, a)


class View:
    def __init__(self, buf, ap):
        self.buf = buf
        self.ap = ap


def _ap(v):
    return v.ap if isinstance(v, View) else v


class Sched:
    def __init__(self, nc, n_dma_sems=48):
        self.nc = nc
        self.engs = {}
        for name, e in (("pe", nc.tensor), ("act", nc.scalar), ("dve", nc.vector), ("pool", nc.gpsimd), ("sp", nc.sync)):
            sem = nc.alloc_semaphore(name=f"sem_{name}") if name != "sp" else None
            self.engs[name] = dict(e=e, sem=sem, cnt=0, known={})
        self.dma_rings = {q: [dict(sem=nc.alloc_semaphore(name=f"dsem_{q}{i}"), val=0) for i in range(n_dma_sems // 2)]
                          for q in ("sp", "pool")}
        self.dma_rr = {"sp": 0, "pool": 0}
        self.nops = 0

    def _wait(self, eng, deps):
        E = self.engs[eng]
        best = {}
        for d in deps:
            if d is None:
                continue
            sem, val = d
            k = id(sem)
            if k not in best or best[k][1] < val:
                best[k] = (sem, val)
        for k, (sem, val) in best.items():
            if E["known"].get(k, 0) >= val:
                continue
            if sem is E["sem"] and (eng == "pe" or val > E["cnt"]):
                continue
            E["e"].wait_ge(sem, val)
            E["known"][k] = val

    def _deps(self, reads, writes):
        deps = []
        for v in reads:
            if isinstance(v, View):
                deps.append(v.buf.last_w)
        for v in writes:
            if isinstance(v, View):
                deps.append(v.buf.last_w)
                deps.extend(v.buf.readers)
        return deps

    def _commit(self, reads, writes, tok):
        for v in writes:
            if isinstance(v, View):
                v.buf.last_w = tok
                v.buf.readers = []
        for v in reads:
            if isinstance(v, View):
                rs = [r for r in v.buf.readers if r[0] is not tok[0]]
                rs.append(tok)
                v.buf.readers = rs

    def op(self, eng, fn, reads, writes, signal=True):
        E = self.engs[eng]
        self._wait(eng, self._deps(reads, writes))
        ins = fn()
        self.nops += 1
        if signal:
            ins.then_inc(E["sem"], 1)
            E["cnt"] += 1
            tok = (E["sem"], E["cnt"])
        else:
            tok = (E["sem"], E["cnt"] + 1)
        self._commit(reads, writes, tok)
        return ins

    def dma(self, q, out, in_, **kw):
        E = self.engs[q]
        ring = self.dma_rings[q]
        slot = ring[self.dma_rr[q]]
        self.dma_rr[q] = (self.dma_rr[q] + 1) % len(ring)
        deps = self._deps([in_], [out])
        if slot["val"] > 0:
            deps.append((slot["sem"], slot["val"]))
        self._wait(q, deps)
        ins = E["e"].dma_start(out=_ap(out), in_=_ap(in_), **kw)
        slot["val"] += 16
        ins.then_inc(slot["sem"], 16)
        tok = (slot["sem"], slot["val"])
        self._commit([in_], [out], tok)
        self.nops += 1
        return tok

    def barrier_tokens(self):
        toks = []
        for name, E in self.engs.items():
            if E["sem"] is not None and E["cnt"] > 0:
                toks.append((E["sem"], E["cnt"]))
        for ring in self.dma_rings.values():
            for s in ring:
                if s["val"] > 0:
                    toks.append((s["sem"], s["val"]))
        return toks

    def wait_all(self, eng):
        self._wait(eng, self.barrier_tokens())

    def mm(self, out, lhsT, rhs, start=True, stop=True, signal=None, **kw):
        if signal is None:
            signal = stop
        return self.op("pe", lambda: self.nc.tensor.matmul(_ap(out), lhsT=_ap(lhsT), rhs=_ap(rhs), start=start, stop=stop, **kw),
                       [lhsT, rhs] + ([] if start else [out]), [out], signal=signal)

    def transpose(self, out, in_, ident, **kw):
        return self.op("pe", lambda: self.nc.tensor.transpose(_ap(out), _ap(in_), _ap(ident), **kw), [in_, ident], [out])

    def act(self, out, in_, func, bias=None, scale=None, accum_out=None, eng="act"):
        kw = {}
        reads = [in_]
        writes = [out]
        if bias is not None:
            kw["bias"] = _ap(bias)
            reads.append(bias)
        if scale is not None:
            kw["scale"] = _ap(scale)
            reads.append(scale)
        if accum_out is not None:
            kw["accum_out"] = _ap(accum_out)
            writes.append(accum_out)
        return self.op("act", lambda: self.nc.scalar.activation(out=_ap(out), in_=_ap(in_), func=func, **kw), reads, writes)

    def _veng(self, eng):
        return {"dve": self.nc.vector, "pool": self.nc.gpsimd}[eng]

    def tt(self, out, in0, in1, op, eng="dve"):
        return self.op(eng, lambda: self._veng(eng).tensor_tensor(out=_ap(out), in0=_ap(in0), in1=_ap(in1), op=op), [in0, in1], [out])

    def ts(self, out, in0, s1, op0, s2=None, op1=None, eng="dve", accum_out=None):
        reads = [in0, s1, s2]
        writes = [out] + ([accum_out] if accum_out is not None else [])
        kw = {}
        if op1 is not None:
            kw["op1"] = op1
        if accum_out is not None:
            kw["accum_out"] = _ap(accum_out)
        return self.op(eng, lambda: self._veng(eng).tensor_scalar(out=_ap(out), in0=_ap(in0), scalar1=_ap(s1), scalar2=_ap(s2), op0=op0, **kw), reads, writes)

    def stt(self, out, in0, scalar, in1, op0, op1, eng="dve"):
        return self.op(eng, lambda: self._veng(eng).scalar_tensor_tensor(out=_ap(out), in0=_ap(in0), scalar=_ap(scalar), in1=_ap(in1), op0=op0, op1=op1), [in0, scalar, in1], [out])

    def copy(self, out, in_, eng="dve"):
        if eng == "act":
            return self.op("act", lambda: self.nc.scalar.copy(out=_ap(out), in_=_ap(in_)), [in_], [out])
        return self.op(eng, lambda: self._veng(eng).tensor_copy(out=_ap(out), in_=_ap(in_)), [in_], [out])

    def memset(self, out, val, eng="dve"):
        return self.op(eng, lambda: self._veng(eng).memset(_ap(out), val), [], [out])

    def scan(self, out, d0, d1, initial, op0, op1, eng="dve"):
        return self.op(eng, lambda: self._veng(eng).tensor_tensor_scan(out=_ap(out), data0=_ap(d0), data1=_ap(d1), initial=_ap(initial), op0=op0, op1=op1), [d0, d1, initial], [out])

    def reduce(self, out, in_, op, axis=AX.X, eng="dve"):
        return self.op(eng, lambda: self._veng(eng).tensor_reduce(out=_ap(out), in_=_ap(in_), axis=axis, op=op), [in_], [out])


DEPTH = 4
ALPHA = (2 * DEPTH) ** 0.25
EPS = 1e-5
EPS_LN = EPS / (ALPHA * ALPHA)
NT = 1024
W_IN = 8192
C_Q, C_K, C_V, C_RX, C_RG, C_HQ, C_HZF, C_HZB, C_HI, C_HG, C_MG = 0, 512, 1024, 1536, 2048, 2560, 3072, 3584, 4096, 4608, 5120


def build(L=DEPTH, n_exp=32, stages=("mix", "moe")):
    nc = bass.Bass("TRN2", target_bir_lowering=False)
    S = Sched(nc)

    def din(name, shape, dt=F32):
        return nc.dram_tensor(name, list(shape), dt, kind="ExternalInput").ap()

    def dout(name, shape, dt=F32):
        return nc.dram_tensor(name, list(shape), dt, kind="ExternalOutput").ap()

    x_d = din("x", [NT, 1024]); cond_d = din("cond", [128, 8])
    kctx_d = din("kctx", [L, 256, 512]); vctx_d = din("vctx", [L, 256, 512])
    rg0_d = din("rg0", [128, L * 8]); hg0_d = din("hg0", [L, 2, 4, 128, 128])
    amk_d = din("amk", [8, 1280]); amq_d = din("amq", [8, 1024])
    ropec_d = din("ropec", [128, NT]); ropes_d = din("ropes", [128, NT]); perm_d = din("perm", [128, 128])
    cmask_d = din("cmask", [128, 3 * NT]); smask_d = din("smask", [128, 2 * NT]); rmask_d = din("rmask", [128, 2 * NT])
    hcm_d = din("hcm", [128, 64]); cb_d = din("cb", [128, 256]); ident_d = din("ident", [128, 128]); sel_d = din("sel", [32, 32 * 128])
    w_ada_d = din("w_ada", [L, 1024, 6144]); b_ada_d = din("b_ada", [128, L * 48]); w_in_d = din("w_in", [L, 1024, W_IN])
    dal_d = din("dal", [128, L * 256]); subln_d = din("subln", [128, L])
    convw_d = din("convw", [128, L * 16]); convb_d = din("convb", [128, L * 4]); gw_d = din("gw", [L, 2, 2, 8, 64, 64])
    gb_d = din("gb", [128, L * 16]); rlam_d = din("rlam", [128, L * 8]); hlb_d = din("hlb", [128, 32]); hnorm_d = din("hnorm", [128, L])
    w_br_d = din("w_br", [L, 3, 512, 1024]); w_out_d = din("w_out", [L, 1024, 1024])
    lnp_d = din("lnp", [128, 4 * L * 8])
    rw_d = din("rw", [L, 1024, 32]); rb_d = din("rb", [128, L * 32])
    NE = 32 if "moe" in stages else 1
    if "mix" in stages:
        stages = tuple(stages) + ("att", "rg", "hg")
    w1_d = din("w1d", [L, NE, 1024, 2048]); b1g_d = din("b1g", [128, L * 256]); b1l_d = din("b1l", [128, L * 256])
    w2_d = din("w2", [L, NE, 1024, 1024]); b2_d = din("b2", [L, NE, 1024])

    y_o = dout("y", [NT, 1024]); k_o = dout("ok", [L, NT, 512]); v_o = dout("ov", [L, NT, 512])
    rg_o = dout("org", [L, 32, 128]); hg_o = dout("ohg", [L, 4, 2, 4, 128, 128])

    es_all = ExitStack()

    uid = [0]

    def sb(es, name, shape, dt=F32):
        uid[0] += 1
        name = f"{name}_{uid[0]}"
        return Buf(es.enter_context(nc.sbuf_tensor(name, list(shape), dt)), name)

    def psb(es, name, shape, dt=F32):
        return Buf(es.enter_context(nc.psum_tensor(name, list(shape), dt)), name)

    def barrier():
        for e in ("pe", "act", "dve", "pool", "sp"):
            S.wait_all(e)

    P = es_all
    ps = [psb(P, f"ps{i}", [128, 512]) for i in range(7)]
    psb16 = psb(P, "psb16", [128, 1024], BF16)
    X = [sb(P, f"x{c}", [128, NT]) for c in range(8)]
    U = [sb(P, f"u{c}", [128, NT], BF16) for c in range(8)]
    ident = sb(P, "ident", [128, 128]); identb = sb(P, "identb", [128, 128], BF16)
    ones = sb(P, "ones", [128, 128]); onesb = sb(P, "onesb", [128, 128], BF16)
    mod = sb(P, "mod", [128, L * 48])
    lnp = sb(P, "lnp", [128, 4 * L * 8])
    small = {}
    for nm, dd, w in (("subln", subln_d, L), ("convw", convw_d, L * 16), ("convb", convb_d, L * 4), ("gb", gb_d, L * 16),
                      ("rlam", rlam_d, L * 8), ("hlb", hlb_d, 32), ("hnorm", hnorm_d, L), ("rg0", rg0_d, L * 8),
                      ("hcm", hcm_d, 64), ("rb", rb_d, L * 32), ("b1g", b1g_d, L * 256), ("b1l", b1l_d, L * 256),
                      ("cond", cond_d, 8)):
        small[nm] = sb(P, nm, [128, w])
        S.dma("sp", small[nm][:], dd[:, :])
    S.dma("sp", ident[:], ident_d[:, :]); S.dma("sp", lnp[:], lnp_d[:, :])
    S.copy(identb[:], ident[:])
    S.memset(ones[:], 1.0); S.memset(onesb[:], 1.0)
    lam_neg = sb(P, "lam_neg", [128, L])
    nsp = sb(P, "nsp", [128, L * 8])
    lbv = sb(P, "lbv", [128, 32]); oml = sb(P, "oml", [128, 32])
    subw = sb(P, "subw", [128, L])
    hcmb = small["hcm"]

    with ExitStack() as E0:
        xt = sb(E0, "xt", [128, 8, 1024])
        S.dma("sp", xt[:], x_d.rearrange("(tb p) f -> p tb f", p=128))
        wst = [sb(E0, f"wst{i}", [128, 6144]) for i in range(2)]
        scond = sb(E0, "scond", [128, 8])
        S.act(scond[:], small["cond"][:], AF.Silu)
        badat = sb(E0, "badat", [128, L * 48]); S.dma("sp", badat[:], b_ada_d[:, :])
        for l in range(L):
            for kc in range(8):
                wt = wst[(l * 8 + kc) % 2]
                S.dma("sp", wt[:], w_ada_d[l, kc * 128:(kc + 1) * 128, :])
                for j in range(48):
                    S.mm(ps[0][:, j:j + 1], wt[:, j * 128:(j + 1) * 128], scond[:, kc:kc + 1], start=True, stop=True, signal=(j == 47))
                S.tt(mod[:, l * 48:(l + 1) * 48], ps[0][:, 0:48], (badat if kc == 0 else mod)[:, l * 48:(l + 1) * 48], ALU.add)
        for l in range(L):
            b = l * 48
            S.ts(mod[:, b + 8:b + 16], mod[:, b + 8:b + 16], 1.0, ALU.add)
            S.ts(mod[:, b + 32:b + 40], mod[:, b + 32:b + 40], 1.0, ALU.add)
            S.ts(mod[:, b + 16:b + 24], mod[:, b + 16:b + 24], 1.0 / ALPHA, ALU.mult)
            S.ts(mod[:, b + 40:b + 48], mod[:, b + 40:b + 48], 1.0 / ALPHA, ALU.mult)
        for c in range(8):
            for hf in range(2):
                for i in range(4):
                    tb = hf * 4 + i
                    S.transpose(ps[1 + hf][:, i * 128:(i + 1) * 128], xt[:, tb, c * 128:(c + 1) * 128], ident[:])
                S.copy(X[c][:, hf * 512:(hf + 1) * 512], ps[1 + hf][:], eng=("dve" if hf == 0 else "act"))
        dal = sb(E0, "dal", [128, L * 256]); S.dma("sp", dal[:], dal_d[:, :])
        t2 = sb(E0, "t2s", [128, L * 2 * 64]); t3 = sb(E0, "t3s", [128, L * 2])
        dv = dal.t[:, :].rearrange("p (l a d) -> p l a d", l=L, a=4)
        for l in range(L):
            for a in range(2):
                S.tt(t2[:, (l * 2 + a) * 64:(l * 2 + a + 1) * 64], dal[:, l * 256 + (2 * a) * 64:l * 256 + (2 * a + 1) * 64],
                     dal[:, l * 256 + (2 * a + 1) * 64:l * 256 + (2 * a + 2) * 64], ALU.mult)
                S.reduce(t3[:, l * 2 + a:l * 2 + a + 1], t2[:, (l * 2 + a) * 64:(l * 2 + a + 1) * 64], ALU.add)
        S.act(t3[:], t3[:], AF.Exp)
        for l in range(L):
            li = 0.8 - 0.6 * math.exp(-0.3 * l)
            S.stt(lam_neg[:, l:l + 1], t3[:, 2 * l + 1:2 * l + 2], -li, t3[:, 2 * l:2 * l + 1], ALU.add, ALU.subtract)
            S.ts(subw[:, l:l + 1], small["subln"][:, l:l + 1], 1.0 - li, ALU.mult)
        S.act(nsp[:], small["rlam"][:], AF.Exp, scale=-1.0)
        S.act(nsp[:], nsp[:], AF.Ln, bias=1.0)
        S.ts(nsp[:], nsp[:], -8.0, ALU.mult)
        eh = sb(E0, "eh", [128, 32]); sh_ = sb(E0, "sh_", [128, 8])
        S.act(eh[:], small["hlb"][:], AF.Exp)
        S.tt(sh_[:], eh[:, 0:8], eh[:, 8:16], ALU.add)
        S.tt(sh_[:], sh_[:], eh[:, 16:24], ALU.add)
        S.tt(sh_[:], sh_[:], eh[:, 24:32], ALU.add)
        S.op("dve", lambda: nc.vector.reciprocal(out=sh_.t[:], in_=sh_.t[:]), [sh_[:]], [sh_[:]])
        for l in range(4):
            S.tt(eh[:, l * 8:(l + 1) * 8], eh[:, l * 8:(l + 1) * 8], sh_[:], ALU.mult)
        S.memset(lbv[:, 0:8], 0.0)
        S.copy(lbv[:, 8:16], eh[:, 8:16])
        S.tt(lbv[:, 16:24], lbv[:, 8:16], eh[:, 16:24], ALU.add)
        S.tt(lbv[:, 24:32], lbv[:, 16:24], eh[:, 24:32], ALU.add)
        S.ts(oml[:], lbv[:], -1.0, ALU.mult, 1.0, ALU.add)
        barrier()

    def layer_norm(l, which, E):
        gcol = (2 * which) * L * 8 + l * 8
        bcol = (2 * which + 1) * L * 8 + l * 8
        sq = [sb(E, f"lnsq{which}_{i}", [128, 512]) for i in range(2)]
        mean = sb(E, f"lnmean{which}", [128, 512]); rstd = sb(E, f"lnrstd{which}", [128, 512]); tmp = sb(E, f"lntmp{which}", [128, 512])
        for th in range(2):
            sl = slice(th * 512, (th + 1) * 512)
            for c in range(8):
                S.mm(ps[0][:], ones[:], X[c][:, sl], start=(c == 0), stop=(c == 7))
            for c in range(8):
                S.act(sq[c % 2][:], X[c][:, sl], AF.Square)
                S.mm(ps[1][:], ones[:], sq[c % 2][:], start=(c == 0), stop=(c == 7), signal=True)
            S.ts(mean[:], ps[0][:], 1.0 / 1024, ALU.mult)
            S.tt(tmp[:], mean[:], mean[:], ALU.mult)
            S.stt(rstd[:], ps[1][:], 1.0 / 1024, tmp[:], ALU.mult, ALU.subtract)
            S.act(rstd[:], rstd[:], AF.Sqrt, bias=EPS_LN)
            S.op("dve", lambda: nc.vector.reciprocal(out=rstd.t[:], in_=rstd.t[:]), [rstd[:]], [rstd[:]])
            for c in range(8):
                S.tt(X[c][:, sl], X[c][:, sl], mean[:], ALU.subtract)
                S.tt(X[c][:, sl], X[c][:, sl], rstd[:], ALU.mult)
                S.ts(X[c][:, sl], X[c][:, sl], lnp[:, gcol + c:gcol + c + 1], ALU.mult, lnp[:, bcol + c:bcol + c + 1], ALU.add)

    def modulate(l, second):
        b = l * 48 + (24 if second else 0)
        for c in range(8):
            S.ts(U[c][:], X[c][:], mod[:, b + 8 + c:b + 9 + c], ALU.mult, mod[:, b + c:b + c + 1], ALU.add)

    wq_rr = [0]

    def load_w_cols(wbufs, src_rows_ap, c0, ncols):
        wb = wbufs[wq_rr[0] % len(wbufs)]
        wq_rr[0] += 1
        S.dma("pool", wb[:, :, 0:ncols], src_rows_ap.rearrange("(kc p) c -> p kc c", p=128)[:, :, c0:c0 + ncols])
        return wb

    def proj_fm(wb, oc, th, out_ps, nk=8, rhs=None):
        rhs = rhs or U
        for kc in range(nk):
            S.mm(out_ps, wb[:, kc, oc * 128:(oc + 1) * 128], rhs[kc][:, th * 512:(th + 1) * 512], start=(kc == 0), stop=(kc == nk - 1))

    def proj_tm(wb, tb, out_ps, ncols=512):
        for kc in range(8):
            S.mm(out_ps, U[kc][:, tb * 128:(tb + 1) * 128], wb[:, kc, 0:ncols], start=(kc == 0), stop=(kc == 7))

    for l in range(L):
        li = 0.8 - 0.6 * math.exp(-0.3 * l)
        modulate(l, False)
        with ExitStack() as EM:
            BR = [[sb(EM, f"br{n}_{c}", [128, NT], BF16) for c in range(4)] for n in range(3)]
            wbufs = [sb(EM, f"wcol{i}", [128, 8, 512], BF16) for i in range(2)]
            if True:
                with ExitStack() as EA:
                  if "att" in stages:
                    Q = [[sb(EA, f"q{h}_{m}", [128, NT], BF16) for m in range(2)] for h in range(4)]
                    K = [[sb(EA, f"k{h}_{m}", [128, 1280], BF16) for m in range(2)] for h in range(4)]
                    for h in range(4 if "noM" not in stages else 0):
                        for m in range(2):
                            oth = slice(64, 128) if m == 0 else slice(0, 64)
                            mrow = slice(64, 72) if m == 0 else slice(0, 8)
                            S.memset(Q[h][m][oth, :], 0.0); S.memset(K[h][m][oth, :], 0.0)
                            S.dma("pool", Q[h][m][mrow, :], amq_d[:, :]); S.dma("pool", K[h][m][mrow, :], amk_d[:, :])
                    V = [sb(EA, f"v{t}", [128, 512], BF16) for t in range(10)]
                    rc = sb(EA, "ropec", [128, NT]); rs = sb(EA, "ropes", [128, NT]); perm = sb(EA, "perm", [128, 128], BF16)
                    S.dma("sp", rc[:], ropec_d[:, :]); S.dma("sp", rs[:], ropes_d[:, :]); S.dma("pool", perm[:], perm_d[:, :])
                    qb = [sb(EA, f"qb{i}", [128, 512], BF16) for i in range(2)]
                    t1 = [sb(EA, f"at1{i}", [128, 512]) for i in range(2)]
                    t2 = [sb(EA, f"at2{i}", [128, 512]) for i in range(2)]
                    stg = [sb(EA, f"stg{i}", [128, 512]) for i in range(2)]
                    for which, c0, DST in (((0, C_Q, Q), (1, C_K, K)) if "noA1" not in stages else ()):
                        wb = load_w_cols(wbufs, w_in_d[l], c0, 512)
                        for h in range(4):
                            for th in range(2):
                                i = th
                                sl = slice(th * 512, (th + 1) * 512)
                                proj_fm(wb, h, th, ps[th][:])
                                if "noR" in stages:
                                    S.copy(t1[i][:], ps[th][:], eng="act")
                                    S.memset(t2[i][:], 0.0)
                                else:
                                    S.copy(qb[i][:], ps[th][:], eng="act")
                                    S.op("dve", lambda: nc.vector.tensor_tensor(out=t1[i].t[:], in0=ps[th].t[:], in1=rc.t[:, sl], op=ALU.mult),
                                         [ps[th][:], rc[:, sl], qb[i][:]], [t1[i][:]])
                                    if "R1" in stages:
                                        S.memset(t2[i][:], 0.0)
                                    else:
                                        S.mm(ps[2 + th][:], perm[:], qb[i][:])
                                        S.tt(t2[i][:], ps[2 + th][:], rs[:, sl], ALU.mult)
                                S.tt(DST[h][0][0:64, sl], t1[i][0:64, :], t2[i][0:64, :], ALU.add)
                                S.tt(DST[h][1][64:128, sl], t1[i][64:128, :], t2[i][64:128, :], ALU.add)
                    wbk = load_w_cols(wbufs, w_in_d[l], C_K, 512)
                    NA2 = 8 if "noA2" not in stages else 0
                    for tb in range(NA2):
                        proj_tm(wbk, tb, ps[tb % 2][:])
                        S.copy(stg[tb % 2][:], ps[tb % 2][:], eng="act")
                        S.dma("sp", k_o[l, tb * 128:(tb + 1) * 128, :], stg[tb % 2][:])
                    wbv = load_w_cols(wbufs, w_in_d[l], C_V, 512)
                    for tb in range(NA2):
                        proj_tm(wbv, tb, ps[tb % 2][:])
                        S.copy(stg[tb % 2][:], ps[tb % 2][:], eng="act")
                        S.copy(V[tb][:], stg[tb % 2][:])
                        S.dma("sp", v_o[l, tb * 128:(tb + 1) * 128, :], stg[tb % 2][:])
                    NA3 = 2 if "noA3" not in stages else 0
                    for blk in range(NA3):
                        S.dma("sp", stg[blk][:], kctx_d[l, blk * 128:(blk + 1) * 128, :])
                        for h in range(4):
                            S.transpose(ps[2][:, h * 128:(h + 1) * 128], stg[blk][:, h * 128:(h + 1) * 128], ident[:])
                        for h in range(4):
                            S.copy(K[h][0][0:64, 1024 + blk * 128:1024 + (blk + 1) * 128], ps[2][0:64, h * 128:(h + 1) * 128])
                            S.copy(K[h][1][64:128, 1024 + blk * 128:1024 + (blk + 1) * 128], ps[2][64:128, h * 128:(h + 1) * 128])
                    for blk in range(NA3):
                        S.dma("pool", V[8 + blk][:], vctx_d[l, blk * 128:(blk + 1) * 128, :])
                    pT = [sb(EA, f"pT{i}", [128, 512], BF16) for i in range(3)]
                    o_a = sb(EA, "o_a", [128, 512]); o_b = sb(EA, "o_b", [128, 512]); rcp = sb(EA, "rcp", [128, 512]); sqa = sb(EA, "sqa", [128, 512])
                    pi = 0
                    for h in range(4 if "noattcore" not in stages else 0):
                        for qh in range(2):
                            qs = slice(qh * 512, (qh + 1) * 512)
                            for m in range(2):
                                ms = slice(m * 64, (m + 1) * 64)
                                for kb in range(10):
                                    sc = ps[4 + (kb % 2)]
                                    S.mm(sc[:], K[h][m][:, kb * 128:(kb + 1) * 128], Q[h][m][:, qs])
                                    p_ = pT[pi % 3]; pi += 1
                                    S.act(p_[:], sc[:], AF.Exp, scale=0.125)
                                    S.mm(ps[2 * m][:], V[kb][:, h * 128:(h + 1) * 128], p_[:], start=(kb == 0), stop=(kb == 9), signal=True)
                                    S.mm(ps[2 * m + 1][:], onesb[:], p_[:], start=(kb == 0), stop=(kb == 9), signal=True)
                            S.op("dve", lambda: nc.vector.reciprocal(out=rcp.t[:], in_=ps[1].t[:]), [ps[1][:]], [rcp[:]])
                            S.tt(o_a[:], ps[0][:], rcp[:], ALU.mult)
                            S.op("dve", lambda: nc.vector.reciprocal(out=rcp.t[:], in_=ps[3].t[:]), [ps[3][:]], [rcp[:]])
                            S.tt(o_b[:], ps[2][:], rcp[:], ALU.mult)
                            S.stt(o_a[:], o_b[:], lam_neg[:, l:l + 1], o_a[:], ALU.mult, ALU.add)
                            S.act(sqa[:], o_a[:], AF.Square)
                            S.mm(ps[6][:], ones[:], sqa[:])
                            S.act(rcp[:], ps[6][:], AF.Sqrt, scale=1.0 / 128, bias=EPS)
                            S.op("dve", lambda: nc.vector.reciprocal(out=rcp.t[:], in_=rcp.t[:]), [rcp[:]], [rcp[:]])
                            S.tt(o_a[:], o_a[:], rcp[:], ALU.mult)
                            S.ts(BR[0][h][:, qs], o_a[:], subw[:, l:l + 1], ALU.mult)
                    barrier()
                with ExitStack() as EB:
                  if "rg" in stages:
                    cm = sb(EB, "cmaskt", [128, 3 * NT], BF16); S.dma("pool", cm[:], cmask_d[:, :])
                    sm = sb(EB, "smaskt", [128, 2 * NT], BF16); S.dma("pool", sm[:], smask_d[:, :])
                    gwt = sb(EB, "gwt", [128, 16 * 128], BF16)
                    S.memset(gwt[:], 0.0)
                    for d in range(2):
                        for g in range(2):
                            for n in range(8):
                                c, half = n // 2, n % 2
                                col = ((d * 2 + g) * 4 + c) * 128 + half * 64
                                S.dma("pool", gwt[half * 64:(half + 1) * 64, col:col + 64], gw_d[l, d, g, n, :, :])
                    rx = sb(EB, "rx", [128, NT]); xr = sb(EB, "xr", [128, NT]); xrb = sb(EB, "xrb", [128, NT], BF16)
                    tmp = sb(EB, "rtmp", [128, NT]); rr = sb(EB, "rr", [128, NT]); ii = sb(EB, "ii", [128, NT])
                    aa = sb(EB, "aa", [128, NT]); bb = sb(EB, "bb", [128, NT]); hh = [sb(EB, f"hh{d}", [128, NT]) for d in range(2)]
                    gg = sb(EB, "gg", [128, NT]); ge = sb(EB, "ge", [128, NT])
                    rgfin = sb(EB, "rgfin", [128, 32])
                    wbx = load_w_cols(wbufs, w_in_d[l], C_RX, 512)
                    wbg = load_w_cols(wbufs, w_in_d[l], C_RG, 512)
                    rgv = rgfin.t[:, :].rearrange("p (s d c) -> p s d c", s=4, d=2)
                    for c in range(4):
                        for th in range(2):
                            proj_fm(wbx, c, th, ps[th][:])
                            S.copy(rx[:, th * 512:(th + 1) * 512], ps[th][:], eng="act")
                        cw = lambda tap: small["convw"][:, l * 16 + tap * 4 + c:l * 16 + tap * 4 + c + 1]
                        S.ts(xr[:], rx[:], cw(2), ALU.mult, small["convb"][:, l * 4 + c:l * 4 + c + 1], ALU.add)
                        S.tt(tmp[:, 2:NT], rx[:, 0:NT - 2], cm[:, 2:NT], ALU.mult)
                        S.stt(xr[:, 2:NT], tmp[:, 2:NT], cw(0), xr[:, 2:NT], ALU.mult, ALU.add)
                        S.tt(tmp[:, 1:NT], rx[:, 0:NT - 1], cm[:, NT + 1:2 * NT], ALU.mult)
                        S.stt(xr[:, 1:NT], tmp[:, 1:NT], cw(1), xr[:, 1:NT], ALU.mult, ALU.add)
                        S.tt(tmp[:, 0:NT - 1], rx[:, 1:NT], cm[:, 2 * NT:3 * NT - 1], ALU.mult)
                        S.stt(xr[:, 0:NT - 1], tmp[:, 0:NT - 1], cw(3), xr[:, 0:NT - 1], ALU.mult, ALU.add)
                        S.copy(xrb[:], xr[:], eng="act")
                        for d in range(2):
                            for g, dst in ((0, rr), (1, ii)):
                                col = ((d * 2 + g) * 4 + c) * 128
                                bcol = l * 16 + (d * 2 + g) * 4 + c
                                for th in range(2):
                                    S.mm(ps[2 + th][:], gwt[:, col:col + 128], xrb[:, th * 512:(th + 1) * 512])
                                    S.act(dst[:, th * 512:(th + 1) * 512], ps[2 + th][:], AF.Sigmoid, bias=small["gb"][:, bcol:bcol + 1])
                            ncol = l * 8 + d * 4 + c
                            S.act(aa[:], rr[:], AF.Exp, scale=nsp[:, ncol:ncol + 1])
                            S.tt(bb[:], aa[:], aa[:], ALU.mult)
                            S.act(bb[:], bb[:], AF.Sqrt, scale=-1.0, bias=1.0)
                            S.tt(bb[:], bb[:], ii[:], ALU.mult)
                            S.tt(bb[:], bb[:], xr[:], ALU.mult)
                            S.tt(aa[:], aa[:], sm[:, d * NT:(d + 1) * NT], ALU.mult)
                            h0 = small["rg0"][:, ncol:ncol + 1]
                            if d == 0:
                                S.scan(hh[0][:], aa[:], bb[:], h0, ALU.mult, ALU.add)
                                S.copy(View(rgfin, rgv[:, :, 0, c]), hh[0][:, 255:NT:256])
                            else:
                                S.scan(hh[1][:, ::-1], aa[:, ::-1], bb[:, ::-1], h0, ALU.mult, ALU.add)
                                S.copy(View(rgfin, rgv[:, :, 1, c]), hh[1][:, 0:NT:256])
                        S.tt(hh[0][:], hh[0][:], hh[1][:], ALU.add)
                        for th in range(2):
                            proj_fm(wbg, c, th, ps[th][:])
                            S.copy(gg[:, th * 512:(th + 1) * 512], ps[th][:], eng="act")
                        S.tt(ge[:], gg[:], gg[:], ALU.mult)
                        S.ts(ge[:], ge[:], 0.044715, ALU.mult, 1.0, ALU.add)
                        S.tt(ge[:], ge[:], gg[:], ALU.mult)
                        S.act(ge[:], ge[:], AF.Sigmoid, scale=2.0 * math.sqrt(2.0 / math.pi))
                        S.tt(ge[:], ge[:], gg[:], ALU.mult)
                        S.tt(BR[1][c][:], hh[0][:], ge[:], ALU.mult)
                    S.transpose(ps[4][0:32, 0:128], rgfin[:], ident[:])
                    rgo = sb(EB, "rgo", [32, 128]); S.copy(rgo[:], ps[4][0:32, 0:128])
                    S.dma("sp", rg_o[l, :, :], rgo[:])
                    barrier()
                with ExitStack() as EC:
                  if "hg" in stages:
                    rm = sb(EC, "rmaskt", [128, 2 * NT], BF16); S.dma("pool", rm[:], rmask_d[:, :])
                    cbm = sb(EC, "cbm", [128, 256], BF16); S.dma("pool", cbm[:], cb_d[:, :])
                    HI = [sb(EC, f"hi{t}", [128, 512], BF16) for t in range(8)]
                    HIc = [sb(EC, f"hic{n}", [32, 512], BF16) for n in range(32)]
                    wbi = load_w_cols(wbufs, w_in_d[l], C_HI, 512)
                    for tb in range(8):
                        proj_tm(wbi, tb, ps[tb % 2][:])
                        S.copy(HI[tb][:], ps[tb % 2][:], eng=("act" if tb % 2 else "dve"))
                    for n in range(32):
                        for kc in range(8):
                            S.mm(ps[2 + n % 2][0:32, :], U[kc][:, n * 32:(n + 1) * 32], wbi[:, kc, 0:512], start=(kc == 0), stop=(kc == 7))
                        S.copy(HIc[n][:], ps[2 + n % 2][0:32, :], eng=("act" if n % 2 else "dve"))
                    wh = [sb(EC, f"wh{i}", [128, 8, 128], BF16) for i in range(4)]
                    qh = sb(EC, "qh", [128, NT]); osum = sb(EC, "osum", [128, NT]); sg = sb(EC, "sg", [128, NT])
                    gl = sb(EC, "gl", [128, NT]); kk = sb(EC, "kk", [128, NT]); bc = sb(EC, "bc", [128, NT]); ex = sb(EC, "ex", [128, NT])
                    qt = sb(EC, "qt", [128, NT], BF16); kt = sb(EC, "kt", [128, NT], BF16); kh = sb(EC, "kh", [128, NT], BF16)
                    Dv = sb(EC, "Dv", [128, 32]); Dcm = sb(EC, "Dcm", [128, 32])
                    KHc = [sb(EC, f"khc{n}", [32, 128], BF16) for n in range(32)]
                    AT = [sb(EC, f"AT{t}", [128, 128], BF16) for t in range(8)]
                    S32 = sb(EC, "S32", [128, 128]); Sb = sb(EC, "Sb", [128, 128], BF16)
                    hgt = sb(EC, "hgt", [128, NT]); sqh = sb(EC, "sqh", [128, 512]); rsh = sb(EC, "rsh", [128, 512])
                    for h in range(4):
                        for i, c0 in enumerate((C_HQ, C_HZF, C_HZB, C_HG)):
                            S.dma("pool", wh[i][:], w_in_d[l].rearrange("(kc p) c -> p kc c", p=128)[:, :, c0 + h * 128:c0 + (h + 1) * 128])
                        for th in range(2):
                            proj_fm(wh[0], 0, th, ps[th][:])
                            S.act(qh[:, th * 512:(th + 1) * 512], ps[th][:], AF.Silu)
                        for d in range(2):
                            lc = l * 8 + d * 4 + h
                            for th in range(2):
                                proj_fm(wh[1 + d], 0, th, ps[th][:])
                                S.act(sg[:, th * 512:(th + 1) * 512], ps[th][:], AF.Sigmoid)
                            S.ts(gl[:], sg[:], oml[:, lc:lc + 1], ALU.mult, lbv[:, lc:lc + 1], ALU.add)
                            S.act(gl[:], gl[:], AF.Ln)
                            S.ts(kk[:], sg[:], oml[:, lc:lc + 1], ALU.mult)
                            S.ts(kk[:], kk[:], -1.0, ALU.mult)
                            S.ts(kk[:], kk[:], oml[:, lc:lc + 1], ALU.add)
                            if d == 0:
                                S.scan(bc[:], rm[:, 0:NT], gl[:], 0.0, ALU.mult, ALU.add)
                                S.act(Dv[:], bc[:, 31:NT:32], AF.Exp)
                            else:
                                S.scan(bc[:, ::-1], rm[:, 2 * NT - 1:NT - 1:-1], gl[:, ::-1], 0.0, ALU.mult, ALU.add)
                                S.act(Dv[:], bc[:, 0:NT:32], AF.Exp)
                            S.act(ex[:], bc[:], AF.Exp)
                            S.tt(qt[:], qh[:], ex[:], ALU.mult)
                            S.act(ex[:], bc[:], AF.Exp, scale=-1.0)
                            S.tt(ex[:], kk[:], ex[:], ALU.mult)
                            S.copy(kt[:], ex[:], eng="act")
                            for n in range(32):
                                S.ts(kh[:, n * 32:(n + 1) * 32], ex[:, n * 32:(n + 1) * 32], Dv[:, n:n + 1], ALU.mult)
                            S.tt(Dcm[:], Dv[:], hcmb[:, d * 32:(d + 1) * 32], ALU.mult)
                            for n in range(32):
                                S.transpose(psb16[0:32, (n % 8) * 128:(n % 8 + 1) * 128], kh[:, n * 32:(n + 1) * 32], identb[:])
                                S.copy(KHc[n][:], psb16[0:32, (n % 8) * 128:(n % 8 + 1) * 128], eng="act")
                            for tb in range(8):
                                pa = ps[4 + (tb // 4) % 2]
                                S.mm(pa[:, (tb % 4) * 128:(tb % 4 + 1) * 128], kt[:, tb * 128:(tb + 1) * 128], qt[:, tb * 128:(tb + 1) * 128])
                                S.tt(AT[tb][:], pa[:, (tb % 4) * 128:(tb % 4 + 1) * 128], cbm[:, d * 128:(d + 1) * 128], ALU.mult)
                            S.dma("sp", S32[:], hg0_d[l, d, h, :, :])
                            first = 0 if d == 0 else 31
                            S.ts(Sb[:], S32[:], hcmb[:, d * 32 + first:d * 32 + first + 1], ALU.mult)
                            po = ps[d]
                            for step in range(32):
                                n = step if d == 0 else 31 - step
                                tb, j = n // 4, n % 4
                                bs = slice((tb % 4) * 128, (tb % 4 + 1) * 128)
                                blk_first = (j == 0) if d == 0 else (j == 3)
                                blk_last = (j == 3) if d == 0 else (j == 0)
                                if blk_first:
                                    S.mm(po[:, bs], HI[tb][:, h * 128:(h + 1) * 128], AT[tb][:], start=True, stop=False, signal=True)
                                S.mm(po[:, (tb % 4) * 128 + j * 32:(tb % 4) * 128 + (j + 1) * 32], Sb[:], qt[:, n * 32:(n + 1) * 32],
                                     start=False, stop=blk_last, signal=True)
                                pk = ps[2 + step % 2]
                                S.mm(pk[:, 0:128], KHc[n][:], HIc[n][:, h * 128:(h + 1) * 128])
                                S.stt(S32[:], S32[:], Dcm[:, n:n + 1], pk[:, 0:128], ALU.mult, ALU.add)
                                seq_end = (n % 8 == 7) if d == 0 else (n % 8 == 0)
                                if seq_end:
                                    S.dma("sp", hg_o[l, n // 8, d, h, :, :], S32[:])
                                nn = n + 1 if d == 0 else n - 1
                                if 0 <= nn < 32:
                                    S.ts(Sb[:], S32[:], hcmb[:, d * 32 + nn:d * 32 + nn + 1], ALU.mult)
                                if blk_last:
                                    ts_ = slice(tb * 128, (tb + 1) * 128)
                                    if d == 0:
                                        S.copy(osum[:, ts_], po[:, bs])
                                    else:
                                        S.tt(osum[:, ts_], osum[:, ts_], po[:, bs], ALU.add)
                        for th in range(2):
                            sl = slice(th * 512, (th + 1) * 512)
                            proj_fm(wh[3], 0, th, ps[th][:])
                            S.act(hgt[:, sl], ps[th][:], AF.Silu)
                            S.act(sqh[:], osum[:, sl], AF.Square)
                            S.mm(ps[6][:], ones[:], sqh[:])
                            S.act(rsh[:], ps[6][:], AF.Sqrt, scale=1.0 / 128, bias=EPS)
                            S.op("dve", lambda: nc.vector.reciprocal(out=rsh.t[:], in_=rsh.t[:]), [rsh[:]], [rsh[:]])
                            S.tt(rsh[:], rsh[:], osum[:, sl], ALU.mult)
                            S.ts(rsh[:], rsh[:], small["hnorm"][:, l:l + 1], ALU.mult)
                            S.tt(BR[2][h][:, sl], rsh[:], hgt[:, sl], ALU.mult)
                    barrier()
            with ExitStack() as ED:
                wbr = sb(ED, "wbr", [128, 12, 1024], BF16)
                S.dma("pool", wbr[:], w_br_d[l].rearrange("n (kc p) c -> p (n kc) c", p=128))
                macc = [sb(ED, f"macc{c}", [128, NT]) for c in range(8)]
                gsb = sb(ED, "gsb", [128, 512]); gp = sb(ED, "gp", [128, 512])
                for n in range(3):
                    for og in range(2):
                        wb = load_w_cols(wbufs, w_in_d[l], C_MG + n * 1024 + og * 512, 512)
                        for oi in range(4):
                            oc = og * 4 + oi
                            for th in range(2):
                                sl = slice(th * 512, (th + 1) * 512)
                                proj_fm(wb, oi, th, ps[th][:])
                                S.act(gsb[:], ps[th][:], AF.Sigmoid)
                                for kc in range(4):
                                    S.mm(ps[2 + th][:], wbr[:, n * 4 + kc, oc * 128:(oc + 1) * 128], BR[n][kc][:, sl], start=(kc == 0), stop=(kc == 3))
                                if n == 0:
                                    S.tt(macc[oc][:, sl], gsb[:], ps[2 + th][:], ALU.mult)
                                else:
                                    S.tt(gp[:], gsb[:], ps[2 + th][:], ALU.mult)
                                    S.tt(macc[oc][:, sl], macc[oc][:, sl], gp[:], ALU.add)
                mb = [sb(ED, f"mb{c}", [128, NT], BF16) for c in range(8)]
                for c in range(8):
                    S.copy(mb[c][:], macc[c][:], eng=("act" if c % 2 else "dve"))
                for og in range(2):
                    wb = load_w_cols(wbufs, w_out_d[l], og * 512, 512)
                    for oi in range(4):
                        oc = og * 4 + oi
                        for th in range(2):
                            sl = slice(th * 512, (th + 1) * 512)
                            proj_fm(wb, oi, th, ps[th][:], rhs=mb)
                            S.stt(X[oc][:, sl], ps[th][:], mod[:, l * 48 + 16 + oc:l * 48 + 17 + oc], X[oc][:, sl], ALU.mult, ALU.add)
                layer_norm(l, 0, ED)
                barrier()
        barrier()
        modulate(l, True)
        with ExitStack() as EE:
            if "moe" in stages:
                lg = sb(EE, "lg", [128, 256]); gate = sb(EE, "gate", [128, 256]); m8 = sb(EE, "m8", [128, 8]); nm = sb(EE, "nm", [128, 1])
                msk = sb(EE, "msk", [128, 32]); den = sb(EE, "den", [128, 1])
                gT = sb(EE, "gT", [32, NT])
                rbt = sb(EE, "rbt", [128, 256])
                for tb in range(8):
                    S.copy(rbt[:, tb * 32:(tb + 1) * 32], small["rb"][:, l * 32:(l + 1) * 32])
                ER = ExitStack()
                u2f = [sb(ER, f"u2f{i}", [128, NT]) for i in range(2)]
                rwt = sb(ER, "rwt", [128, 8, 32]); S.dma("sp", rwt[:], rw_d[l].rearrange("(kc p) e -> p kc e", p=128))
                b = l * 48 + 24
                for c in range(8):
                    uf = u2f[c % 2]
                    S.ts(uf[:], X[c][:], mod[:, b + 8 + c:b + 9 + c], ALU.mult, mod[:, b + c:b + c + 1], ALU.add)
                    for tb in range(8):
                        S.mm(ps[0][:, tb * 32:(tb + 1) * 32], uf[:, tb * 128:(tb + 1) * 128], rwt[:, c, :], start=True, stop=True, signal=(tb == 7))
                    S.tt(lg[:], ps[0][:, 0:256], (rbt if c == 0 else lg)[:], ALU.add)
                for tb in range(8):
                    lt = lg[:, tb * 32:(tb + 1) * 32]
                    S.op("dve", lambda: nc.vector.max(out=m8.t[:], in_=lg.t[:, tb * 32:(tb + 1) * 32]), [lt], [m8[:]])
                    S.ts(nm[:], m8[:, 0:1], -1.0, ALU.mult)
                    S.ts(msk[:], lt, m8[:, 3:4], ALU.is_ge)
                    gt_ = gate[:, tb * 32:(tb + 1) * 32]
                    S.act(gt_, lt, AF.Exp, bias=nm[:])
                    S.tt(gt_, gt_, msk[:], ALU.mult)
                    S.reduce(den[:], gt_, ALU.add)
                    S.op("dve", lambda: nc.vector.reciprocal(out=den.t[:], in_=den.t[:]), [den[:]], [den[:]])
                    S.ts(gt_, gt_, den[:], ALU.mult)
                    if tb < 4:
                        S.transpose(ps[1][0:32, tb * 128:(tb + 1) * 128], gt_, ident[:])
                S.copy(gT[:, 0:512], ps[1][0:32, 0:512]);
                for tb in range(4, 8):
                    pass
                for tb in range(4, 8):
                    S.transpose(ps[2][0:32, (tb - 4) * 128:(tb - 3) * 128], gate[:, tb * 32:(tb + 1) * 32], ident[:])
                S.copy(gT[:, 512:1024], ps[2][0:32, 0:512])
                barrier()
                ER.close()
                b2t = sb(EE, "b2t", [32, 1024]); S.dma("sp", b2t[:], b2_d[l, :, :])
                selt = sb(EE, "selt", [32, 32 * 128], BF16); S.dma("pool", selt[:], sel_d[:, :])
                gTb = sb(EE, "gTb", [32, NT], BF16); S.copy(gTb[:], gT[:])
                g2c = l * 48 + 40
                for dc in range(8):
                    for th in range(2):
                        sl = slice(th * 512, (th + 1) * 512)
                        S.mm(ps[3 + th][:], b2t[:, dc * 128:(dc + 1) * 128], gT[:, sl])
                        S.stt(X[dc][:, sl], ps[3 + th][:], mod[:, g2c + dc:g2c + dc + 1], X[dc][:, sl], ALU.mult, ALU.add)
                ring = [sb(EE, f"wring{i}", [128, 8, 1024], BF16) for i in range(4)]
                ACTB = [[sb(EE, f"actb{i}_{j}", [128, NT], BF16) for j in range(8)] for i in range(2)]
                gbc = [sb(EE, "gbc0", [128, NT])] * 2
                glu = [sb(EE, f"glu{i}", [128, 512]) for i in range(2)]; sgm = [sb(EE, f"sgm{i}", [128, 512]) for i in range(2)]
                lin = [sb(EE, f"lin{i}", [128, 512]) for i in range(2)]
                rr_ = [0]

                def load_piece(src):
                    wb = ring[rr_[0] % 4]; rr_[0] += 1
                    S.dma("pool", wb[:], src)
                    return wb

                def pieces(e):
                    v1 = w1_d[l, e].rearrange("(kc p) c -> p kc c", p=128)
                    return (load_piece(v1[:, :, 0:1024]), load_piece(v1[:, :, 1024:2048]),
                            load_piece(w2_d[l, e].rearrange("(kc p) c -> p kc c", p=128)))

                nxt = pieces(0)
                it = 0
                for e in range(n_exp):
                    wg, wl, w2b = nxt
                    ab = ACTB[e % 2]
                    for th in range(2):
                        S.mm(ps[5][:], selt[:, e * 128:(e + 1) * 128], gTb[:, th * 512:(th + 1) * 512])
                        S.copy(gbc[e % 2][:, th * 512:(th + 1) * 512], ps[5][:], eng="act")
                    bcol = (l * 32 + e) * 8
                    for j in range(8):
                        for th in range(2):
                            sl = slice(th * 512, (th + 1) * 512)
                            i = it % 2; it += 1
                            proj_fm(wg, j, th, ps[2 * i][:])
                            proj_fm(wl, j, th, ps[2 * i + 1][:])
                            S.ts(glu[i][:], ps[2 * i][:], small["b1g"][:, bcol + j:bcol + j + 1], ALU.add)
                            S.ts(glu[i][:], glu[i][:], 7.0, ALU.min)
                            S.act(sgm[i][:], glu[i][:], AF.Sigmoid, scale=1.702)
                            S.ts(lin[i][:], ps[2 * i + 1][:], small["b1l"][:, bcol + j:bcol + j + 1], ALU.add)
                            S.ts(lin[i][:], lin[i][:], 7.0, ALU.min, -7.0, ALU.max)
                            S.ts(lin[i][:], lin[i][:], 1.0, ALU.add)
                            S.tt(glu[i][:], glu[i][:], sgm[i][:], ALU.mult)
                            S.tt(lin[i][:], lin[i][:], gbc[e % 2][:, sl], ALU.mult)
                            S.tt(ab[j][:, sl], glu[i][:], lin[i][:], ALU.mult)
                    if e + 1 < n_exp:
                        nxt = pieces(e + 1)
                    for dc in range(8):
                        for th in range(2):
                            sl = slice(th * 512, (th + 1) * 512)
                            i = it % 2; it += 1
                            proj_fm(w2b, dc, th, ps[2 * i][:], rhs=ab)
                            S.stt(X[dc][:, sl], ps[2 * i][:], mod[:, g2c + dc:g2c + dc + 1], X[dc][:, sl], ALU.mult, ALU.add)
            layer_norm(l, 1, EE)
            barrier()

    with ExitStack() as EF:
        yo = [sb(EF, f"yo{i}", [128, 1024]) for i in range(2)]
        for tb in range(8):
            for hf in range(2):
                for i in range(4):
                    c = hf * 4 + i
                    S.transpose(ps[hf][:, i * 128:(i + 1) * 128], X[c][:, tb * 128:(tb + 1) * 128], ident[:])
                S.copy(yo[tb % 2][:, hf * 512:(hf + 1) * 512], ps[hf][:], eng=("act" if hf else "dve"))
            S.dma("sp", y_o[tb * 128:(tb + 1) * 128, :], yo[tb % 2][:])
        barrier()
    es_all.close()
    return nc


def _cols(a, L):
    a = np.asarray(a, np.float32)
    lead = a.shape[:-1]
    n = a.shape[-1] // 128
    a = a.reshape(*lead, n, 128)
    a = np.moveaxis(a, -1, 0)
    return np.ascontiguousarray(a.reshape(128, -1))


def _structural(role):
    t = np.arange(NT)
    BIG = 32768.0
    amk = np.zeros((8, 1280), np.float32); amq = np.zeros((8, 1024), np.float32)
    if role == 1:
        amk[0, :] = -BIG; amq[0, :] = 1.0
        for g in range(4):
            amk[1 + g, g * 256:(g + 1) * 256] = BIG
            amq[1 + g, g * 256:(g + 1) * 256] = 1.0
    cos = np.ones((128, NT), np.float32); sin = np.zeros((128, NT), np.float32)
    perm = np.zeros((128, 128), np.float32)
    inv = 10000.0 ** (-np.arange(0, 32, 2, dtype=np.float32) / 32)
    row = (t // 64).astype(np.float32); col = (t % 64).astype(np.float32)
    for p in range(128):
        d = p % 64
        pos = row if d < 32 else col
        dd = d % 32
        j = dd % 16
        first = dd < 16
        partner = p + 16 if first else p - 16
        perm[partner, p] = 1.0
        if role == 0:
            ang = pos * inv[j]
            cos[p] = np.cos(ang)
            sin[p] = -np.sin(ang) if first else np.sin(ang)
    seg = (t % 256) if role == 1 else t
    seglen = 256 if role == 1 else NT
    cm = np.ones((3, NT), np.float32)
    cm[0, seg < 2] = 0; cm[1, seg < 1] = 0; cm[2, seg == seglen - 1] = 0
    smk = np.ones((2, NT), np.float32)
    if role == 1:
        smk[0, seg == 0] = 0; smk[1, seg == seglen - 1] = 0
    rmk = np.ones((2, NT), np.float32)
    rmk[0, t % 32 == 0] = 0; rmk[1, t % 32 == 31] = 0
    hcm = np.ones((2, 32), np.float32)
    if role == 1:
        hcm[0, np.arange(32) % 8 == 0] = 0; hcm[1, np.arange(32) % 8 == 7] = 0
    s_ = np.arange(128)[:, None]; t_ = np.arange(128)[None, :]
    same = (s_ // 32) == (t_ // 32)
    cb = np.concatenate([(same & (s_ <= t_)).astype(np.float32), (same & (s_ >= t_)).astype(np.float32)], axis=1)
    rep = lambda a: np.ascontiguousarray(np.broadcast_to(a.reshape(1, -1), (128, a.size)))
    return dict(amk=amk, amq=amq, ropec=cos, ropes=sin, perm=perm, cmask=rep(cm), smask=rep(smk), rmask=rep(rmk),
                hcm=rep(hcm), cb=np.ascontiguousarray(cb))


def prep_shared(inp, L=DEPTH):
    f = lambda k: np.asarray(inp[k], np.float32)
    sh = {}
    sh["w_ada"] = f("w_ada")[:L]; sh["w_in"] = f("w_in")[:L]; sh["w_br"] = f("w_branch")[:L]; sh["w_out"] = f("w_out")[:L]
    sh["rw"] = f("router_w")[:L]; sh["w2"] = f("w2")[:L]; sh["b2"] = f("b2")[:L]; sh["gw"] = f("rg_gate_w")[:L]
    w1 = f("w1")[:L]
    sh["w1d"] = np.ascontiguousarray(w1.reshape(L, 32, 1024, 1024, 2).transpose(0, 1, 2, 4, 3)).reshape(L, 32, 1024, 2048)
    b1 = f("b1")[:L].reshape(L, 32, 1024, 2)
    sh["b1g"] = _cols(b1[..., 0], L); sh["b1l"] = _cols(b1[..., 1], L)
    sh["b_ada"] = _cols(f("b_ada")[:L], L)
    sh["dal"] = np.ascontiguousarray(np.broadcast_to(f("da_lambda")[:L].reshape(1, -1), (128, L * 256)))
    sh["subln"] = _cols(f("da_subln")[:L], L); sh["hnorm"] = _cols(f("hg_norm")[:L], L)
    sh["convw"] = _cols(f("rg_conv_w")[:L], L); sh["convb"] = _cols(f("rg_conv_b")[:L], L)
    sh["gb"] = _cols(f("rg_gate_b")[:L], L); sh["rlam"] = _cols(f("rg_lambda")[:L], L)
    sh["hlb"] = _cols(f("hg_lb"), 4)
    sh["lnp"] = _cols(np.stack([f("ln1_g")[:L], f("ln1_b")[:L], f("ln2_g")[:L], f("ln2_b")[:L]]), L)
    sh["rb"] = np.ascontiguousarray(np.broadcast_to(f("router_b")[:L].reshape(1, -1), (128, L * 32)))
    sh["ident"] = np.eye(128, dtype=np.float32)
    sel = np.zeros((32, 32, 128), np.float32)
    for e in range(32):
        sel[e, e, :] = 1.0
    sh["sel"] = sel.reshape(32, 32 * 128)
    return sh


def prep_core(inp, c, L=DEPTH):
    f = lambda k: np.asarray(inp[k], np.float32)
    d = {}
    if c < 4:
        d["x"] = f("x_sample")[c]
        d["cond"] = _cols(f("c")[c], 1)
        d["kctx"] = f("cache_attn_k")[c, :L].reshape(L, 256, 512)
        d["vctx"] = f("cache_attn_v")[c, :L].reshape(L, 256, 512)
        d["rg0"] = _cols(f("state_rglru")[c, :L], L)
        d["hg0"] = f("state_hgrn")[c, :L]
        d.update(_structural(0))
    else:
        i = c - 4
        d["x"] = f("x_prompt")[4 * i:4 * i + 4].reshape(NT, 1024)
        d["cond"] = _cols(f("c_ctx"), 1)
        d["kctx"] = np.zeros((L, 256, 512), np.float32); d["vctx"] = np.zeros((L, 256, 512), np.float32)
        d["rg0"] = np.zeros((128, L * 8), np.float32); d["hg0"] = np.zeros((L, 2, 4, 128, 128), np.float32)
        d.update(_structural(1))
    return {k: np.ascontiguousarray(v, dtype=np.float32) for k, v in d.items()}


_NC_CACHE = {}


def kernel(**inputs):
    L = DEPTH
    if L not in _NC_CACHE:
        _NC_CACHE[L] = build(L)
    nc = _NC_CACHE[L]
    sh = prep_shared(inputs, L)
    in_maps = []
    for c in range(8):
        m = dict(sh)
        m.update(prep_core(inputs, c, L))
        in_maps.append(m)
    res = run_bass_kernel_spmd(nc, in_maps, core_ids=list(range(8))).results
    y_sample = np.stack([res[c]["y"] for c in range(4)]).astype(np.float32)
    y_prompt = np.concatenate([res[c]["y"].reshape(4, 256, 1024) for c in range(4, 8)]).astype(np.float32)
    ks, vs, rgs, hgs = [], [], [], []
    for c in range(4, 8):
        r = res[c]
        ks.append(r["ok"].reshape(L, 4, 256, 4, 2, 64).transpose(1, 0, 2, 3, 4, 5))
        vs.append(r["ov"].reshape(L, 4, 256, 4, 128).transpose(1, 0, 2, 3, 4))
        rgs.append(r["org"].reshape(L, 4, 2, 4, 128).transpose(1, 0, 2, 3, 4).reshape(4, L, 2, 512))
        hgs.append(r["ohg"].transpose(1, 0, 2, 3, 4, 5))
    cat = lambda xs: np.ascontiguousarray(np.concatenate(xs, axis=0), dtype=np.float32)
    return (y_prompt, y_sample, cat(ks), cat(vs), cat(rgs), cat(hgs))
```

```python
import math
from contextlib import ExitStack
from concourse.bass_utils import run_bass_kernel_spmd
import numpy as np
import concourse.bass as bass
import concourse.mybir as mybir

F32 = mybir.dt.float32
BF16 = mybir.dt.bfloat16
I32 = mybir.dt.int32
AF = mybir.ActivationFunctionType
ALU = mybir.AluOpType
AX = mybir.AxisListType


class Buf:
    def __init__(self, t, name=""):
        self.t = t
        self.name = name
        self.last_w = None
        self.readers = []

    def __getitem__(self, idx):
        return View(self, self.t[idx])

    def ap(self, a):
        return View(self, a)


class View:
    def __init__(self, buf, ap):
        self.buf = buf
        self.ap = ap


def _ap(v):
    return v.ap if isinstance(v, View) else v


class Sched:
    def __init__(self, nc, n_dma_sems=48):
        self.nc = nc
        self.engs = {}
        for name, e in (("pe", nc.tensor), ("act", nc.scalar), ("dve", nc.vector), ("pool", nc.gpsimd), ("sp", nc.sync)):
            sem = nc.alloc_semaphore(name=f"sem_{name}") if name != "sp" else None
            self.engs[name] = dict(e=e, sem=sem, cnt=0, known={})
        self.dma_rings = {q: [dict(sem=nc.alloc_semaphore(name=f"dsem_{q}{i}"), val=0) for i in range(n_dma_sems // 2)]
                          for q in ("sp", "pool")}
        self.dma_rr = {"sp": 0, "pool": 0}
        self.nops = 0

    def _wait(self, eng, deps):
        E = self.engs[eng]
        best = {}
        for d in deps:
            if d is None:
                continue
            sem, val = d
            k = id(sem)
            if k not in best or best[k][1] < val:
                best[k] = (sem, val)
        for k, (sem, val) in best.items():
            if E["known"].get(k, 0) >= val:
                continue
            if sem is E["sem"] and (eng == "pe" or val > E["cnt"]):
                continue
            E["e"].wait_ge(sem, val)
            E["known"][k] = val

    def _deps(self, reads, writes):
        deps = []
        for v in reads:
            if isinstance(v, View):
                deps.append(v.buf.last_w)
        for v in writes:
            if isinstance(v, View):
                deps.append(v.buf.last_w)
                deps.extend(v.buf.readers)
        return deps

    def _commit(self, reads, writes, tok):
        for v in writes:
            if isinstance(v, View):
                v.buf.last_w = tok
                v.buf.readers = []
        for v in reads:
            if isinstance(v, View):
                rs = [r for r in v.buf.readers if r[0] is not tok[0]]
                rs.append(tok)
                v.buf.readers = rs

    def op(self, eng, fn, reads, writes, signal=True):
        E = self.engs[eng]
        self._wait(eng, self._deps(reads, writes))
        ins = fn()
        self.nops += 1
        if signal:
            ins.then_inc(E["sem"], 1)
            E["cnt"] += 1
            tok = (E["sem"], E["cnt"])
        else:
            tok = (E["sem"], E["cnt"] + 1)
        self._commit(reads, writes, tok)
        return ins

    def dma(self, q, out, in_, **kw):
        E = self.engs[q]
        ring = self.dma_rings[q]
        slot = ring[self.dma_rr[q]]
        self.dma_rr[q] = (self.dma_rr[q] + 1) % len(ring)
        deps = self._deps([in_], [out])
        if slot["val"] > 0:
            deps.append((slot["sem"], slot["val"]))
        self._wait(q, deps)
        ins = E["e"].dma_start(out=_ap(out), in_=_ap(in_), **kw)
        slot["val"] += 16
        ins.then_inc(slot["sem"], 16)
        tok = (slot["sem"], slot["val"])
        self._commit([in_], [out], tok)
        self.nops += 1
        return tok

    def barrier_tokens(self):
        toks = []
        for name, E in self.engs.items():
            if E["sem"] is not None and E["cnt"] > 0:
                toks.append((E["sem"], E["cnt"]))
        for ring in self.dma_rings.values():
            for s in ring:
                if s["val"] > 0:
                    toks.append((s["sem"], s["val"]))
        return toks

    def wait_all(self, eng):
        self._wait(eng, self.barrier_tokens())

    def mm(self, out, lhsT, rhs, start=True, stop=True, signal=None, **kw):
        if signal is None:
            signal = stop
        return self.op("pe", lambda: self.nc.tensor.matmul(_ap(out), lhsT=_ap(lhsT), rhs=_ap(rhs), start=start, stop=stop, **kw),
                       [lhsT, rhs] + ([] if start else [out]), [out], signal=signal)

    def transpose(self, out, in_, ident, **kw):
        return self.op("pe", lambda: self.nc.tensor.transpose(_ap(out), _ap(in_), _ap(ident), **kw), [in_, ident], [out])

    def act(self, out, in_, func, bias=None, scale=None, accum_out=None, eng="act"):
        kw = {}
        reads = [in_]
        writes = [out]
        if bias is not None:
            kw["bias"] = _ap(bias)
            reads.append(bias)
        if scale is not None:
            kw["scale"] = _ap(scale)
            reads.append(scale)
        if accum_out is not None:
            kw["accum_out"] = _ap(accum_out)
            writes.append(accum_out)
        return self.op("act", lambda: self.nc.scalar.activation(out=_ap(out), in_=_ap(in_), func=func, **kw), reads, writes)

    def _veng(self, eng):
        return {"dve": self.nc.vector, "pool": self.nc.gpsimd}[eng]

    def tt(self, out, in0, in1, op, eng="dve"):
        return self.op(eng, lambda: self._veng(eng).tensor_tensor(out=_ap(out), in0=_ap(in0), in1=_ap(in1), op=op), [in0, in1], [out])

    def ts(self, out, in0, s1, op0, s2=None, op1=None, eng="dve", accum_out=None):
        reads = [in0, s1, s2]
        writes = [out] + ([accum_out] if accum_out is not None else [])
        kw = {}
        if op1 is not None:
            kw["op1"] = op1
        if accum_out is not None:
            kw["accum_out"] = _ap(accum_out)
        return self.op(eng, lambda: self._veng(eng).tensor_scalar(out=_ap(out), in0=_ap(in0), scalar1=_ap(s1), scalar2=_ap(s2), op0=op0, **kw), reads, writes)

    def stt(self, out, in0, scalar, in1, op0, op1, eng="dve"):
        return self.op(eng, lambda: self._veng(eng).scalar_tensor_tensor(out=_ap(out), in0=_ap(in0), scalar=_ap(scalar), in1=_ap(in1), op0=op0, op1=op1), [in0, scalar, in1], [out])

    def copy(self, out, in_, eng="dve"):
        if eng == "act":
            return self.op("act", lambda: self.nc.scalar.copy(out=_ap(out), in_=_ap(in_)), [in_], [out])
        return self.op(eng, lambda: self._veng(eng).tensor_copy(out=_ap(out), in_=_ap(in_)), [in_], [out])

    def memset(self, out, val, eng="dve"):
        return self.op(eng, lambda: self._veng(eng).memset(_ap(out), val), [], [out])

    def scan(self, out, d0, d1, initial, op0, op1, eng="dve"):
        return self.op(eng, lambda: self._veng(eng).tensor_tensor_scan(out=_ap(out), data0=_ap(d0), data1=_ap(d1), initial=_ap(initial), op0=op0, op1=op1), [d0, d1, initial], [out])

    def reduce(self, out, in_, op, axis=AX.X, eng="dve"):
        return self.op(eng, lambda: self._veng(eng).tensor_reduce(out=_ap(out), in_=_ap(in_), axis=axis, op=op), [in_], [out])


DEPTH = 4
ALPHA = (2 * DEPTH) ** 0.25
EPS = 1e-5
EPS_LN = EPS / (ALPHA * ALPHA)
NT = 1024
W_IN = 8192
C_Q, C_K, C_V, C_RX, C_RG, C_HQ, C_HZF, C_HZB, C_HI, C_HG, C_MG = 0, 512, 1024, 1536, 2048, 2560, 3072, 3584, 4096, 4608, 5120


def build(L=DEPTH, n_exp=32, stages=("mix", "moe")):
    nc = bass.Bass("TRN2", target_bir_lowering=False)
    S = Sched(nc)

    def din(name, shape, dt=F32):
        return nc.dram_tensor(name, list(shape), dt, kind="ExternalInput").ap()

    def dout(name, shape, dt=F32):
        return nc.dram_tensor(name, list(shape), dt, kind="ExternalOutput").ap()

    x_d = din("x", [NT, 1024]); cond_d = din("cond", [128, 8])
    kctx_d = din("kctx", [L, 256, 512]); vctx_d = din("vctx", [L, 256, 512])
    rg0_d = din("rg0", [128, L * 8]); hg0_d = din("hg0", [L, 2, 4, 128, 128])
    amk_d = din("amk", [8, 1280]); amq_d = din("amq", [8, 1024])
    ropec_d = din("ropec", [128, NT]); ropes_d = din("ropes", [128, NT]); perm_d = din("perm", [128, 128])
    cmask_d = din("cmask", [128, 3 * NT]); smask_d = din("smask", [128, 2 * NT]); rmask_d = din("rmask", [128, 2 * NT])
    hcm_d = din("hcm", [128, 64]); cb_d = din("cb", [128, 256]); ident_d = din("ident", [128, 128]); sel_d = din("sel", [32, 32 * 128])
    w_ada_d = din("w_ada", [L, 1024, 6144]); b_ada_d = din("b_ada", [128, L * 48]); w_in_d = din("w_in", [L, 1024, W_IN])
    dal_d = din("dal", [128, L * 256]); subln_d = din("subln", [128, L])
    convw_d = din("convw", [128, L * 16]); convb_d = din("convb", [128, L * 4]); gw_d = din("gw", [L, 2, 2, 8, 64, 64])
    gb_d = din("gb", [128, L * 16]); rlam_d = din("rlam", [128, L * 8]); hlb_d = din("hlb", [128, 32]); hnorm_d = din("hnorm", [128, L])
    w_br_d = din("w_br", [L, 3, 512, 1024]); w_out_d = din("w_out", [L, 1024, 1024])
    lnp_d = din("lnp", [128, 4 * L * 8])
    rw_d = din("rw", [L, 1024, 32]); rb_d = din("rb", [128, L * 32])
    NE = 32 if "moe" in stages else 1
    if "mix" in stages:
        stages = tuple(stages) + ("att", "rg", "hg")
    w1_d = din("w1d", [L, NE, 1024, 2048]); b1g_d = din("b1g", [128, L * 256]); b1l_d = din("b1l", [128, L * 256])
    w2_d = din("w2", [L, NE, 1024, 1024]); b2_d = din("b2", [L, NE, 1024])

    y_o = dout("y", [NT, 1024]); k_o = dout("ok", [L, NT, 512]); v_o = dout("ov", [L, NT, 512])
    rg_o = dout("org", [L, 32, 128]); hg_o = dout("ohg", [L, 4, 2, 4, 128, 128])

    es_all = ExitStack()

    uid = [0]

    def sb(es, name, shape, dt=F32):
        uid[0] += 1
        name = f"{name}_{uid[0]}"
        return Buf(es.enter_context(nc.sbuf_tensor(name, list(shape), dt)), name)

    def psb(es, name, shape, dt=F32):
        return Buf(es.enter_context(nc.psum_tensor(name, list(shape), dt)), name)

    def barrier():
        for e in ("pe", "act", "dve", "pool", "sp"):
            S.wait_all(e)

    P = es_all
    ps = [psb(P, f"ps{i}", [128, 512]) for i in range(7)]
    psb16 = psb(P, "psb16", [128, 1024], BF16)
    X = [sb(P, f"x{c}", [128, NT]) for c in range(8)]
    U = [sb(P, f"u{c}", [128, NT], BF16) for c in range(8)]
    ident = sb(P, "ident", [128, 128]); identb = sb(P, "identb", [128, 128], BF16)
    ones = sb(P, "ones", [128, 128]); onesb = sb(P, "onesb", [128, 128], BF16)
    mod = sb(P, "mod", [128, L * 48])
    lnp = sb(P, "lnp", [128, 4 * L * 8])
    small = {}
    for nm, dd, w in (("subln", subln_d, L), ("convw", convw_d, L * 16), ("convb", convb_d, L * 4), ("gb", gb_d, L * 16),
                      ("rlam", rlam_d, L * 8), ("hlb", hlb_d, 32), ("hnorm", hnorm_d, L), ("rg0", rg0_d, L * 8),
                      ("hcm", hcm_d, 64), ("rb", rb_d, L * 32), ("b1g", b1g_d, L * 256), ("b1l", b1l_d, L * 256),
                      ("cond", cond_d, 8)):
        small[nm] = sb(P, nm, [128, w])
        S.dma("sp", small[nm][:], dd[:, :])
    S.dma("sp", ident[:], ident_d[:, :]); S.dma("sp", lnp[:], lnp_d[:, :])
    S.ts(small["b1l"][:], small["b1l"][:], 1.0, ALU.add)
    S.copy(identb[:], ident[:])
    S.memset(ones[:], 1.0); S.memset(onesb[:], 1.0)
    lam_neg = sb(P, "lam_neg", [128, L])
    nsp = sb(P, "nsp", [128, L * 8])
    lbv = sb(P, "lbv", [128, 32]); oml = sb(P, "oml", [128, 32])
    subw = sb(P, "subw", [128, L])
    hcmb = small["hcm"]

    with ExitStack() as E0:
        xt = sb(E0, "xt", [128, 8, 1024])
        S.dma("sp", xt[:], x_d.rearrange("(tb p) f -> p tb f", p=128))
        wst = [sb(E0, f"wst{i}", [128, 6144]) for i in range(2)]
        scond = sb(E0, "scond", [128, 8])
        S.act(scond[:], small["cond"][:], AF.Silu)
        badat = sb(E0, "badat", [128, L * 48]); S.dma("sp", badat[:], b_ada_d[:, :])
        for l in range(L):
            for kc in range(8):
                wt = wst[(l * 8 + kc) % 2]
                S.dma("sp", wt[:], w_ada_d[l, kc * 128:(kc + 1) * 128, :])
                for j in range(48):
                    S.mm(ps[0][:, j:j + 1], wt[:, j * 128:(j + 1) * 128], scond[:, kc:kc + 1], start=True, stop=True, signal=(j == 47))
                S.tt(mod[:, l * 48:(l + 1) * 48], ps[0][:, 0:48], (badat if kc == 0 else mod)[:, l * 48:(l + 1) * 48], ALU.add)
        for l in range(L):
            b = l * 48
            S.ts(mod[:, b + 8:b + 16], mod[:, b + 8:b + 16], 1.0, ALU.add)
            S.ts(mod[:, b + 32:b + 40], mod[:, b + 32:b + 40], 1.0, ALU.add)
            S.ts(mod[:, b + 16:b + 24], mod[:, b + 16:b + 24], 1.0 / ALPHA, ALU.mult)
            S.ts(mod[:, b + 40:b + 48], mod[:, b + 40:b + 48], 1.0 / ALPHA, ALU.mult)
        for c in range(8):
            for hf in range(2):
                for i in range(4):
                    tb = hf * 4 + i
                    S.transpose(ps[1 + hf][:, i * 128:(i + 1) * 128], xt[:, tb, c * 128:(c + 1) * 128], ident[:])
                S.copy(X[c][:, hf * 512:(hf + 1) * 512], ps[1 + hf][:], eng=("dve" if hf == 0 else "act"))
        dal = sb(E0, "dal", [128, L * 256]); S.dma("sp", dal[:], dal_d[:, :])
        t2 = sb(E0, "t2s", [128, L * 2 * 64]); t3 = sb(E0, "t3s", [128, L * 2])
        dv = dal.t[:, :].rearrange("p (l a d) -> p l a d", l=L, a=4)
        for l in range(L):
            for a in range(2):
                S.tt(t2[:, (l * 2 + a) * 64:(l * 2 + a + 1) * 64], dal[:, l * 256 + (2 * a) * 64:l * 256 + (2 * a + 1) * 64],
                     dal[:, l * 256 + (2 * a + 1) * 64:l * 256 + (2 * a + 2) * 64], ALU.mult)
                S.reduce(t3[:, l * 2 + a:l * 2 + a + 1], t2[:, (l * 2 + a) * 64:(l * 2 + a + 1) * 64], ALU.add)
        S.act(t3[:], t3[:], AF.Exp)
        for l in range(L):
            li = 0.8 - 0.6 * math.exp(-0.3 * l)
            S.stt(lam_neg[:, l:l + 1], t3[:, 2 * l + 1:2 * l + 2], -li, t3[:, 2 * l:2 * l + 1], ALU.add, ALU.subtract)
            S.ts(subw[:, l:l + 1], small["subln"][:, l:l + 1], 1.0 - li, ALU.mult)
        S.act(nsp[:], small["rlam"][:], AF.Exp, scale=-1.0)
        S.act(nsp[:], nsp[:], AF.Ln, bias=1.0)
        S.ts(nsp[:], nsp[:], -8.0, ALU.mult)
        eh = sb(E0, "eh", [128, 32]); sh_ = sb(E0, "sh_", [128, 8])
        S.act(eh[:], small["hlb"][:], AF.Exp)
        S.tt(sh_[:], eh[:, 0:8], eh[:, 8:16], ALU.add)
        S.tt(sh_[:], sh_[:], eh[:, 16:24], ALU.add)
        S.tt(sh_[:], sh_[:], eh[:, 24:32], ALU.add)
        S.op("dve", lambda: nc.vector.reciprocal(out=sh_.t[:], in_=sh_.t[:]), [sh_[:]], [sh_[:]])
        for l in range(4):
            S.tt(eh[:, l * 8:(l + 1) * 8], eh[:, l * 8:(l + 1) * 8], sh_[:], ALU.mult)
        S.memset(lbv[:, 0:8], 0.0)
        S.copy(lbv[:, 8:16], eh[:, 8:16])
        S.tt(lbv[:, 16:24], lbv[:, 8:16], eh[:, 16:24], ALU.add)
        S.tt(lbv[:, 24:32], lbv[:, 16:24], eh[:, 24:32], ALU.add)
        S.ts(oml[:], lbv[:], -1.0, ALU.mult, 1.0, ALU.add)
        barrier()

    def layer_norm(l, which, E):
        gcol = (2 * which) * L * 8 + l * 8
        bcol = (2 * which + 1) * L * 8 + l * 8
        sq = [sb(E, f"lnsq{which}_{i}", [128, 512]) for i in range(2)]
        mean = sb(E, f"lnmean{which}", [128, 512]); rstd = sb(E, f"lnrstd{which}", [128, 512]); tmp = sb(E, f"lntmp{which}", [128, 512])
        for th in range(2):
            sl = slice(th * 512, (th + 1) * 512)
            for c in range(8):
                S.mm(ps[0][:], ones[:], X[c][:, sl], start=(c == 0), stop=(c == 7))
            for c in range(8):
                S.act(sq[c % 2][:], X[c][:, sl], AF.Square)
                S.mm(ps[1][:], ones[:], sq[c % 2][:], start=(c == 0), stop=(c == 7), signal=True)
            S.ts(mean[:], ps[0][:], 1.0 / 1024, ALU.mult)
            S.tt(tmp[:], mean[:], mean[:], ALU.mult)
            S.stt(rstd[:], ps[1][:], 1.0 / 1024, tmp[:], ALU.mult, ALU.subtract)
            S.act(rstd[:], rstd[:], AF.Sqrt, bias=EPS_LN)
            S.op("dve", lambda: nc.vector.reciprocal(out=rstd.t[:], in_=rstd.t[:]), [rstd[:]], [rstd[:]])
            for c in range(8):
                S.tt(X[c][:, sl], X[c][:, sl], mean[:], ALU.subtract)
                S.tt(X[c][:, sl], X[c][:, sl], rstd[:], ALU.mult)
                S.ts(X[c][:, sl], X[c][:, sl], lnp[:, gcol + c:gcol + c + 1], ALU.mult, lnp[:, bcol + c:bcol + c + 1], ALU.add)

    def modulate(l, second):
        b = l * 48 + (24 if second else 0)
        for c in range(8):
            S.ts(U[c][:], X[c][:], mod[:, b + 8 + c:b + 9 + c], ALU.mult, mod[:, b + c:b + c + 1], ALU.add)

    wq_rr = [0]

    def load_w_cols(wbufs, src_rows_ap, c0, ncols):
        wb = wbufs[wq_rr[0] % len(wbufs)]
        wq_rr[0] += 1
        S.dma("pool", wb[:, :, 0:ncols], src_rows_ap.rearrange("(kc p) c -> p kc c", p=128)[:, :, c0:c0 + ncols])
        return wb

    def proj_fm(wb, oc, th, out_ps, nk=8, rhs=None):
        rhs = rhs or U
        for kc in range(nk):
            S.mm(out_ps, wb[:, kc, oc * 128:(oc + 1) * 128], rhs[kc][:, th * 512:(th + 1) * 512], start=(kc == 0), stop=(kc == nk - 1))

    def proj_tm(wb, tb, out_ps, ncols=512):
        for kc in range(8):
            S.mm(out_ps, U[kc][:, tb * 128:(tb + 1) * 128], wb[:, kc, 0:ncols], start=(kc == 0), stop=(kc == 7))

    for l in range(L):
        li = 0.8 - 0.6 * math.exp(-0.3 * l)
        modulate(l, False)
        with ExitStack() as EM:
            BR = [[sb(EM, f"br{n}_{c}", [128, NT], BF16) for c in range(4)] for n in range(3)]
            wbufs = [sb(EM, f"wcol{i}", [128, 8, 512], BF16) for i in range(2)]
            if True:
                with ExitStack() as EA:
                  if "att" in stages:
                    Q = [[sb(EA, f"q{h}_{m}", [128, NT], BF16) for m in range(2)] for h in range(4)]
                    K = [[sb(EA, f"k{h}_{m}", [128, 1280], BF16) for m in range(2)] for h in range(4)]
                    for h in range(4 if "noM" not in stages else 0):
                        for m in range(2):
                            oth = slice(64, 128) if m == 0 else slice(0, 64)
                            mrow = slice(64, 72) if m == 0 else slice(0, 8)
                            S.memset(Q[h][m][oth, :], 0.0); S.memset(K[h][m][oth, :], 0.0)
                            S.dma("pool", Q[h][m][mrow, :], amq_d[:, :]); S.dma("pool", K[h][m][mrow, :], amk_d[:, :])
                    V = [sb(EA, f"v{t}", [128, 512], BF16) for t in range(10)]
                    rc = sb(EA, "ropec", [128, NT]); rs = sb(EA, "ropes", [128, NT]); perm = sb(EA, "perm", [128, 128], BF16)
                    S.dma("sp", rc[:], ropec_d[:, :]); S.dma("sp", rs[:], ropes_d[:, :]); S.dma("pool", perm[:], perm_d[:, :])
                    qb = [sb(EA, f"qb{i}", [128, 512], BF16) for i in range(2)]
                    t1 = [sb(EA, f"at1{i}", [128, 512]) for i in range(2)]
                    t2 = [sb(EA, f"at2{i}", [128, 512]) for i in range(2)]
                    stg = [sb(EA, f"stg{i}", [128, 512]) for i in range(2)]
                    for which, c0, DST in (((0, C_Q, Q), (1, C_K, K)) if "noA1" not in stages else ()):
                        wb = load_w_cols(wbufs, w_in_d[l], c0, 512)
                        for h in range(4):
                            for th in range(2):
                                i = th
                                sl = slice(th * 512, (th + 1) * 512)
                                proj_fm(wb, h, th, ps[th][:])
                                if "noR" in stages:
                                    S.copy(t1[i][:], ps[th][:], eng="act")
                                    S.memset(t2[i][:], 0.0)
                                else:
                                    S.copy(qb[i][:], ps[th][:], eng="act")
                                    S.op("dve", lambda: nc.vector.tensor_tensor(out=t1[i].t[:], in0=ps[th].t[:], in1=rc.t[:, sl], op=ALU.mult),
                                         [ps[th][:], rc[:, sl], qb[i][:]], [t1[i][:]])
                                    if "R1" in stages:
                                        S.memset(t2[i][:], 0.0)
                                    else:
                                        S.mm(ps[2 + th][:], perm[:], qb[i][:])
                                        S.tt(t2[i][:], ps[2 + th][:], rs[:, sl], ALU.mult)
                                S.tt(DST[h][0][0:64, sl], t1[i][0:64, :], t2[i][0:64, :], ALU.add)
                                S.tt(DST[h][1][64:128, sl], t1[i][64:128, :], t2[i][64:128, :], ALU.add)
                    wbk = load_w_cols(wbufs, w_in_d[l], C_K, 512)
                    NA2 = 8 if "noA2" not in stages else 0
                    for tb in range(NA2):
                        proj_tm(wbk, tb, ps[tb % 2][:])
                        S.copy(stg[tb % 2][:], ps[tb % 2][:], eng="act")
                        S.dma("sp", k_o[l, tb * 128:(tb + 1) * 128, :], stg[tb % 2][:])
                    wbv = load_w_cols(wbufs, w_in_d[l], C_V, 512)
                    for tb in range(NA2):
                        proj_tm(wbv, tb, ps[tb % 2][:])
                        S.copy(stg[tb % 2][:], ps[tb % 2][:], eng="act")
                        S.copy(V[tb][:], stg[tb % 2][:])
                        S.dma("sp", v_o[l, tb * 128:(tb + 1) * 128, :], stg[tb % 2][:])
                    NA3 = 2 if "noA3" not in stages else 0
                    for blk in range(NA3):
                        S.dma("sp", stg[blk][:], kctx_d[l, blk * 128:(blk + 1) * 128, :])
                        for h in range(4):
                            S.transpose(ps[2][:, h * 128:(h + 1) * 128], stg[blk][:, h * 128:(h + 1) * 128], ident[:])
                        for h in range(4):
                            S.copy(K[h][0][0:64, 1024 + blk * 128:1024 + (blk + 1) * 128], ps[2][0:64, h * 128:(h + 1) * 128])
                            S.copy(K[h][1][64:128, 1024 + blk * 128:1024 + (blk + 1) * 128], ps[2][64:128, h * 128:(h + 1) * 128])
                    for blk in range(NA3):
                        S.dma("pool", V[8 + blk][:], vctx_d[l, blk * 128:(blk + 1) * 128, :])
                    pT = [sb(EA, f"pT{i}", [128, 512], BF16) for i in range(3)]
                    o_a = sb(EA, "o_a", [128, 512]); o_b = sb(EA, "o_b", [128, 512]); rcp = sb(EA, "rcp", [128, 512]); sqa = sb(EA, "sqa", [128, 512])
                    pi = 0
                    for h in range(4 if "noattcore" not in stages else 0):
                        for qh in range(2):
                            qs = slice(qh * 512, (qh + 1) * 512)
                            for m in range(2):
                                ms = slice(m * 64, (m + 1) * 64)
                                for kb in range(10):
                                    sc = ps[4 + (kb % 2)]
                                    S.mm(sc[:], K[h][m][:, kb * 128:(kb + 1) * 128], Q[h][m][:, qs])
                                    p_ = pT[pi % 3]; pi += 1
                                    S.act(p_[:], sc[:], AF.Exp, scale=0.125)
                                    S.mm(ps[2 * m][:], V[kb][:, h * 128:(h + 1) * 128], p_[:], start=(kb == 0), stop=(kb == 9), signal=True)
                                    S.mm(ps[2 * m + 1][:], onesb[:], p_[:], start=(kb == 0), stop=(kb == 9), signal=True)
                            S.op("dve", lambda: nc.vector.reciprocal(out=rcp.t[:], in_=ps[1].t[:]), [ps[1][:]], [rcp[:]])
                            S.tt(o_a[:], ps[0][:], rcp[:], ALU.mult)
                            S.op("dve", lambda: nc.vector.reciprocal(out=rcp.t[:], in_=ps[3].t[:]), [ps[3][:]], [rcp[:]])
                            S.tt(o_b[:], ps[2][:], rcp[:], ALU.mult)
                            S.stt(o_a[:], o_b[:], lam_neg[:, l:l + 1], o_a[:], ALU.mult, ALU.add)
                            S.act(sqa[:], o_a[:], AF.Square)
                            S.mm(ps[6][:], ones[:], sqa[:])
                            S.act(rcp[:], ps[6][:], AF.Sqrt, scale=1.0 / 128, bias=EPS)
                            S.op("dve", lambda: nc.vector.reciprocal(out=rcp.t[:], in_=rcp.t[:]), [rcp[:]], [rcp[:]])
                            S.tt(o_a[:], o_a[:], rcp[:], ALU.mult)
                            S.ts(BR[0][h][:, qs], o_a[:], subw[:, l:l + 1], ALU.mult)
                    barrier()
                with ExitStack() as EB:
                  if "rg" in stages:
                    cm = sb(EB, "cmaskt", [128, 3 * NT], BF16); S.dma("pool", cm[:], cmask_d[:, :])
                    sm = sb(EB, "smaskt", [128, 2 * NT], BF16); S.dma("pool", sm[:], smask_d[:, :])
                    gwt = sb(EB, "gwt", [128, 16 * 128], BF16)
                    S.memset(gwt[:], 0.0)
                    for d in range(2):
                        for g in range(2):
                            for n in range(8):
                                c, half = n // 2, n % 2
                                col = ((d * 2 + g) * 4 + c) * 128 + half * 64
                                S.dma("pool", gwt[half * 64:(half + 1) * 64, col:col + 64], gw_d[l, d, g, n, :, :])
                    rx = sb(EB, "rx", [128, NT]); xr = sb(EB, "xr", [128, NT]); xrb = sb(EB, "xrb", [128, NT], BF16)
                    tmp = sb(EB, "rtmp", [128, NT]); rr = sb(EB, "rr", [128, NT]); ii = sb(EB, "ii", [128, NT])
                    aa = sb(EB, "aa", [128, NT]); bb = sb(EB, "bb", [128, NT]); hh = [sb(EB, f"hh{d}", [128, NT]) for d in range(2)]
                    gg = sb(EB, "gg", [128, NT]); ge = sb(EB, "ge", [128, NT])
                    rgfin = sb(EB, "rgfin", [128, 32])
                    wbx = load_w_cols(wbufs, w_in_d[l], C_RX, 512)
                    wbg = load_w_cols(wbufs, w_in_d[l], C_RG, 512)
                    rgv = rgfin.t[:, :].rearrange("p (s d c) -> p s d c", s=4, d=2)
                    for c in range(4):
                        for th in range(2):
                            proj_fm(wbx, c, th, ps[th][:])
                            S.copy(rx[:, th * 512:(th + 1) * 512], ps[th][:], eng="act")
                        cw = lambda tap: small["convw"][:, l * 16 + tap * 4 + c:l * 16 + tap * 4 + c + 1]
                        S.ts(xr[:], rx[:], cw(2), ALU.mult, small["convb"][:, l * 4 + c:l * 4 + c + 1], ALU.add)
                        S.tt(tmp[:, 2:NT], rx[:, 0:NT - 2], cm[:, 2:NT], ALU.mult)
                        S.stt(xr[:, 2:NT], tmp[:, 2:NT], cw(0), xr[:, 2:NT], ALU.mult, ALU.add)
                        S.tt(tmp[:, 1:NT], rx[:, 0:NT - 1], cm[:, NT + 1:2 * NT], ALU.mult)
                        S.stt(xr[:, 1:NT], tmp[:, 1:NT], cw(1), xr[:, 1:NT], ALU.mult, ALU.add)
                        S.tt(tmp[:, 0:NT - 1], rx[:, 1:NT], cm[:, 2 * NT:3 * NT - 1], ALU.mult)
                        S.stt(xr[:, 0:NT - 1], tmp[:, 0:NT - 1], cw(3), xr[:, 0:NT - 1], ALU.mult, ALU.add)
                        S.copy(xrb[:], xr[:], eng="act")
                        for d in range(2):
                            for g, dst in ((0, rr), (1, ii)):
                                col = ((d * 2 + g) * 4 + c) * 128
                                bcol = l * 16 + (d * 2 + g) * 4 + c
                                for th in range(2):
                                    S.mm(ps[2 + th][:], gwt[:, col:col + 128], xrb[:, th * 512:(th + 1) * 512])
                                    S.act(dst[:, th * 512:(th + 1) * 512], ps[2 + th][:], AF.Sigmoid, bias=small["gb"][:, bcol:bcol + 1])
                            ncol = l * 8 + d * 4 + c
                            S.act(aa[:], rr[:], AF.Exp, scale=nsp[:, ncol:ncol + 1])
                            S.tt(bb[:], aa[:], aa[:], ALU.mult)
                            S.act(bb[:], bb[:], AF.Sqrt, scale=-1.0, bias=1.0)
                            S.tt(bb[:], bb[:], ii[:], ALU.mult)
                            S.tt(bb[:], bb[:], xr[:], ALU.mult)
                            S.tt(aa[:], aa[:], sm[:, d * NT:(d + 1) * NT], ALU.mult)
                            h0 = small["rg0"][:, ncol:ncol + 1]
                            if d == 0:
                                S.scan(hh[0][:], aa[:], bb[:], h0, ALU.mult, ALU.add)
                                S.copy(View(rgfin, rgv[:, :, 0, c]), hh[0][:, 255:NT:256])
                            else:
                                S.scan(hh[1][:, ::-1], aa[:, ::-1], bb[:, ::-1], h0, ALU.mult, ALU.add)
                                S.copy(View(rgfin, rgv[:, :, 1, c]), hh[1][:, 0:NT:256])
                        S.tt(hh[0][:], hh[0][:], hh[1][:], ALU.add)
                        for th in range(2):
                            proj_fm(wbg, c, th, ps[th][:])
                            S.copy(gg[:, th * 512:(th + 1) * 512], ps[th][:], eng="act")
                        S.tt(ge[:], gg[:], gg[:], ALU.mult)
                        S.ts(ge[:], ge[:], 0.044715, ALU.mult, 1.0, ALU.add)
                        S.tt(ge[:], ge[:], gg[:], ALU.mult)
                        S.act(ge[:], ge[:], AF.Sigmoid, scale=2.0 * math.sqrt(2.0 / math.pi))
                        S.tt(ge[:], ge[:], gg[:], ALU.mult)
                        S.tt(BR[1][c][:], hh[0][:], ge[:], ALU.mult)
                    S.transpose(ps[4][0:32, 0:128], rgfin[:], ident[:])
                    rgo = sb(EB, "rgo", [32, 128]); S.copy(rgo[:], ps[4][0:32, 0:128])
                    S.dma("sp", rg_o[l, :, :], rgo[:])
                    barrier()
                with ExitStack() as EC:
                  if "hg" in stages:
                    rm = sb(EC, "rmaskt", [128, 2 * NT], BF16); S.dma("pool", rm[:], rmask_d[:, :])
                    cbm = sb(EC, "cbm", [128, 256], BF16); S.dma("pool", cbm[:], cb_d[:, :])
                    HI = [sb(EC, f"hi{t}", [128, 512], BF16) for t in range(8)]
                    HIc = [sb(EC, f"hic{n}", [32, 512], BF16) for n in range(32)]
                    wbi = wbufs[0]
                    S.dma("pool", wbi[:, :, 0:512], w_in_d[l].rearrange("(kc p) c -> p kc c", p=128)[:, :, C_HI:C_HI + 512])
                    for tb in range(8):
                        proj_tm(wbi, tb, ps[tb % 2][:])
                        S.copy(HI[tb][:], ps[tb % 2][:], eng=("act" if tb % 2 else "dve"))
                    for n in range(32):
                        for kc in range(8):
                            S.mm(ps[2 + n % 2][0:32, :], U[kc][:, n * 32:(n + 1) * 32], wbi[:, kc, 0:512], start=(kc == 0), stop=(kc == 7))
                        S.copy(HIc[n][:], ps[2 + n % 2][0:32, :], eng=("act" if n % 2 else "dve"))
                    wh = [Buf(wbufs[1].t[:, :, i * 128:(i + 1) * 128], f"wh{i}") for i in range(4)]
                    qh = sb(EC, "qh", [128, NT]); osum = sb(EC, "osum", [128, NT]); sg = sb(EC, "sg", [128, NT])
                    gl = sb(EC, "gl", [128, NT]); kk = sb(EC, "kk", [128, NT]); bc = sb(EC, "bc", [128, NT]); ex = sb(EC, "ex", [128, NT])
                    kt = sb(EC, "kt", [128, NT], BF16); kh = sb(EC, "kh", [128, NT], BF16)
                    qt = [sb(EC, f"qt{d}", [128, NT], BF16) for d in range(2)]
                    Dv = [sb(EC, f"Dv{d}", [128, 32]) for d in range(2)]; Dcm = [sb(EC, f"Dcm{d}", [128, 32]) for d in range(2)]
                    KHc = [[sb(EC, f"khc{d}_{n}", [32, 128], BF16) for n in range(32)] for d in range(2)]
                    AT = [[sb(EC, f"AT{d}_{t}", [128, 128], BF16) for t in range(8)] for d in range(2)]
                    S32 = [sb(EC, f"S32{d}", [128, 128]) for d in range(2)]; Sb = [sb(EC, f"Sb{d}", [128, 128], BF16) for d in range(2)]
                    for h in range(4):
                        for i, c0 in enumerate((C_HQ, C_HZF, C_HZB, C_HG)):
                            S.dma("pool", wh[i][:], w_in_d[l].rearrange("(kc p) c -> p kc c", p=128)[:, :, c0 + h * 128:c0 + (h + 1) * 128])
                        for th in range(2):
                            proj_fm(wh[0], 0, th, ps[4 + th][:])
                            S.act(qh[:, th * 512:(th + 1) * 512], ps[4 + th][:], AF.Silu)
                        S.memset(osum[:], 0.0)
                        for d in range(2):
                            lc = l * 8 + d * 4 + h
                            for th in range(2):
                                proj_fm(wh[1 + d], 0, th, ps[4 + th][:])
                                S.act(sg[:, th * 512:(th + 1) * 512], ps[4 + th][:], AF.Sigmoid)
                            S.ts(gl[:], sg[:], oml[:, lc:lc + 1], ALU.mult, lbv[:, lc:lc + 1], ALU.add)
                            S.act(gl[:], gl[:], AF.Ln)
                            S.ts(kk[:], sg[:], oml[:, lc:lc + 1], ALU.mult)
                            S.ts(kk[:], kk[:], -1.0, ALU.mult)
                            S.ts(kk[:], kk[:], oml[:, lc:lc + 1], ALU.add)
                            if d == 0:
                                S.scan(bc[:], rm[:, 0:NT], gl[:], 0.0, ALU.mult, ALU.add)
                                S.act(Dv[d][:], bc[:, 31:NT:32], AF.Exp)
                            else:
                                S.scan(bc[:, ::-1], rm[:, 2 * NT - 1:NT - 1:-1], gl[:, ::-1], 0.0, ALU.mult, ALU.add)
                                S.act(Dv[d][:], bc[:, 0:NT:32], AF.Exp)
                            S.act(ex[:], bc[:], AF.Exp)
                            S.tt(qt[d][:], qh[:], ex[:], ALU.mult)
                            S.act(ex[:], bc[:], AF.Exp, scale=-1.0)
                            S.tt(ex[:], kk[:], ex[:], ALU.mult)
                            S.copy(kt[:], ex[:], eng="act")
                            for n in range(32):
                                S.ts(kh[:, n * 32:(n + 1) * 32], ex[:, n * 32:(n + 1) * 32], Dv[d][:, n:n + 1], ALU.mult)
                            S.tt(Dcm[d][:], Dv[d][:], hcmb[:, d * 32:(d + 1) * 32], ALU.mult)
                            for n in range(32):
                                S.transpose(psb16[0:32, (n % 8) * 128:(n % 8 + 1) * 128], kh[:, n * 32:(n + 1) * 32], identb[:])
                                S.copy(KHc[d][n][:], psb16[0:32, (n % 8) * 128:(n % 8 + 1) * 128], eng="act")
                            for tb in range(8):
                                pa = ps[6]
                                S.mm(pa[:, (tb % 4) * 128:(tb % 4 + 1) * 128], kt[:, tb * 128:(tb + 1) * 128], qt[d][:, tb * 128:(tb + 1) * 128])
                                S.tt(AT[d][tb][:], pa[:, (tb % 4) * 128:(tb % 4 + 1) * 128], cbm[:, d * 128:(d + 1) * 128], ALU.mult)
                            S.dma("sp", S32[d][:], hg0_d[l, d, h, :, :])
                            first = 0 if d == 0 else 31
                            S.ts(Sb[d][:], S32[d][:], hcmb[:, d * 32 + first:d * 32 + first + 1], ALU.mult)
                        for step in range(32):
                            for d in range(2):
                                po = ps[d]
                                n = step if d == 0 else 31 - step
                                tb, j = n // 4, n % 4
                                bs = slice((tb % 4) * 128, (tb % 4 + 1) * 128)
                                blk_first = (j == 0) if d == 0 else (j == 3)
                                blk_last = (j == 3) if d == 0 else (j == 0)
                                if blk_first:
                                    S.mm(po[:, bs], HI[tb][:, h * 128:(h + 1) * 128], AT[d][tb][:], start=True, stop=False, signal=True)
                                S.mm(po[:, (tb % 4) * 128 + j * 32:(tb % 4) * 128 + (j + 1) * 32], Sb[d][:], qt[d][:, n * 32:(n + 1) * 32],
                                     start=False, stop=blk_last, signal=True)
                                pk = ps[2 + d]
                                S.mm(pk[:, 0:128], KHc[d][n][:], HIc[n][:, h * 128:(h + 1) * 128])
                                S.stt(S32[d][:], S32[d][:], Dcm[d][:, n:n + 1], pk[:, 0:128], ALU.mult, ALU.add)
                                seq_end = (n % 8 == 7) if d == 0 else (n % 8 == 0)
                                if seq_end:
                                    S.dma("sp", hg_o[l, n // 8, d, h, :, :], S32[d][:])
                                nn = n + 1 if d == 0 else n - 1
                                if 0 <= nn < 32:
                                    S.ts(Sb[d][:], S32[d][:], hcmb[:, d * 32 + nn:d * 32 + nn + 1], ALU.mult)
                                if blk_last:
                                    ts_ = slice(tb * 128, (tb + 1) * 128)
                                    S.tt(osum[:, ts_], osum[:, ts_], po[:, bs], ALU.add)
                        for th in range(2):
                            sl = slice(th * 512, (th + 1) * 512)
                            proj_fm(wh[3], 0, th, ps[4 + th][:])
                            S.act(sg[:, sl], ps[4 + th][:], AF.Silu)
                            S.act(gl[:, 0:512], osum[:, sl], AF.Square)
                            S.mm(ps[6][:], ones[:], gl[:, 0:512])
                            S.act(gl[:, 512:1024], ps[6][:], AF.Sqrt, scale=1.0 / 128, bias=EPS)
                            S.op("dve", lambda: nc.vector.reciprocal(out=gl.t[:, 512:1024], in_=gl.t[:, 512:1024]), [gl[:, 512:1024]], [gl[:, 512:1024]])
                            S.tt(gl[:, 512:1024], gl[:, 512:1024], osum[:, sl], ALU.mult)
                            S.ts(gl[:, 512:1024], gl[:, 512:1024], small["hnorm"][:, l:l + 1], ALU.mult)
                            S.tt(BR[2][h][:, sl], gl[:, 512:1024], sg[:, sl], ALU.mult)
                    barrier()
            with ExitStack() as ED:
                wbr = sb(ED, "wbr", [128, 12, 1024], BF16)
                S.dma("pool", wbr[:], w_br_d[l].rearrange("n (kc p) c -> p (n kc) c", p=128))
                macc = [sb(ED, f"macc{c}", [128, NT]) for c in range(8)]
                gsb = sb(ED, "gsb", [128, 512]); gp = sb(ED, "gp", [128, 512])
                for n in range(3):
                    for og in range(2):
                        wb = load_w_cols(wbufs, w_in_d[l], C_MG + n * 1024 + og * 512, 512)
                        for oi in range(4):
                            oc = og * 4 + oi
                            for th in range(2):
                                sl = slice(th * 512, (th + 1) * 512)
                                proj_fm(wb, oi, th, ps[th][:])
                                S.act(gsb[:], ps[th][:], AF.Sigmoid)
                                for kc in range(4):
                                    S.mm(ps[2 + th][:], wbr[:, n * 4 + kc, oc * 128:(oc + 1) * 128], BR[n][kc][:, sl], start=(kc == 0), stop=(kc == 3))
                                if n == 0:
                                    S.tt(macc[oc][:, sl], gsb[:], ps[2 + th][:], ALU.mult)
                                else:
                                    S.tt(gp[:], gsb[:], ps[2 + th][:], ALU.mult)
                                    S.tt(macc[oc][:, sl], macc[oc][:, sl], gp[:], ALU.add)
                mb = [sb(ED, f"mb{c}", [128, NT], BF16) for c in range(8)]
                for c in range(8):
                    S.copy(mb[c][:], macc[c][:], eng=("act" if c % 2 else "dve"))
                for og in range(2):
                    wb = load_w_cols(wbufs, w_out_d[l], og * 512, 512)
                    for oi in range(4):
                        oc = og * 4 + oi
                        for th in range(2):
                            sl = slice(th * 512, (th + 1) * 512)
                            proj_fm(wb, oi, th, ps[th][:], rhs=mb)
                            S.stt(X[oc][:, sl], ps[th][:], mod[:, l * 48 + 16 + oc:l * 48 + 17 + oc], X[oc][:, sl], ALU.mult, ALU.add)
                layer_norm(l, 0, ED)
                barrier()
        barrier()
        modulate(l, True)
        with ExitStack() as EE:
            if "moe" in stages:
                lg = sb(EE, "lg", [128, 256]); gate = sb(EE, "gate", [128, 256]); m8 = sb(EE, "m8", [128, 8]); nm = sb(EE, "nm", [128, 1])
                msk = sb(EE, "msk", [128, 32]); den = sb(EE, "den", [128, 1])
                gT = sb(EE, "gT", [32, NT])
                rbt = sb(EE, "rbt", [128, 256])
                for tb in range(8):
                    S.copy(rbt[:, tb * 32:(tb + 1) * 32], small["rb"][:, l * 32:(l + 1) * 32])
                ER = ExitStack()
                u2f = [sb(ER, f"u2f{i}", [128, NT]) for i in range(2)]
                rwt = sb(ER, "rwt", [128, 8, 32]); S.dma("sp", rwt[:], rw_d[l].rearrange("(kc p) e -> p kc e", p=128))
                b = l * 48 + 24
                for c in range(8):
                    uf = u2f[c % 2]
                    S.ts(uf[:], X[c][:], mod[:, b + 8 + c:b + 9 + c], ALU.mult, mod[:, b + c:b + c + 1], ALU.add)
                    for tb in range(8):
                        S.mm(ps[0][:, tb * 32:(tb + 1) * 32], uf[:, tb * 128:(tb + 1) * 128], rwt[:, c, :], start=True, stop=True, signal=(tb == 7))
                    S.tt(lg[:], ps[0][:, 0:256], (rbt if c == 0 else lg)[:], ALU.add)
                for tb in range(8):
                    lt = lg[:, tb * 32:(tb + 1) * 32]
                    S.op("dve", lambda: nc.vector.max(out=m8.t[:], in_=lg.t[:, tb * 32:(tb + 1) * 32]), [lt], [m8[:]])
                    S.ts(nm[:], m8[:, 0:1], -1.0, ALU.mult)
                    S.ts(msk[:], lt, m8[:, 3:4], ALU.is_ge)
                    gt_ = gate[:, tb * 32:(tb + 1) * 32]
                    S.act(gt_, lt, AF.Exp, bias=nm[:])
                    S.tt(gt_, gt_, msk[:], ALU.mult)
                    S.reduce(den[:], gt_, ALU.add)
                    S.op("dve", lambda: nc.vector.reciprocal(out=den.t[:], in_=den.t[:]), [den[:]], [den[:]])
                    S.ts(gt_, gt_, den[:], ALU.mult)
                    if tb < 4:
                        S.transpose(ps[1][0:32, tb * 128:(tb + 1) * 128], gt_, ident[:])
                S.copy(gT[:, 0:512], ps[1][0:32, 0:512]);
                for tb in range(4, 8):
                    pass
                for tb in range(4, 8):
                    S.transpose(ps[2][0:32, (tb - 4) * 128:(tb - 3) * 128], gate[:, tb * 32:(tb + 1) * 32], ident[:])
                S.copy(gT[:, 512:1024], ps[2][0:32, 0:512])
                barrier()
                ER.close()
                b2t = sb(EE, "b2t", [32, 1024]); S.dma("sp", b2t[:], b2_d[l, :, :])
                selt = sb(EE, "selt", [32, 32 * 128], BF16); S.dma("pool", selt[:], sel_d[:, :])
                gTb = sb(EE, "gTb", [32, NT], BF16); S.copy(gTb[:], gT[:])
                g2c = l * 48 + 40
                for dc in range(8):
                    for th in range(2):
                        sl = slice(th * 512, (th + 1) * 512)
                        S.mm(ps[3 + th][:], b2t[:, dc * 128:(dc + 1) * 128], gT[:, sl])
                        S.stt(X[dc][:, sl], ps[3 + th][:], mod[:, g2c + dc:g2c + dc + 1], X[dc][:, sl], ALU.mult, ALU.add)
                ring = [sb(EE, f"wring{i}", [128, 8, 1024], BF16) for i in range(4)]
                ACTB = [[sb(EE, f"actb{i}_{j}", [128, NT], BF16) for j in range(8)] for i in range(2)]
                gbc = [sb(EE, "gbc0", [128, NT])] * 2
                NR = 3
                Gb = [sb(EE, f"Gb{i}", [128, 512], BF16) for i in range(NR)]; Lb = [sb(EE, f"Lb{i}", [128, 512], BF16) for i in range(NR)]
                sgm = [sb(EE, f"sgm{i}", [128, 512], BF16) for i in range(NR)]
                t1m = [sb(EE, f"t1m{i}", [128, 512], BF16) for i in range(NR)]; t2m = [sb(EE, f"t2m{i}", [128, 512], BF16) for i in range(NR)]
                rr_ = [0]

                def load_piece(src):
                    wb = ring[rr_[0] % 4]; rr_[0] += 1
                    S.dma("pool", wb[:], src)
                    return wb

                def pieces(e):
                    v1 = w1_d[l, e].rearrange("(kc p) c -> p kc c", p=128)
                    return (load_piece(v1[:, :, 0:1024]), load_piece(v1[:, :, 1024:2048]),
                            load_piece(w2_d[l, e].rearrange("(kc p) c -> p kc c", p=128)))

                nxt = pieces(0)
                it = 0
                for e in range(n_exp):
                    wg, wl, w2b = nxt
                    ab = ACTB[e % 2]
                    for th in range(2):
                        S.mm(ps[5][:], selt[:, e * 128:(e + 1) * 128], gTb[:, th * 512:(th + 1) * 512])
                        S.copy(gbc[e % 2][:, th * 512:(th + 1) * 512], ps[5][:], eng="act")
                    bcol = (l * 32 + e) * 8
                    for j in range(8):
                        for th in range(2):
                            sl = slice(th * 512, (th + 1) * 512)
                            i = it % 2; r = it % NR; it += 1
                            proj_fm(wg, j, th, ps[2 * i][:])
                            proj_fm(wl, j, th, ps[2 * i + 1][:])
                            S.act(Gb[r][:], ps[2 * i][:], AF.Identity, bias=small["b1g"][:, bcol + j:bcol + j + 1])
                            S.act(Lb[r][:], ps[2 * i + 1][:], AF.Identity, bias=small["b1l"][:, bcol + j:bcol + j + 1])
                            S.ts(Gb[r][:], Gb[r][:], 7.0, ALU.min)
                            S.act(sgm[r][:], Gb[r][:], AF.Sigmoid, scale=1.702)
                            S.ts(Lb[r][:], Lb[r][:], 8.0, ALU.min, -6.0, ALU.max)
                            S.tt(t2m[r][:], Lb[r][:], gbc[e % 2][:, sl], ALU.mult, eng="pool")
                            S.tt(t1m[r][:], Gb[r][:], sgm[r][:], ALU.mult, eng="pool")
                            S.tt(ab[j][:, sl], t1m[r][:], t2m[r][:], ALU.mult)
                    if e + 1 < n_exp:
                        nxt = pieces(e + 1)
                    for dc in range(8):
                        for th in range(2):
                            sl = slice(th * 512, (th + 1) * 512)
                            i = it % 2; it += 1
                            proj_fm(w2b, dc, th, ps[2 * i][:], rhs=ab)
                            S.stt(X[dc][:, sl], ps[2 * i][:], mod[:, g2c + dc:g2c + dc + 1], X[dc][:, sl], ALU.mult, ALU.add)
            layer_norm(l, 1, EE)
            barrier()

    with ExitStack() as EF:
        yo = [sb(EF, f"yo{i}", [128, 1024]) for i in range(2)]
        for tb in range(8):
            for hf in range(2):
                for i in range(4):
                    c = hf * 4 + i
                    S.transpose(ps[hf][:, i * 128:(i + 1) * 128], X[c][:, tb * 128:(tb + 1) * 128], ident[:])
                S.copy(yo[tb % 2][:, hf * 512:(hf + 1) * 512], ps[hf][:], eng=("act" if hf else "dve"))
            S.dma("sp", y_o[tb * 128:(tb + 1) * 128, :], yo[tb % 2][:])
        barrier()
    es_all.close()
    return nc


def _cols(a, L):
    a = np.asarray(a, np.float32)
    lead = a.shape[:-1]
    n = a.shape[-1] // 128
    a = a.reshape(*lead, n, 128)
    a = np.moveaxis(a, -1, 0)
    return np.ascontiguousarray(a.reshape(128, -1))


def _structural(role):
    t = np.arange(NT)
    BIG = 32768.0
    amk = np.zeros((8, 1280), np.float32); amq = np.zeros((8, 1024), np.float32)
    if role == 1:
        amk[0, :] = -BIG; amq[0, :] = 1.0
        for g in range(4):
            amk[1 + g, g * 256:(g + 1) * 256] = BIG
            amq[1 + g, g * 256:(g + 1) * 256] = 1.0
    cos = np.ones((128, NT), np.float32); sin = np.zeros((128, NT), np.float32)
    perm = np.zeros((128, 128), np.float32)
    inv = 10000.0 ** (-np.arange(0, 32, 2, dtype=np.float32) / 32)
    row = (t // 64).astype(np.float32); col = (t % 64).astype(np.float32)
    for p in range(128):
        d = p % 64
        pos = row if d < 32 else col
        dd = d % 32
        j = dd % 16
        first = dd < 16
        partner = p + 16 if first else p - 16
        perm[partner, p] = 1.0
        if role == 0:
            ang = pos * inv[j]
            cos[p] = np.cos(ang)
            sin[p] = -np.sin(ang) if first else np.sin(ang)
    seg = (t % 256) if role == 1 else t
    seglen = 256 if role == 1 else NT
    cm = np.ones((3, NT), np.float32)
    cm[0, seg < 2] = 0; cm[1, seg < 1] = 0; cm[2, seg == seglen - 1] = 0
    smk = np.ones((2, NT), np.float32)
    if role == 1:
        smk[0, seg == 0] = 0; smk[1, seg == seglen - 1] = 0
    rmk = np.ones((2, NT), np.float32)
    rmk[0, t % 32 == 0] = 0; rmk[1, t % 32 == 31] = 0
    hcm = np.ones((2, 32), np.float32)
    if role == 1:
        hcm[0, np.arange(32) % 8 == 0] = 0; hcm[1, np.arange(32) % 8 == 7] = 0
    s_ = np.arange(128)[:, None]; t_ = np.arange(128)[None, :]
    same = (s_ // 32) == (t_ // 32)
    cb = np.concatenate([(same & (s_ <= t_)).astype(np.float32), (same & (s_ >= t_)).astype(np.float32)], axis=1)
    rep = lambda a: np.ascontiguousarray(np.broadcast_to(a.reshape(1, -1), (128, a.size)))
    return dict(amk=amk, amq=amq, ropec=cos, ropes=sin, perm=perm, cmask=rep(cm), smask=rep(smk), rmask=rep(rmk),
                hcm=rep(hcm), cb=np.ascontiguousarray(cb))


def prep_shared(inp, L=DEPTH):
    f = lambda k: np.asarray(inp[k], np.float32)
    sh = {}
    sh["w_ada"] = f("w_ada")[:L]; sh["w_in"] = f("w_in")[:L]; sh["w_br"] = f("w_branch")[:L]; sh["w_out"] = f("w_out")[:L]
    sh["rw"] = f("router_w")[:L]; sh["w2"] = f("w2")[:L]; sh["b2"] = f("b2")[:L]; sh["gw"] = f("rg_gate_w")[:L]
    w1 = f("w1")[:L]
    sh["w1d"] = np.ascontiguousarray(w1.reshape(L, 32, 1024, 1024, 2).transpose(0, 1, 2, 4, 3)).reshape(L, 32, 1024, 2048)
    b1 = f("b1")[:L].reshape(L, 32, 1024, 2)
    sh["b1g"] = _cols(b1[..., 0], L); sh["b1l"] = _cols(b1[..., 1], L)
    sh["b_ada"] = _cols(f("b_ada")[:L], L)
    sh["dal"] = np.ascontiguousarray(np.broadcast_to(f("da_lambda")[:L].reshape(1, -1), (128, L * 256)))
    sh["subln"] = _cols(f("da_subln")[:L], L); sh["hnorm"] = _cols(f("hg_norm")[:L], L)
    sh["convw"] = _cols(f("rg_conv_w")[:L], L); sh["convb"] = _cols(f("rg_conv_b")[:L], L)
    sh["gb"] = _cols(f("rg_gate_b")[:L], L); sh["rlam"] = _cols(f("rg_lambda")[:L], L)
    sh["hlb"] = _cols(f("hg_lb"), 4)
    sh["lnp"] = _cols(np.stack([f("ln1_g")[:L], f("ln1_b")[:L], f("ln2_g")[:L], f("ln2_b")[:L]]), L)
    sh["rb"] = np.ascontiguousarray(np.broadcast_to(f("router_b")[:L].reshape(1, -1), (128, L * 32)))
    sh["ident"] = np.eye(128, dtype=np.float32)
    sel = np.zeros((32, 32, 128), np.float32)
    for e in range(32):
        sel[e, e, :] = 1.0
    sh["sel"] = sel.reshape(32, 32 * 128)
    return sh


def prep_core(inp, c, L=DEPTH):
    f = lambda k: np.asarray(inp[k], np.float32)
    d = {}
    if c < 4:
        d["x"] = f("x_sample")[c]
        d["cond"] = _cols(f("c")[c], 1)
        d["kctx"] = f("cache_attn_k")[c, :L].reshape(L, 256, 512)
        d["vctx"] = f("cache_attn_v")[c, :L].reshape(L, 256, 512)
        d["rg0"] = _cols(f("state_rglru")[c, :L], L)
        d["hg0"] = f("state_hgrn")[c, :L]
        d.update(_structural(0))
    else:
        i = c - 4
        d["x"] = f("x_prompt")[4 * i:4 * i + 4].reshape(NT, 1024)
        d["cond"] = _cols(f("c_ctx"), 1)
        d["kctx"] = np.zeros((L, 256, 512), np.float32); d["vctx"] = np.zeros((L, 256, 512), np.float32)
        d["rg0"] = np.zeros((128, L * 8), np.float32); d["hg0"] = np.zeros((L, 2, 4, 128, 128), np.float32)
        d.update(_structural(1))
    return {k: np.ascontiguousarray(v, dtype=np.float32) for k, v in d.items()}


_NC_CACHE = {}


def kernel(**inputs):
    L = DEPTH
    if L not in _NC_CACHE:
        _NC_CACHE[L] = build(L)
    nc = _NC_CACHE[L]
    sh = prep_shared(inputs, L)
    in_maps = []
    for c in range(8):
        m = dict(sh)
        m.update(prep_core(inputs, c, L))
        in_maps.append(m)
    res = run_bass_kernel_spmd(nc, in_maps, core_ids=list(range(8))).results
    y_sample = np.stack([res[c]["y"] for c in range(4)]).astype(np.float32)
    y_prompt = np.concatenate([res[c]["y"].reshape(4, 256, 1024) for c in range(4, 8)]).astype(np.float32)
    ks, vs, rgs, hgs = [], [], [], []
    for c in range(4, 8):
        r = res[c]
        ks.append(r["ok"].reshape(L, 4, 256, 4, 2, 64).transpose(1, 0, 2, 3, 4, 5))
        vs.append(r["ov"].reshape(L, 4, 256, 4, 128).transpose(1, 0, 2, 3, 4))
        rgs.append(r["org"].reshape(L, 4, 2, 4, 128).transpose(1, 0, 2, 3, 4).reshape(4, L, 2, 512))
        hgs.append(r["ohg"].transpose(1, 0, 2, 3, 4, 5))
    cat = lambda xs: np.ascontiguousarray(np.concatenate(xs, axis=0), dtype=np.float32)
    return (y_prompt, y_sample, cat(ks), cat(vs), cat(rgs), cat(hgs))
```

```python
import math
from contextlib import ExitStack
from concourse.bass_utils import run_bass_kernel_spmd
import numpy as np
import concourse.bass as bass
import concourse.mybir as mybir

F32 = mybir.dt.float32
BF16 = mybir.dt.bfloat16
I32 = mybir.dt.int32
AF = mybir.ActivationFunctionType
ALU = mybir.AluOpType
AX = mybir.AxisListType


class Buf:
    def __init__(self, t, name=""):
        self.t = t
        self.name = name
        self.last_w = None
        self.readers = []

    def __getitem__(self, idx):
        return View(self, self.t[idx])

    def ap(self, a):
        return View(self, a)


class View:
    def __init__(self, buf, ap):
        self.buf = buf
        self.ap = ap


def _ap(v):
    return v.ap if isinstance(v, View) else v


class Sched:
    def __init__(self, nc, n_dma_sems=48):
        self.nc = nc
        self.engs = {}
        for name, e in (("pe", nc.tensor), ("act", nc.scalar), ("dve", nc.vector), ("pool", nc.gpsimd), ("sp", nc.sync)):
            sem = nc.alloc_semaphore(name=f"sem_{name}") if name != "sp" else None
            self.engs[name] = dict(e=e, sem=sem, cnt=0, known={})
        self.dma_rings = {q: [dict(sem=nc.alloc_semaphore(name=f"dsem_{q}{i}"), val=0) for i in range(n_dma_sems // 2)]
                          for q in ("sp", "pool")}
        self.dma_rr = {"sp": 0, "pool": 0}
        self.nops = 0

    def _wait(self, eng, deps):
        E = self.engs[eng]
        best = {}
        for d in deps:
            if d is None:
                continue
            sem, val = d
            k = id(sem)
            if k not in best or best[k][1] < val:
                best[k] = (sem, val)
        for k, (sem, val) in best.items():
            if E["known"].get(k, 0) >= val:
                continue
            if sem is E["sem"] and (eng == "pe" or val > E["cnt"]):
                continue
            E["e"].wait_ge(sem, val)
            E["known"][k] = val

    def _deps(self, reads, writes):
        deps = []
        for v in reads:
            if isinstance(v, View):
                deps.append(v.buf.last_w)
        for v in writes:
            if isinstance(v, View):
                deps.append(v.buf.last_w)
                deps.extend(v.buf.readers)
        return deps

    def _commit(self, reads, writes, tok):
        for v in writes:
            if isinstance(v, View):
                v.buf.last_w = tok
                v.buf.readers = []
        for v in reads:
            if isinstance(v, View):
                rs = [r for r in v.buf.readers if r[0] is not tok[0]]
                rs.append(tok)
                v.buf.readers = rs

    def op(self, eng, fn, reads, writes, signal=True):
        E = self.engs[eng]
        self._wait(eng, self._deps(reads, writes))
        ins = fn()
        self.nops += 1
        if signal:
            ins.then_inc(E["sem"], 1)
            E["cnt"] += 1
            tok = (E["sem"], E["cnt"])
        else:
            tok = (E["sem"], E["cnt"] + 1)
        self._commit(reads, writes, tok)
        return ins

    def dma(self, q, out, in_, **kw):
        E = self.engs[q]
        ring = self.dma_rings[q]
        slot = ring[self.dma_rr[q]]
        self.dma_rr[q] = (self.dma_rr[q] + 1) % len(ring)
        deps = self._deps([in_], [out])
        if slot["val"] > 0:
            deps.append((slot["sem"], slot["val"]))
        self._wait(q, deps)
        ins = E["e"].dma_start(out=_ap(out), in_=_ap(in_), **kw)
        slot["val"] += 16
        ins.then_inc(slot["sem"], 16)
        tok = (slot["sem"], slot["val"])
        self._commit([in_], [out], tok)
        self.nops += 1
        return tok

    def barrier_tokens(self):
        toks = []
        for name, E in self.engs.items():
            if E["sem"] is not None and E["cnt"] > 0:
                toks.append((E["sem"], E["cnt"]))
        for ring in self.dma_rings.values():
            for s in ring:
                if s["val"] > 0:
                    toks.append((s["sem"], s["val"]))
        return toks

    def wait_all(self, eng):
        self._wait(eng, self.barrier_tokens())

    def mm(self, out, lhsT, rhs, start=True, stop=True, signal=None, **kw):
        if signal is None:
            signal = stop
        return self.op("pe", lambda: self.nc.tensor.matmul(_ap(out), lhsT=_ap(lhsT), rhs=_ap(rhs), start=start, stop=stop, **kw),
                       [lhsT, rhs] + ([] if start else [out]), [out], signal=signal)

    def transpose(self, out, in_, ident, **kw):
        return self.op("pe", lambda: self.nc.tensor.transpose(_ap(out), _ap(in_), _ap(ident), **kw), [in_, ident], [out])

    def act(self, out, in_, func, bias=None, scale=None, accum_out=None, eng="act"):
        kw = {}
        reads = [in_]
        writes = [out]
        if bias is not None:
            kw["bias"] = _ap(bias)
            reads.append(bias)
        if scale is not None:
            kw["scale"] = _ap(scale)
            reads.append(scale)
        if accum_out is not None:
            kw["accum_out"] = _ap(accum_out)
            writes.append(accum_out)
        return self.op("act", lambda: self.nc.scalar.activation(out=_ap(out), in_=_ap(in_), func=func, **kw), reads, writes)

    def _veng(self, eng):
        return {"dve": self.nc.vector, "pool": self.nc.gpsimd}[eng]

    def tt(self, out, in0, in1, op, eng="dve"):
        return self.op(eng, lambda: self._veng(eng).tensor_tensor(out=_ap(out), in0=_ap(in0), in1=_ap(in1), op=op), [in0, in1], [out])

    def ts(self, out, in0, s1, op0, s2=None, op1=None, eng="dve", accum_out=None):
        reads = [in0, s1, s2]
        writes = [out] + ([accum_out] if accum_out is not None else [])
        kw = {}
        if op1 is not None:
            kw["op1"] = op1
        if accum_out is not None:
            kw["accum_out"] = _ap(accum_out)
        return self.op(eng, lambda: self._veng(eng).tensor_scalar(out=_ap(out), in0=_ap(in0), scalar1=_ap(s1), scalar2=_ap(s2), op0=op0, **kw), reads, writes)

    def stt(self, out, in0, scalar, in1, op0, op1, eng="dve"):
        return self.op(eng, lambda: self._veng(eng).scalar_tensor_tensor(out=_ap(out), in0=_ap(in0), scalar=_ap(scalar), in1=_ap(in1), op0=op0, op1=op1), [in0, scalar, in1], [out])

    def copy(self, out, in_, eng="dve"):
        if eng == "act":
            return self.op("act", lambda: self.nc.scalar.copy(out=_ap(out), in_=_ap(in_)), [in_], [out])
        return self.op(eng, lambda: self._veng(eng).tensor_copy(out=_ap(out), in_=_ap(in_)), [in_], [out])

    def memset(self, out, val, eng="dve"):
        return self.op(eng, lambda: self._veng(eng).memset(_ap(out), val), [], [out])

    def scan(self, out, d0, d1, initial, op0, op1, eng="dve"):
        return self.op(eng, lambda: self._veng(eng).tensor_tensor_scan(out=_ap(out), data0=_ap(d0), data1=_ap(d1), initial=_ap(initial), op0=op0, op1=op1), [d0, d1, initial], [out])

    def reduce(self, out, in_, op, axis=AX.X, eng="dve"):
        return self.op(eng, lambda: self._veng(eng).tensor_reduce(out=_ap(out), in_=_ap(in_), axis=axis, op=op), [in_], [out])


DEPTH = 4
ALPHA = (2 * DEPTH) ** 0.25
EPS = 1e-5
EPS_LN = EPS / (ALPHA * ALPHA)
NT = 1024
W_IN = 8192
C_Q, C_K, C_V, C_RX, C_RG, C_HQ, C_HZF, C_HZB, C_HI, C_HG, C_MG = 0, 512, 1024, 1536, 2048, 2560, 3072, 3584, 4096, 4608, 5120


def build(L=DEPTH, n_exp=32, stages=("mix", "moe")):
    nc = bass.Bass("TRN2", target_bir_lowering=False)
    S = Sched(nc)

    def din(name, shape, dt=F32):
        return nc.dram_tensor(name, list(shape), dt, kind="ExternalInput").ap()

    def dout(name, shape, dt=F32):
        return nc.dram_tensor(name, list(shape), dt, kind="ExternalOutput").ap()

    x_d = din("x", [NT, 1024]); cond_d = din("cond", [128, 8])
    kctx_d = din("kctx", [L, 256, 512]); vctx_d = din("vctx", [L, 256, 512])
    rg0_d = din("rg0", [128, L * 8]); hg0_d = din("hg0", [L, 2, 4, 128, 128])
    amk_d = din("amk", [8, 1280]); amq_d = din("amq", [8, 1024])
    ropec_d = din("ropec", [128, NT]); ropes_d = din("ropes", [128, NT]); perm_d = din("perm", [128, 128])
    cmask_d = din("cmask", [128, 3 * NT]); smask_d = din("smask", [128, 2 * NT]); rmask_d = din("rmask", [128, 2 * NT])
    hcm_d = din("hcm", [128, 64]); cb_d = din("cb", [128, 256]); ident_d = din("ident", [128, 128]); sel_d = din("sel", [32, 32 * 128])
    w_ada_d = din("w_ada", [L, 1024, 6144]); b_ada_d = din("b_ada", [128, L * 48]); w_in_d = din("w_in", [L, 1024, W_IN])
    dal_d = din("dal", [128, L * 256]); subln_d = din("subln", [128, L])
    convw_d = din("convw", [128, L * 16]); convb_d = din("convb", [128, L * 4]); gw_d = din("gw", [L, 2, 2, 8, 64, 64])
    gb_d = din("gb", [128, L * 16]); rlam_d = din("rlam", [128, L * 8]); hlb_d = din("hlb", [128, 32]); hnorm_d = din("hnorm", [128, L])
    w_br_d = din("w_br", [L, 3, 512, 1024]); w_out_d = din("w_out", [L, 1024, 1024])
    lnp_d = din("lnp", [128, 4 * L * 8])
    rw_d = din("rw", [L, 1024, 32]); rb_d = din("rb", [128, L * 32])
    NE = 32 if "moe" in stages else 1
    if "mix" in stages:
        stages = tuple(stages) + ("att", "rg", "hg")
    w1_d = din("w1d", [L, NE, 1024, 2048]); b1g_d = din("b1g", [128, L * 256]); b1l_d = din("b1l", [128, L * 256])
    w2_d = din("w2", [L, NE, 1024, 1024]); b2_d = din("b2", [L, NE, 1024])

    y_o = dout("y", [NT, 1024]); k_o = dout("ok", [L, NT, 512]); v_o = dout("ov", [L, NT, 512])
    rg_o = dout("org", [L, 32, 128]); hg_o = dout("ohg", [L, 4, 2, 4, 128, 128])

    es_all = ExitStack()

    uid = [0]

    def sb(es, name, shape, dt=F32):
        uid[0] += 1
        name = f"{name}_{uid[0]}"
        return Buf(es.enter_context(nc.sbuf_tensor(name, list(shape), dt)), name)

    def psb(es, name, shape, dt=F32):
        return Buf(es.enter_context(nc.psum_tensor(name, list(shape), dt)), name)

    def barrier():
        for e in ("pe", "act", "dve", "pool", "sp"):
            S.wait_all(e)

    P = es_all
    ps = [psb(P, f"ps{i}", [128, 512]) for i in range(7)]
    psb16 = psb(P, "psb16", [128, 1024], BF16)
    X = [sb(P, f"x{c}", [128, NT]) for c in range(8)]
    U = [sb(P, f"u{c}", [128, NT], BF16) for c in range(8)]
    ident = sb(P, "ident", [128, 128]); identb = sb(P, "identb", [128, 128], BF16)
    ones = sb(P, "ones", [128, 128]); onesb = sb(P, "onesb", [128, 128], BF16)
    mod = sb(P, "mod", [128, L * 48])
    lnp = sb(P, "lnp", [128, 4 * L * 8])
    small = {}
    for nm, dd, w in (("subln", subln_d, L), ("convw", convw_d, L * 16), ("convb", convb_d, L * 4), ("gb", gb_d, L * 16),
                      ("rlam", rlam_d, L * 8), ("hlb", hlb_d, 32), ("hnorm", hnorm_d, L), ("rg0", rg0_d, L * 8),
                      ("hcm", hcm_d, 64), ("rb", rb_d, L * 32), ("b1g", b1g_d, L * 256), ("b1l", b1l_d, L * 256),
                      ("cond", cond_d, 8)):
        small[nm] = sb(P, nm, [128, w])
        S.dma("sp", small[nm][:], dd[:, :])
    S.dma("sp", ident[:], ident_d[:, :]); S.dma("sp", lnp[:], lnp_d[:, :])
    S.ts(small["b1l"][:], small["b1l"][:], 1.0, ALU.add)
    S.copy(identb[:], ident[:])
    S.memset(ones[:], 1.0); S.memset(onesb[:], 1.0)
    lam_neg = sb(P, "lam_neg", [128, L])
    nsp = sb(P, "nsp", [128, L * 8])
    lbv = sb(P, "lbv", [128, 32]); oml = sb(P, "oml", [128, 32])
    subw = sb(P, "subw", [128, L])
    hcmb = small["hcm"]

    with ExitStack() as E0:
        xt = sb(E0, "xt", [128, 8, 1024])
        S.dma("sp", xt[:], x_d.rearrange("(tb p) f -> p tb f", p=128))
        wst = [sb(E0, f"wst{i}", [128, 6144]) for i in range(2)]
        scond = sb(E0, "scond", [128, 8])
        S.act(scond[:], small["cond"][:], AF.Silu)
        badat = sb(E0, "badat", [128, L * 48]); S.dma("sp", badat[:], b_ada_d[:, :])
        for l in range(L):
            for kc in range(8):
                wt = wst[(l * 8 + kc) % 2]
                S.dma("sp", wt[:], w_ada_d[l, kc * 128:(kc + 1) * 128, :])
                for j in range(48):
                    S.mm(ps[0][:, j:j + 1], wt[:, j * 128:(j + 1) * 128], scond[:, kc:kc + 1], start=True, stop=True, signal=(j == 47))
                S.tt(mod[:, l * 48:(l + 1) * 48], ps[0][:, 0:48], (badat if kc == 0 else mod)[:, l * 48:(l + 1) * 48], ALU.add)
        for l in range(L):
            b = l * 48
            S.ts(mod[:, b + 8:b + 16], mod[:, b + 8:b + 16], 1.0, ALU.add)
            S.ts(mod[:, b + 32:b + 40], mod[:, b + 32:b + 40], 1.0, ALU.add)
            S.ts(mod[:, b + 16:b + 24], mod[:, b + 16:b + 24], 1.0 / ALPHA, ALU.mult)
            S.ts(mod[:, b + 40:b + 48], mod[:, b + 40:b + 48], 1.0 / ALPHA, ALU.mult)
        for c in range(8):
            for hf in range(2):
                for i in range(4):
                    tb = hf * 4 + i
                    S.transpose(ps[1 + hf][:, i * 128:(i + 1) * 128], xt[:, tb, c * 128:(c + 1) * 128], ident[:])
                S.copy(X[c][:, hf * 512:(hf + 1) * 512], ps[1 + hf][:], eng=("dve" if hf == 0 else "act"))
        dal = sb(E0, "dal", [128, L * 256]); S.dma("sp", dal[:], dal_d[:, :])
        t2 = sb(E0, "t2s", [128, L * 2 * 64]); t3 = sb(E0, "t3s", [128, L * 2])
        dv = dal.t[:, :].rearrange("p (l a d) -> p l a d", l=L, a=4)
        for l in range(L):
            for a in range(2):
                S.tt(t2[:, (l * 2 + a) * 64:(l * 2 + a + 1) * 64], dal[:, l * 256 + (2 * a) * 64:l * 256 + (2 * a + 1) * 64],
                     dal[:, l * 256 + (2 * a + 1) * 64:l * 256 + (2 * a + 2) * 64], ALU.mult)
                S.reduce(t3[:, l * 2 + a:l * 2 + a + 1], t2[:, (l * 2 + a) * 64:(l * 2 + a + 1) * 64], ALU.add)
        S.act(t3[:], t3[:], AF.Exp)
        for l in range(L):
            li = 0.8 - 0.6 * math.exp(-0.3 * l)
            S.stt(lam_neg[:, l:l + 1], t3[:, 2 * l + 1:2 * l + 2], -li, t3[:, 2 * l:2 * l + 1], ALU.add, ALU.subtract)
            S.ts(subw[:, l:l + 1], small["subln"][:, l:l + 1], 1.0 - li, ALU.mult)
        S.act(nsp[:], small["rlam"][:], AF.Exp, scale=-1.0)
        S.act(nsp[:], nsp[:], AF.Ln, bias=1.0)
        S.ts(nsp[:], nsp[:], -8.0, ALU.mult)
        eh = sb(E0, "eh", [128, 32]); sh_ = sb(E0, "sh_", [128, 8])
        S.act(eh[:], small["hlb"][:], AF.Exp)
        S.tt(sh_[:], eh[:, 0:8], eh[:, 8:16], ALU.add)
        S.tt(sh_[:], sh_[:], eh[:, 16:24], ALU.add)
        S.tt(sh_[:], sh_[:], eh[:, 24:32], ALU.add)
        S.op("dve", lambda: nc.vector.reciprocal(out=sh_.t[:], in_=sh_.t[:]), [sh_[:]], [sh_[:]])
        for l in range(4):
            S.tt(eh[:, l * 8:(l + 1) * 8], eh[:, l * 8:(l + 1) * 8], sh_[:], ALU.mult)
        S.memset(lbv[:, 0:8], 0.0)
        S.copy(lbv[:, 8:16], eh[:, 8:16])
        S.tt(lbv[:, 16:24], lbv[:, 8:16], eh[:, 16:24], ALU.add)
        S.tt(lbv[:, 24:32], lbv[:, 16:24], eh[:, 24:32], ALU.add)
        S.ts(oml[:], lbv[:], -1.0, ALU.mult, 1.0, ALU.add)
        barrier()

    def layer_norm(l, which, E):
        gcol = (2 * which) * L * 8 + l * 8
        bcol = (2 * which + 1) * L * 8 + l * 8
        sq = [sb(E, f"lnsq{which}_{i}", [128, 512]) for i in range(2)]
        mean = sb(E, f"lnmean{which}", [128, 512]); rstd = sb(E, f"lnrstd{which}", [128, 512]); tmp = sb(E, f"lntmp{which}", [128, 512])
        for th in range(2):
            sl = slice(th * 512, (th + 1) * 512)
            for c in range(8):
                S.mm(ps[0][:], ones[:], X[c][:, sl], start=(c == 0), stop=(c == 7))
            for c in range(8):
                S.act(sq[c % 2][:], X[c][:, sl], AF.Square)
                S.mm(ps[1][:], ones[:], sq[c % 2][:], start=(c == 0), stop=(c == 7), signal=True)
            S.ts(mean[:], ps[0][:], 1.0 / 1024, ALU.mult)
            S.tt(tmp[:], mean[:], mean[:], ALU.mult)
            S.stt(rstd[:], ps[1][:], 1.0 / 1024, tmp[:], ALU.mult, ALU.subtract)
            S.act(rstd[:], rstd[:], AF.Sqrt, bias=EPS_LN)
            S.op("dve", lambda: nc.vector.reciprocal(out=rstd.t[:], in_=rstd.t[:]), [rstd[:]], [rstd[:]])
            for c in range(8):
                S.tt(X[c][:, sl], X[c][:, sl], mean[:], ALU.subtract)
                S.tt(X[c][:, sl], X[c][:, sl], rstd[:], ALU.mult)
                S.ts(X[c][:, sl], X[c][:, sl], lnp[:, gcol + c:gcol + c + 1], ALU.mult, lnp[:, bcol + c:bcol + c + 1], ALU.add)

    def modulate(l, second):
        b = l * 48 + (24 if second else 0)
        for c in range(8):
            S.ts(U[c][:], X[c][:], mod[:, b + 8 + c:b + 9 + c], ALU.mult, mod[:, b + c:b + c + 1], ALU.add)

    wq_rr = [0]

    def load_w_cols(wbufs, src_rows_ap, c0, ncols):
        wb = wbufs[wq_rr[0] % len(wbufs)]
        wq_rr[0] += 1
        S.dma("pool", wb[:, :, 0:ncols], src_rows_ap.rearrange("(kc p) c -> p kc c", p=128)[:, :, c0:c0 + ncols])
        return wb

    def proj_fm(wb, oc, th, out_ps, nk=8, rhs=None):
        rhs = rhs or U
        for kc in range(nk):
            S.mm(out_ps, wb[:, kc, oc * 128:(oc + 1) * 128], rhs[kc][:, th * 512:(th + 1) * 512], start=(kc == 0), stop=(kc == nk - 1))

    def proj_tm(wb, tb, out_ps, ncols=512):
        for kc in range(8):
            S.mm(out_ps, U[kc][:, tb * 128:(tb + 1) * 128], wb[:, kc, 0:ncols], start=(kc == 0), stop=(kc == 7))

    for l in range(L):
        li = 0.8 - 0.6 * math.exp(-0.3 * l)
        modulate(l, False)
        with ExitStack() as EM:
            BR = [[sb(EM, f"br{n}_{c}", [128, NT], BF16) for c in range(4)] for n in range(3)]
            wbufs = [sb(EM, f"wcol{i}", [128, 8, 512], BF16) for i in range(2)]
            if True:
                with ExitStack() as EA:
                  if "att" in stages:
                    Q = [[sb(EA, f"q{h}_{m}", [128, NT], BF16) for m in range(2)] for h in range(4)]
                    K = [[sb(EA, f"k{h}_{m}", [128, 1280], BF16) for m in range(2)] for h in range(4)]
                    for h in range(4 if "noM" not in stages else 0):
                        for m in range(2):
                            oth = slice(64, 128) if m == 0 else slice(0, 64)
                            mrow = slice(64, 72) if m == 0 else slice(0, 8)
                            S.memset(Q[h][m][oth, :], 0.0); S.memset(K[h][m][oth, :], 0.0)
                            S.dma("pool", Q[h][m][mrow, :], amq_d[:, :]); S.dma("pool", K[h][m][mrow, :], amk_d[:, :])
                    V = [sb(EA, f"v{t}", [128, 512], BF16) for t in range(10)]
                    rc = sb(EA, "ropec", [128, NT]); rs = sb(EA, "ropes", [128, NT]); perm = sb(EA, "perm", [128, 128], BF16)
                    S.dma("sp", rc[:], ropec_d[:, :]); S.dma("sp", rs[:], ropes_d[:, :]); S.dma("pool", perm[:], perm_d[:, :])
                    qb = [sb(EA, f"qb{i}", [128, 512], BF16) for i in range(2)]
                    t1 = [sb(EA, f"at1{i}", [128, 512]) for i in range(2)]
                    t2 = [sb(EA, f"at2{i}", [128, 512]) for i in range(2)]
                    stg = [sb(EA, f"stg{i}", [128, 512]) for i in range(2)]
                    for which, c0, DST in (((0, C_Q, Q), (1, C_K, K)) if "noA1" not in stages else ()):
                        wb = load_w_cols(wbufs, w_in_d[l], c0, 512)
                        for h in range(4):
                            for th in range(2):
                                i = th
                                sl = slice(th * 512, (th + 1) * 512)
                                proj_fm(wb, h, th, ps[th][:])
                                if "noR" in stages:
                                    S.copy(t1[i][:], ps[th][:], eng="act")
                                    S.memset(t2[i][:], 0.0)
                                else:
                                    S.copy(qb[i][:], ps[th][:], eng="act")
                                    S.op("dve", lambda: nc.vector.tensor_tensor(out=t1[i].t[:], in0=ps[th].t[:], in1=rc.t[:, sl], op=ALU.mult),
                                         [ps[th][:], rc[:, sl], qb[i][:]], [t1[i][:]])
                                    if "R1" in stages:
                                        S.memset(t2[i][:], 0.0)
                                    else:
                                        S.mm(ps[2 + th][:], perm[:], qb[i][:])
                                        S.tt(t2[i][:], ps[2 + th][:], rs[:, sl], ALU.mult)
                                S.tt(DST[h][0][0:64, sl], t1[i][0:64, :], t2[i][0:64, :], ALU.add)
                                S.tt(DST[h][1][64:128, sl], t1[i][64:128, :], t2[i][64:128, :], ALU.add)
                    wbk = load_w_cols(wbufs, w_in_d[l], C_K, 512)
                    NA2 = 8 if "noA2" not in stages else 0
                    for tb in range(NA2):
                        proj_tm(wbk, tb, ps[tb % 2][:])
                        S.copy(stg[tb % 2][:], ps[tb % 2][:], eng="act")
                        S.dma("sp", k_o[l, tb * 128:(tb + 1) * 128, :], stg[tb % 2][:])
                    wbv = load_w_cols(wbufs, w_in_d[l], C_V, 512)
                    for tb in range(NA2):
                        proj_tm(wbv, tb, ps[tb % 2][:])
                        S.copy(stg[tb % 2][:], ps[tb % 2][:], eng="act")
                        S.copy(V[tb][:], stg[tb % 2][:])
                        S.dma("sp", v_o[l, tb * 128:(tb + 1) * 128, :], stg[tb % 2][:])
                    NA3 = 2 if "noA3" not in stages else 0
                    for blk in range(NA3):
                        S.dma("sp", stg[blk][:], kctx_d[l, blk * 128:(blk + 1) * 128, :])
                        for h in range(4):
                            S.transpose(ps[2][:, h * 128:(h + 1) * 128], stg[blk][:, h * 128:(h + 1) * 128], ident[:])
                        for h in range(4):
                            S.copy(K[h][0][0:64, 1024 + blk * 128:1024 + (blk + 1) * 128], ps[2][0:64, h * 128:(h + 1) * 128])
                            S.copy(K[h][1][64:128, 1024 + blk * 128:1024 + (blk + 1) * 128], ps[2][64:128, h * 128:(h + 1) * 128])
                    for blk in range(NA3):
                        S.dma("pool", V[8 + blk][:], vctx_d[l, blk * 128:(blk + 1) * 128, :])
                    pT = [sb(EA, f"pT{i}", [128, 512], BF16) for i in range(4)]
                    o_a = sb(EA, "o_a", [128, 512]); o_b = sb(EA, "o_b", [128, 512]); rcp = sb(EA, "rcp", [128, 512]); sqa = sb(EA, "sqa", [128, 512])
                    pi = 0
                    for h in range(4 if "noattcore" not in stages else 0):
                        for qh in range(2):
                            qs = slice(qh * 512, (qh + 1) * 512)
                            for m in range(2):
                                ms = slice(m * 64, (m + 1) * 64)
                                for kb in range(10):
                                    sc = ps[4 + (pi % 3)]
                                    S.mm(sc[:], K[h][m][:, kb * 128:(kb + 1) * 128], Q[h][m][:, qs])
                                    p_ = pT[pi % 4]; pi += 1
                                    S.act(p_[:], sc[:], AF.Exp, scale=0.125)
                                    S.mm(ps[2 * m][:], V[kb][:, h * 128:(h + 1) * 128], p_[:], start=(kb == 0), stop=(kb == 9), signal=True)
                                    S.mm(ps[2 * m + 1][:], onesb[:], p_[:], start=(kb == 0), stop=(kb == 9), signal=True)
                            S.op("dve", lambda: nc.vector.reciprocal(out=rcp.t[:], in_=ps[1].t[:]), [ps[1][:]], [rcp[:]])
                            S.tt(o_a[:], ps[0][:], rcp[:], ALU.mult)
                            S.op("dve", lambda: nc.vector.reciprocal(out=rcp.t[:], in_=ps[3].t[:]), [ps[3][:]], [rcp[:]])
                            S.tt(o_b[:], ps[2][:], rcp[:], ALU.mult)
                            S.stt(o_a[:], o_b[:], lam_neg[:, l:l + 1], o_a[:], ALU.mult, ALU.add)
                            S.act(sqa[:], o_a[:], AF.Square)
                            S.mm(ps[1][:], ones[:], sqa[:])
                            S.act(rcp[:], ps[1][:], AF.Sqrt, scale=1.0 / 128, bias=EPS)
                            S.op("dve", lambda: nc.vector.reciprocal(out=rcp.t[:], in_=rcp.t[:]), [rcp[:]], [rcp[:]])
                            S.tt(o_a[:], o_a[:], rcp[:], ALU.mult)
                            S.ts(BR[0][h][:, qs], o_a[:], subw[:, l:l + 1], ALU.mult)
                    barrier()
                with ExitStack() as EB:
                  if "rg" in stages:
                    cm = sb(EB, "cmaskt", [128, 3 * NT], BF16); S.dma("pool", cm[:], cmask_d[:, :])
                    sm = sb(EB, "smaskt", [128, 2 * NT], BF16); S.dma("pool", sm[:], smask_d[:, :])
                    gwt = sb(EB, "gwt", [128, 16 * 128], BF16)
                    S.memset(gwt[:], 0.0)
                    for d in range(2):
                        for g in range(2):
                            for n in range(8):
                                c, half = n // 2, n % 2
                                col = ((d * 2 + g) * 4 + c) * 128 + half * 64
                                S.dma("pool", gwt[half * 64:(half + 1) * 64, col:col + 64], gw_d[l, d, g, n, :, :])
                    rx = sb(EB, "rx", [128, NT]); xr = sb(EB, "xr", [128, NT]); xrb = sb(EB, "xrb", [128, NT], BF16)
                    tmp = sb(EB, "rtmp", [128, NT]); rr = sb(EB, "rr", [128, NT]); ii = sb(EB, "ii", [128, NT])
                    aa = sb(EB, "aa", [128, NT]); bb = sb(EB, "bb", [128, NT]); hh = [sb(EB, f"hh{d}", [128, NT]) for d in range(2)]
                    gg = sb(EB, "gg", [128, NT]); ge = sb(EB, "ge", [128, NT])
                    rgfin = sb(EB, "rgfin", [128, 32])
                    wbx = load_w_cols(wbufs, w_in_d[l], C_RX, 512)
                    wbg = load_w_cols(wbufs, w_in_d[l], C_RG, 512)
                    rgv = rgfin.t[:, :].rearrange("p (s d c) -> p s d c", s=4, d=2)
                    for c in range(4):
                        for th in range(2):
                            proj_fm(wbx, c, th, ps[th][:])
                            S.copy(rx[:, th * 512:(th + 1) * 512], ps[th][:], eng="act")
                        cw = lambda tap: small["convw"][:, l * 16 + tap * 4 + c:l * 16 + tap * 4 + c + 1]
                        S.ts(xr[:], rx[:], cw(2), ALU.mult, small["convb"][:, l * 4 + c:l * 4 + c + 1], ALU.add)
                        S.tt(tmp[:, 2:NT], rx[:, 0:NT - 2], cm[:, 2:NT], ALU.mult)
                        S.stt(xr[:, 2:NT], tmp[:, 2:NT], cw(0), xr[:, 2:NT], ALU.mult, ALU.add)
                        S.tt(tmp[:, 1:NT], rx[:, 0:NT - 1], cm[:, NT + 1:2 * NT], ALU.mult)
                        S.stt(xr[:, 1:NT], tmp[:, 1:NT], cw(1), xr[:, 1:NT], ALU.mult, ALU.add)
                        S.tt(tmp[:, 0:NT - 1], rx[:, 1:NT], cm[:, 2 * NT:3 * NT - 1], ALU.mult)
                        S.stt(xr[:, 0:NT - 1], tmp[:, 0:NT - 1], cw(3), xr[:, 0:NT - 1], ALU.mult, ALU.add)
                        S.copy(xrb[:], xr[:], eng="act")
                        for d in range(2):
                            for g, dst in ((0, rr), (1, ii)):
                                col = ((d * 2 + g) * 4 + c) * 128
                                bcol = l * 16 + (d * 2 + g) * 4 + c
                                for th in range(2):
                                    S.mm(ps[2 + th][:], gwt[:, col:col + 128], xrb[:, th * 512:(th + 1) * 512])
                                    S.act(dst[:, th * 512:(th + 1) * 512], ps[2 + th][:], AF.Sigmoid, bias=small["gb"][:, bcol:bcol + 1])
                            ncol = l * 8 + d * 4 + c
                            S.act(aa[:], rr[:], AF.Exp, scale=nsp[:, ncol:ncol + 1])
                            S.tt(bb[:], aa[:], aa[:], ALU.mult)
                            S.act(bb[:], bb[:], AF.Sqrt, scale=-1.0, bias=1.0)
                            S.tt(bb[:], bb[:], ii[:], ALU.mult)
                            S.tt(bb[:], bb[:], xr[:], ALU.mult)
                            S.tt(aa[:], aa[:], sm[:, d * NT:(d + 1) * NT], ALU.mult)
                            h0 = small["rg0"][:, ncol:ncol + 1]
                            if d == 0:
                                S.scan(hh[0][:], aa[:], bb[:], h0, ALU.mult, ALU.add)
                                S.copy(View(rgfin, rgv[:, :, 0, c]), hh[0][:, 255:NT:256])
                            else:
                                S.scan(hh[1][:, ::-1], aa[:, ::-1], bb[:, ::-1], h0, ALU.mult, ALU.add)
                                S.copy(View(rgfin, rgv[:, :, 1, c]), hh[1][:, 0:NT:256])
                        S.tt(hh[0][:], hh[0][:], hh[1][:], ALU.add)
                        for th in range(2):
                            proj_fm(wbg, c, th, ps[th][:])
                            S.copy(gg[:, th * 512:(th + 1) * 512], ps[th][:], eng="act")
                        S.tt(ge[:], gg[:], gg[:], ALU.mult)
                        S.ts(ge[:], ge[:], 0.044715, ALU.mult, 1.0, ALU.add)
                        S.tt(ge[:], ge[:], gg[:], ALU.mult)
                        S.act(ge[:], ge[:], AF.Sigmoid, scale=2.0 * math.sqrt(2.0 / math.pi))
                        S.tt(ge[:], ge[:], gg[:], ALU.mult)
                        S.tt(BR[1][c][:], hh[0][:], ge[:], ALU.mult)
                    S.transpose(ps[4][0:32, 0:128], rgfin[:], ident[:])
                    rgo = sb(EB, "rgo", [32, 128]); S.copy(rgo[:], ps[4][0:32, 0:128])
                    S.dma("sp", rg_o[l, :, :], rgo[:])
                    barrier()
                with ExitStack() as EC:
                  if "hg" in stages:
                    rm = sb(EC, "rmaskt", [128, 2 * NT], BF16); S.dma("pool", rm[:], rmask_d[:, :])
                    cbm = sb(EC, "cbm", [128, 256], BF16); S.dma("pool", cbm[:], cb_d[:, :])
                    HI = [sb(EC, f"hi{t}", [128, 512], BF16) for t in range(8)]
                    HIc = [sb(EC, f"hic{n}", [32, 512], BF16) for n in range(32)]
                    wbi = wbufs[0]
                    S.dma("pool", wbi[:, :, 0:512], w_in_d[l].rearrange("(kc p) c -> p kc c", p=128)[:, :, C_HI:C_HI + 512])
                    for tb in range(8):
                        proj_tm(wbi, tb, ps[tb % 2][:])
                        S.copy(HI[tb][:], ps[tb % 2][:], eng=("act" if tb % 2 else "dve"))
                    for n in range(32):
                        for kc in range(8):
                            S.mm(ps[2 + n % 2][0:32, :], U[kc][:, n * 32:(n + 1) * 32], wbi[:, kc, 0:512], start=(kc == 0), stop=(kc == 7))
                        S.copy(HIc[n][:], ps[2 + n % 2][0:32, :], eng=("act" if n % 2 else "dve"))
                    wh = [Buf(wbufs[1].t[:, :, i * 128:(i + 1) * 128], f"wh{i}") for i in range(4)]
                    qh = sb(EC, "qh", [128, NT]); osum = sb(EC, "osum", [128, NT]); sg = sb(EC, "sg", [128, NT])
                    gl = sb(EC, "gl", [128, NT]); kk = sb(EC, "kk", [128, NT]); bc = sb(EC, "bc", [128, NT]); ex = sb(EC, "ex", [128, NT])
                    kt = sb(EC, "kt", [128, NT], BF16); kh = sb(EC, "kh", [128, NT], BF16)
                    qt = [sb(EC, f"qt{d}", [128, NT], BF16) for d in range(2)]
                    Dv = [sb(EC, f"Dv{d}", [128, 32]) for d in range(2)]; Dcm = [sb(EC, f"Dcm{d}", [128, 32]) for d in range(2)]
                    KHc = [[sb(EC, f"khc{d}_{n}", [32, 128], BF16) for n in range(32)] for d in range(2)]
                    AT = [[sb(EC, f"AT{d}_{t}", [128, 128], BF16) for t in range(8)] for d in range(2)]
                    S32 = [sb(EC, f"S32{d}", [128, 128]) for d in range(2)]; Sb = [sb(EC, f"Sb{d}", [128, 128], BF16) for d in range(2)]
                    for h in range(4):
                        for i, c0 in enumerate((C_HQ, C_HZF, C_HZB, C_HG)):
                            S.dma("pool", wh[i][:], w_in_d[l].rearrange("(kc p) c -> p kc c", p=128)[:, :, c0 + h * 128:c0 + (h + 1) * 128])
                        for th in range(2):
                            proj_fm(wh[0], 0, th, ps[6][:])
                            S.act(qh[:, th * 512:(th + 1) * 512], ps[6][:], AF.Silu)
                        S.memset(osum[:], 0.0)
                        for d in range(2):
                            lc = l * 8 + d * 4 + h
                            for th in range(2):
                                proj_fm(wh[1 + d], 0, th, ps[6][:])
                                S.act(sg[:, th * 512:(th + 1) * 512], ps[6][:], AF.Sigmoid)
                            S.ts(gl[:], sg[:], oml[:, lc:lc + 1], ALU.mult, lbv[:, lc:lc + 1], ALU.add)
                            S.act(gl[:], gl[:], AF.Ln)
                            S.ts(kk[:], sg[:], oml[:, lc:lc + 1], ALU.mult)
                            S.ts(kk[:], kk[:], -1.0, ALU.mult)
                            S.ts(kk[:], kk[:], oml[:, lc:lc + 1], ALU.add)
                            if d == 0:
                                S.scan(bc[:], rm[:, 0:NT], gl[:], 0.0, ALU.mult, ALU.add)
                                S.act(Dv[d][:], bc[:, 31:NT:32], AF.Exp)
                            else:
                                S.scan(bc[:, ::-1], rm[:, 2 * NT - 1:NT - 1:-1], gl[:, ::-1], 0.0, ALU.mult, ALU.add)
                                S.act(Dv[d][:], bc[:, 0:NT:32], AF.Exp)
                            S.act(ex[:], bc[:], AF.Exp)
                            S.tt(qt[d][:], qh[:], ex[:], ALU.mult)
                            S.act(ex[:], bc[:], AF.Exp, scale=-1.0)
                            S.tt(ex[:], kk[:], ex[:], ALU.mult)
                            S.copy(kt[:], ex[:], eng="act")
                            for n in range(32):
                                S.ts(kh[:, n * 32:(n + 1) * 32], ex[:, n * 32:(n + 1) * 32], Dv[d][:, n:n + 1], ALU.mult)
                            S.tt(Dcm[d][:], Dv[d][:], hcmb[:, d * 32:(d + 1) * 32], ALU.mult)
                            for n in range(32):
                                S.transpose(psb16[0:32, (n % 8) * 128:(n % 8 + 1) * 128], kh[:, n * 32:(n + 1) * 32], identb[:])
                                S.copy(KHc[d][n][:], psb16[0:32, (n % 8) * 128:(n % 8 + 1) * 128], eng="act")
                            for tb in range(8):
                                pa = ps[6]
                                S.mm(pa[:, (tb % 4) * 128:(tb % 4 + 1) * 128], kt[:, tb * 128:(tb + 1) * 128], qt[d][:, tb * 128:(tb + 1) * 128])
                                S.tt(AT[d][tb][:], pa[:, (tb % 4) * 128:(tb % 4 + 1) * 128], cbm[:, d * 128:(d + 1) * 128], ALU.mult)
                            S.dma("sp", S32[d][:], hg0_d[l, d, h, :, :])
                            first = 0 if d == 0 else 31
                            S.ts(Sb[d][:], S32[d][:], hcmb[:, d * 32 + first:d * 32 + first + 1], ALU.mult)
                        def chunk_of(d, step):
                            return step if d == 0 else 31 - step

                        def emit_kv(d, step):
                            n = chunk_of(d, step)
                            S.mm(ps[2 + 2 * d + step % 2][:, 0:128], KHc[d][n][:], HIc[n][:, h * 128:(h + 1) * 128])

                        for d in range(2):
                            emit_kv(d, 0)
                        for step in range(32):
                            for d in range(2):
                                po = ps[d]
                                n = chunk_of(d, step)
                                tb, j = n // 4, n % 4
                                bs = slice((tb % 4) * 128, (tb % 4 + 1) * 128)
                                blk_first = (j == 0) if d == 0 else (j == 3)
                                blk_last = (j == 3) if d == 0 else (j == 0)
                                if step + 1 < 32:
                                    emit_kv(d, step + 1)
                                if blk_first:
                                    S.mm(po[:, bs], HI[tb][:, h * 128:(h + 1) * 128], AT[d][tb][:], start=True, stop=False, signal=True)
                                S.mm(po[:, (tb % 4) * 128 + j * 32:(tb % 4) * 128 + (j + 1) * 32], Sb[d][:], qt[d][:, n * 32:(n + 1) * 32],
                                     start=False, stop=blk_last, signal=True)
                                pk = ps[2 + 2 * d + step % 2]
                                S.stt(S32[d][:], S32[d][:], Dcm[d][:, n:n + 1], pk[:, 0:128], ALU.mult, ALU.add)
                                seq_end = (n % 8 == 7) if d == 0 else (n % 8 == 0)
                                if seq_end:
                                    S.dma("sp", hg_o[l, n // 8, d, h, :, :], S32[d][:])
                                nn = n + 1 if d == 0 else n - 1
                                if 0 <= nn < 32:
                                    S.ts(Sb[d][:], S32[d][:], hcmb[:, d * 32 + nn:d * 32 + nn + 1], ALU.mult)
                                if blk_last:
                                    ts_ = slice(tb * 128, (tb + 1) * 128)
                                    S.tt(osum[:, ts_], osum[:, ts_], po[:, bs], ALU.add)
                        for th in range(2):
                            sl = slice(th * 512, (th + 1) * 512)
                            proj_fm(wh[3], 0, th, ps[6][:])
                            S.act(sg[:, sl], ps[6][:], AF.Silu)
                            S.act(gl[:, 0:512], osum[:, sl], AF.Square)
                            S.mm(ps[6][:], ones[:], gl[:, 0:512])
                            S.act(gl[:, 512:1024], ps[6][:], AF.Sqrt, scale=1.0 / 128, bias=EPS)
                            S.op("dve", lambda: nc.vector.reciprocal(out=gl.t[:, 512:1024], in_=gl.t[:, 512:1024]), [gl[:, 512:1024]], [gl[:, 512:1024]])
                            S.tt(gl[:, 512:1024], gl[:, 512:1024], osum[:, sl], ALU.mult)
                            S.ts(gl[:, 512:1024], gl[:, 512:1024], small["hnorm"][:, l:l + 1], ALU.mult)
                            S.tt(BR[2][h][:, sl], gl[:, 512:1024], sg[:, sl], ALU.mult)
                    barrier()
            with ExitStack() as ED:
                wbr = sb(ED, "wbr", [128, 12, 1024], BF16)
                S.dma("pool", wbr[:], w_br_d[l].rearrange("n (kc p) c -> p (n kc) c", p=128))
                macc = [sb(ED, f"macc{c}", [128, NT]) for c in range(8)]
                gsb = sb(ED, "gsb", [128, 512]); gp = sb(ED, "gp", [128, 512])
                for n in range(3):
                    for og in range(2):
                        wb = load_w_cols(wbufs, w_in_d[l], C_MG + n * 1024 + og * 512, 512)
                        for oi in range(4):
                            oc = og * 4 + oi
                            for th in range(2):
                                sl = slice(th * 512, (th + 1) * 512)
                                proj_fm(wb, oi, th, ps[th][:])
                                S.act(gsb[:], ps[th][:], AF.Sigmoid)
                                for kc in range(4):
                                    S.mm(ps[2 + th][:], wbr[:, n * 4 + kc, oc * 128:(oc + 1) * 128], BR[n][kc][:, sl], start=(kc == 0), stop=(kc == 3))
                                if n == 0:
                                    S.tt(macc[oc][:, sl], gsb[:], ps[2 + th][:], ALU.mult)
                                else:
                                    S.tt(gp[:], gsb[:], ps[2 + th][:], ALU.mult)
                                    S.tt(macc[oc][:, sl], macc[oc][:, sl], gp[:], ALU.add)
                mb = [sb(ED, f"mb{c}", [128, NT], BF16) for c in range(8)]
                for c in range(8):
                    S.copy(mb[c][:], macc[c][:], eng=("act" if c % 2 else "dve"))
                for og in range(2):
                    wb = load_w_cols(wbufs, w_out_d[l], og * 512, 512)
                    for oi in range(4):
                        oc = og * 4 + oi
                        for th in range(2):
                            sl = slice(th * 512, (th + 1) * 512)
                            proj_fm(wb, oi, th, ps[th][:], rhs=mb)
                            S.stt(X[oc][:, sl], ps[th][:], mod[:, l * 48 + 16 + oc:l * 48 + 17 + oc], X[oc][:, sl], ALU.mult, ALU.add)
                layer_norm(l, 0, ED)
                barrier()
        barrier()
        modulate(l, True)
        with ExitStack() as EE:
            if "moe" in stages:
                lg = sb(EE, "lg", [128, 256]); gate = sb(EE, "gate", [128, 256]); m8 = sb(EE, "m8", [128, 8]); nm = sb(EE, "nm", [128, 1])
                msk = sb(EE, "msk", [128, 32]); den = sb(EE, "den", [128, 1])
                gT = sb(EE, "gT", [32, NT])
                rbt = sb(EE, "rbt", [128, 256])
                for tb in range(8):
                    S.copy(rbt[:, tb * 32:(tb + 1) * 32], small["rb"][:, l * 32:(l + 1) * 32])
                ER = ExitStack()
                u2f = [sb(ER, f"u2f{i}", [128, NT]) for i in range(2)]
                rwt = sb(ER, "rwt", [128, 8, 32]); S.dma("sp", rwt[:], rw_d[l].rearrange("(kc p) e -> p kc e", p=128))
                b = l * 48 + 24
                for c in range(8):
                    uf = u2f[c % 2]
                    S.ts(uf[:], X[c][:], mod[:, b + 8 + c:b + 9 + c], ALU.mult, mod[:, b + c:b + c + 1], ALU.add)
                    for tb in range(8):
                        S.mm(ps[0][:, tb * 32:(tb + 1) * 32], uf[:, tb * 128:(tb + 1) * 128], rwt[:, c, :], start=True, stop=True, signal=(tb == 7))
                    S.tt(lg[:], ps[0][:, 0:256], (rbt if c == 0 else lg)[:], ALU.add)
                for tb in range(8):
                    lt = lg[:, tb * 32:(tb + 1) * 32]
                    S.op("dve", lambda: nc.vector.max(out=m8.t[:], in_=lg.t[:, tb * 32:(tb + 1) * 32]), [lt], [m8[:]])
                    S.ts(nm[:], m8[:, 0:1], -1.0, ALU.mult)
                    S.ts(msk[:], lt, m8[:, 3:4], ALU.is_ge)
                    gt_ = gate[:, tb * 32:(tb + 1) * 32]
                    S.act(gt_, lt, AF.Exp, bias=nm[:])
                    S.tt(gt_, gt_, msk[:], ALU.mult)
                    S.reduce(den[:], gt_, ALU.add)
                    S.op("dve", lambda: nc.vector.reciprocal(out=den.t[:], in_=den.t[:]), [den[:]], [den[:]])
                    S.ts(gt_, gt_, den[:], ALU.mult)
                    if tb < 4:
                        S.transpose(ps[1][0:32, tb * 128:(tb + 1) * 128], gt_, ident[:])
                S.copy(gT[:, 0:512], ps[1][0:32, 0:512]);
                for tb in range(4, 8):
                    pass
                for tb in range(4, 8):
                    S.transpose(ps[2][0:32, (tb - 4) * 128:(tb - 3) * 128], gate[:, tb * 32:(tb + 1) * 32], ident[:])
                S.copy(gT[:, 512:1024], ps[2][0:32, 0:512])
                barrier()
                ER.close()
                b2t = sb(EE, "b2t", [32, 1024]); S.dma("sp", b2t[:], b2_d[l, :, :])
                selt = sb(EE, "selt", [32, 32 * 128], BF16); S.dma("pool", selt[:], sel_d[:, :])
                gTb = sb(EE, "gTb", [32, NT], BF16); S.copy(gTb[:], gT[:])
                g2c = l * 48 + 40
                for dc in range(8):
                    for th in range(2):
                        sl = slice(th * 512, (th + 1) * 512)
                        S.mm(ps[3 + th][:], b2t[:, dc * 128:(dc + 1) * 128], gT[:, sl])
                        S.stt(X[dc][:, sl], ps[3 + th][:], mod[:, g2c + dc:g2c + dc + 1], X[dc][:, sl], ALU.mult, ALU.add)
                ring = [sb(EE, f"wring{i}", [128, 8, 1024], BF16) for i in range(4)]
                ACTB = [[sb(EE, f"actb{i}_{j}", [128, NT], BF16) for j in range(8)] for i in range(2)]
                gbc = [sb(EE, "gbc0", [128, NT])] * 2
                NR = 3
                Gb = [sb(EE, f"Gb{i}", [128, 512], BF16) for i in range(NR)]; Lb = [sb(EE, f"Lb{i}", [128, 512], BF16) for i in range(NR)]
                sgm = [sb(EE, f"sgm{i}", [128, 512], BF16) for i in range(NR)]
                t1m = [sb(EE, f"t1m{i}", [128, 512], BF16) for i in range(NR)]; t2m = [sb(EE, f"t2m{i}", [128, 512], BF16) for i in range(NR)]
                rr_ = [0]

                def load_piece(src):
                    wb = ring[rr_[0] % 4]; rr_[0] += 1
                    S.dma("pool", wb[:], src)
                    return wb

                def pieces(e):
                    v1 = w1_d[l, e].rearrange("(kc p) c -> p kc c", p=128)
                    return (load_piece(v1[:, :, 0:1024]), load_piece(v1[:, :, 1024:2048]),
                            load_piece(w2_d[l, e].rearrange("(kc p) c -> p kc c", p=128)))

                nxt = pieces(0)
                it = 0
                for e in range(n_exp):
                    wg, wl, w2b = nxt
                    ab = ACTB[e % 2]
                    for th in range(2):
                        S.mm(ps[5][:], selt[:, e * 128:(e + 1) * 128], gTb[:, th * 512:(th + 1) * 512])
                        S.copy(gbc[e % 2][:, th * 512:(th + 1) * 512], ps[5][:], eng="act")
                    bcol = (l * 32 + e) * 8
                    for j in range(8):
                        for th in range(2):
                            sl = slice(th * 512, (th + 1) * 512)
                            i = it % 2; r = it % NR; it += 1
                            proj_fm(wg, j, th, ps[2 * i][:])
                            proj_fm(wl, j, th, ps[2 * i + 1][:])
                            S.act(Gb[r][:], ps[2 * i][:], AF.Identity, bias=small["b1g"][:, bcol + j:bcol + j + 1])
                            S.act(Lb[r][:], ps[2 * i + 1][:], AF.Identity, bias=small["b1l"][:, bcol + j:bcol + j + 1])
                            S.ts(Gb[r][:], Gb[r][:], 7.0, ALU.min)
                            S.act(sgm[r][:], Gb[r][:], AF.Sigmoid, scale=1.702)
                            S.ts(Lb[r][:], Lb[r][:], 8.0, ALU.min, -6.0, ALU.max)
                            S.tt(t2m[r][:], Lb[r][:], gbc[e % 2][:, sl], ALU.mult, eng="pool")
                            S.tt(t1m[r][:], Gb[r][:], sgm[r][:], ALU.mult, eng="pool")
                            S.tt(ab[j][:, sl], t1m[r][:], t2m[r][:], ALU.mult)
                    if e + 1 < n_exp:
                        nxt = pieces(e + 1)
                    for dc in range(8):
                        for th in range(2):
                            sl = slice(th * 512, (th + 1) * 512)
                            i = it % 2; it += 1
                            proj_fm(w2b, dc, th, ps[2 * i][:], rhs=ab)
                            S.stt(X[dc][:, sl], ps[2 * i][:], mod[:, g2c + dc:g2c + dc + 1], X[dc][:, sl], ALU.mult, ALU.add)
            layer_norm(l, 1, EE)
            barrier()

    with ExitStack() as EF:
        yo = [sb(EF, f"yo{i}", [128, 1024]) for i in range(2)]
        for tb in range(8):
            for hf in range(2):
                for i in range(4):
                    c = hf * 4 + i
                    S.transpose(ps[hf][:, i * 128:(i + 1) * 128], X[c][:, tb * 128:(tb + 1) * 128], ident[:])
                S.copy(yo[tb % 2][:, hf * 512:(hf + 1) * 512], ps[hf][:], eng=("act" if hf else "dve"))
            S.dma("sp", y_o[tb * 128:(tb + 1) * 128, :], yo[tb % 2][:])
        barrier()
    es_all.close()
    return nc


def _cols(a, L):
    a = np.asarray(a, np.float32)
    lead = a.shape[:-1]
    n = a.shape[-1] // 128
    a = a.reshape(*lead, n, 128)
    a = np.moveaxis(a, -1, 0)
    return np.ascontiguousarray(a.reshape(128, -1))


def _structural(role):
    t = np.arange(NT)
    BIG = 32768.0
    amk = np.zeros((8, 1280), np.float32); amq = np.zeros((8, 1024), np.float32)
    if role == 1:
        amk[0, :] = -BIG; amq[0, :] = 1.0
        for g in range(4):
            amk[1 + g, g * 256:(g + 1) * 256] = BIG
            amq[1 + g, g * 256:(g + 1) * 256] = 1.0
    cos = np.ones((128, NT), np.float32); sin = np.zeros((128, NT), np.float32)
    perm = np.zeros((128, 128), np.float32)
    inv = 10000.0 ** (-np.arange(0, 32, 2, dtype=np.float32) / 32)
    row = (t // 64).astype(np.float32); col = (t % 64).astype(np.float32)
    for p in range(128):
        d = p % 64
        pos = row if d < 32 else col
        dd = d % 32
        j = dd % 16
        first = dd < 16
        partner = p + 16 if first else p - 16
        perm[partner, p] = 1.0
        if role == 0:
            ang = pos * inv[j]
            cos[p] = np.cos(ang)
            sin[p] = -np.sin(ang) if first else np.sin(ang)
    seg = (t % 256) if role == 1 else t
    seglen = 256 if role == 1 else NT
    cm = np.ones((3, NT), np.float32)
    cm[0, seg < 2] = 0; cm[1, seg < 1] = 0; cm[2, seg == seglen - 1] = 0
    smk = np.ones((2, NT), np.float32)
    if role == 1:
        smk[0, seg == 0] = 0; smk[1, seg == seglen - 1] = 0
    rmk = np.ones((2, NT), np.float32)
    rmk[0, t % 32 == 0] = 0; rmk[1, t % 32 == 31] = 0
    hcm = np.ones((2, 32), np.float32)
    if role == 1:
        hcm[0, np.arange(32) % 8 == 0] = 0; hcm[1, np.arange(32) % 8 == 7] = 0
    s_ = np.arange(128)[:, None]; t_ = np.arange(128)[None, :]
    same = (s_ // 32) == (t_ // 32)
    cb = np.concatenate([(same & (s_ <= t_)).astype(np.float32), (same & (s_ >= t_)).astype(np.float32)], axis=1)
    rep = lambda a: np.ascontiguousarray(np.broadcast_to(a.reshape(1, -1), (128, a.size)))
    return dict(amk=amk, amq=amq, ropec=cos, ropes=sin, perm=perm, cmask=rep(cm), smask=rep(smk), rmask=rep(rmk),
                hcm=rep(hcm), cb=np.ascontiguousarray(cb))


def prep_shared(inp, L=DEPTH):
    f = lambda k: np.asarray(inp[k], np.float32)
    sh = {}
    sh["w_ada"] = f("w_ada")[:L]; sh["w_in"] = f("w_in")[:L]; sh["w_br"] = f("w_branch")[:L]; sh["w_out"] = f("w_out")[:L]
    sh["rw"] = f("router_w")[:L]; sh["w2"] = f("w2")[:L]; sh["b2"] = f("b2")[:L]; sh["gw"] = f("rg_gate_w")[:L]
    w1 = f("w1")[:L]
    sh["w1d"] = np.ascontiguousarray(w1.reshape(L, 32, 1024, 1024, 2).transpose(0, 1, 2, 4, 3)).reshape(L, 32, 1024, 2048)
    b1 = f("b1")[:L].reshape(L, 32, 1024, 2)
    sh["b1g"] = _cols(b1[..., 0], L); sh["b1l"] = _cols(b1[..., 1], L)
    sh["b_ada"] = _cols(f("b_ada")[:L], L)
    sh["dal"] = np.ascontiguousarray(np.broadcast_to(f("da_lambda")[:L].reshape(1, -1), (128, L * 256)))
    sh["subln"] = _cols(f("da_subln")[:L], L); sh["hnorm"] = _cols(f("hg_norm")[:L], L)
    sh["convw"] = _cols(f("rg_conv_w")[:L], L); sh["convb"] = _cols(f("rg_conv_b")[:L], L)
    sh["gb"] = _cols(f("rg_gate_b")[:L], L); sh["rlam"] = _cols(f("rg_lambda")[:L], L)
    sh["hlb"] = _cols(f("hg_lb"), 4)
    sh["lnp"] = _cols(np.stack([f("ln1_g")[:L], f("ln1_b")[:L], f("ln2_g")[:L], f("ln2_b")[:L]]), L)
    sh["rb"] = np.ascontiguousarray(np.broadcast_to(f("router_b")[:L].reshape(1, -1), (128, L * 32)))
    sh["ident"] = np.eye(128, dtype=np.float32)
    sel = np.zeros((32, 32, 128), np.float32)
    for e in range(32):
        sel[e, e, :] = 1.0
    sh["sel"] = sel.reshape(32, 32 * 128)
    return sh


def prep_core(inp, c, L=DEPTH):
    f = lambda k: np.asarray(inp[k], np.float32)
    d = {}
    if c < 4:
        d["x"] = f("x_sample")[c]
        d["cond"] = _cols(f("c")[c], 1)
        d["kctx"] = f("cache_attn_k")[c, :L].reshape(L, 256, 512)
        d["vctx"] = f("cache_attn_v")[c, :L].reshape(L, 256, 512)
        d["rg0"] = _cols(f("state_rglru")[c, :L], L)
        d["hg0"] = f("state_hgrn")[c, :L]
        d.update(_structural(0))
    else:
        i = c - 4
        d["x"] = f("x_prompt")[4 * i:4 * i + 4].reshape(NT, 1024)
        d["cond"] = _cols(f("c_ctx"), 1)
        d["kctx"] = np.zeros((L, 256, 512), np.float32); d["vctx"] = np.zeros((L, 256, 512), np.float32)
        d["rg0"] = np.zeros((128, L * 8), np.float32); d["hg0"] = np.zeros((L, 2, 4, 128, 128), np.float32)
        d.update(_structural(1))
    return {k: np.ascontiguousarray(v, dtype=np.float32) for k, v in d.items()}


_NC_CACHE = {}


def kernel(**inputs):
    L = DEPTH
    if L not in _NC_CACHE:
        _NC_CACHE[L] = build(L)
    nc = _NC_CACHE[L]
    sh = prep_shared(inputs, L)
    in_maps = []
    for c in range(8):
        m = dict(sh)
        m.update(prep_core(inputs, c, L))
        in_maps.append(m)
    res = run_bass_kernel_spmd(nc, in_maps, core_ids=list(range(8))).results
    y_sample = np.stack([res[c]["y"] for c in range(4)]).astype(np.float32)
    y_prompt = np.concatenate([res[c]["y"].reshape(4, 256, 1024) for c in range(4, 8)]).astype(np.float32)
    ks, vs, rgs, hgs = [], [], [], []
    for c in range(4, 8):
        r = res[c]
        ks.append(r["ok"].reshape(L, 4, 256, 4, 2, 64).transpose(1, 0, 2, 3, 4, 5))
        vs.append(r["ov"].reshape(L, 4, 256, 4, 128).transpose(1, 0, 2, 3, 4))
        rgs.append(r["org"].reshape(L, 4, 2, 4, 128).transpose(1, 0, 2, 3, 4).reshape(4, L, 2, 512))
        hgs.append(r["ohg"].transpose(1, 0, 2, 3, 4, 5))
    cat = lambda xs: np.ascontiguousarray(np.concatenate(xs, axis=0), dtype=np.float32)
    return (y_prompt, y_sample, cat(ks), cat(vs), cat(rgs), cat(hgs))
```

```python
import math
from contextlib import ExitStack
from concourse.bass_utils import run_bass_kernel_spmd
import numpy as np
import concourse.bass as bass
import concourse.mybir as mybir

F32 = mybir.dt.float32
BF16 = mybir.dt.bfloat16
I32 = mybir.dt.int32
AF = mybir.ActivationFunctionType
ALU = mybir.AluOpType
AX = mybir.AxisListType


class Buf:
    def __init__(self, t, name=""):
        self.t = t
        self.name = name
        self.last_w = None
        self.readers = []

    def __getitem__(self, idx):
        return View(self, self.t[idx])

    def ap(self, a):
        return View(self, a)


class View:
    def __init__(self, buf, ap):
        self.buf = buf
        self.ap = ap


def _ap(v):
    return v.ap if isinstance(v, View) else v


class Sched:
    def __init__(self, nc, n_dma_sems=48):
        self.nc = nc
        self.engs = {}
        for name, e in (("pe", nc.tensor), ("act", nc.scalar), ("dve", nc.vector), ("pool", nc.gpsimd), ("sp", nc.sync)):
            sem = nc.alloc_semaphore(name=f"sem_{name}") if name != "sp" else None
            self.engs[name] = dict(e=e, sem=sem, cnt=0, known={})
        self.dma_rings = {q: [dict(sem=nc.alloc_semaphore(name=f"dsem_{q}{i}"), val=0) for i in range(n_dma_sems // 2)]
                          for q in ("sp", "pool")}
        self.dma_rr = {"sp": 0, "pool": 0}
        self.nops = 0

    def _wait(self, eng, deps):
        E = self.engs[eng]
        best = {}
        for d in deps:
            if d is None:
                continue
            sem, val = d
            k = id(sem)
            if k not in best or best[k][1] < val:
                best[k] = (sem, val)
        for k, (sem, val) in best.items():
            if E["known"].get(k, 0) >= val:
                continue
            if sem is E["sem"] and (eng == "pe" or val > E["cnt"]):
                continue
            E["e"].wait_ge(sem, val)
            E["known"][k] = val

    def _deps(self, reads, writes):
        deps = []
        for v in reads:
            if isinstance(v, View):
                deps.append(v.buf.last_w)
        for v in writes:
            if isinstance(v, View):
                deps.append(v.buf.last_w)
                deps.extend(v.buf.readers)
        return deps

    def _commit(self, reads, writes, tok):
        for v in writes:
            if isinstance(v, View):
                v.buf.last_w = tok
                v.buf.readers = []
        for v in reads:
            if isinstance(v, View):
                rs = [r for r in v.buf.readers if r[0] is not tok[0]]
                rs.append(tok)
                v.buf.readers = rs

    def op(self, eng, fn, reads, writes, signal=True):
        E = self.engs[eng]
        self._wait(eng, self._deps(reads, writes))
        ins = fn()
        self.nops += 1
        if signal:
            ins.then_inc(E["sem"], 1)
            E["cnt"] += 1
            tok = (E["sem"], E["cnt"])
        else:
            tok = (E["sem"], E["cnt"] + 1)
        self._commit(reads, writes, tok)
        return ins

    def dma(self, q, out, in_, **kw):
        E = self.engs[q]
        ring = self.dma_rings[q]
        slot = ring[self.dma_rr[q]]
        self.dma_rr[q] = (self.dma_rr[q] + 1) % len(ring)
        deps = self._deps([in_], [out])
        if slot["val"] > 0:
            deps.append((slot["sem"], slot["val"]))
        self._wait(q, deps)
        ins = E["e"].dma_start(out=_ap(out), in_=_ap(in_), **kw)
        slot["val"] += 16
        ins.then_inc(slot["sem"], 16)
        tok = (slot["sem"], slot["val"])
        self._commit([in_], [out], tok)
        self.nops += 1
        return tok

    def barrier_tokens(self):
        toks = []
        for name, E in self.engs.items():
            if E["sem"] is not None and E["cnt"] > 0:
                toks.append((E["sem"], E["cnt"]))
        for ring in self.dma_rings.values():
            for s in ring:
                if s["val"] > 0:
                    toks.append((s["sem"], s["val"]))
        return toks

    def wait_all(self, eng):
        self._wait(eng, self.barrier_tokens())

    def mm(self, out, lhsT, rhs, start=True, stop=True, signal=None, **kw):
        if signal is None:
            signal = stop
        return self.op("pe", lambda: self.nc.tensor.matmul(_ap(out), lhsT=_ap(lhsT), rhs=_ap(rhs), start=start, stop=stop, **kw),
                       [lhsT, rhs] + ([] if start else [out]), [out], signal=signal)

    def transpose(self, out, in_, ident, **kw):
        return self.op("pe", lambda: self.nc.tensor.transpose(_ap(out), _ap(in_), _ap(ident), **kw), [in_, ident], [out])

    def act(self, out, in_, func, bias=None, scale=None, accum_out=None, eng="act"):
        kw = {}
        reads = [in_]
        writes = [out]
        if bias is not None:
            kw["bias"] = _ap(bias)
            reads.append(bias)
        if scale is not None:
            kw["scale"] = _ap(scale)
            reads.append(scale)
        if accum_out is not None:
            kw["accum_out"] = _ap(accum_out)
            writes.append(accum_out)
        return self.op("act", lambda: self.nc.scalar.activation(out=_ap(out), in_=_ap(in_), func=func, **kw), reads, writes)

    def _veng(self, eng):
        return {"dve": self.nc.vector, "pool": self.nc.gpsimd}[eng]

    def tt(self, out, in0, in1, op, eng="dve"):
        return self.op(eng, lambda: self._veng(eng).tensor_tensor(out=_ap(out), in0=_ap(in0), in1=_ap(in1), op=op), [in0, in1], [out])

    def ts(self, out, in0, s1, op0, s2=None, op1=None, eng="dve", accum_out=None):
        reads = [in0, s1, s2]
        writes = [out] + ([accum_out] if accum_out is not None else [])
        kw = {}
        if op1 is not None:
            kw["op1"] = op1
        if accum_out is not None:
            kw["accum_out"] = _ap(accum_out)
        return self.op(eng, lambda: self._veng(eng).tensor_scalar(out=_ap(out), in0=_ap(in0), scalar1=_ap(s1), scalar2=_ap(s2), op0=op0, **kw), reads, writes)

    def stt(self, out, in0, scalar, in1, op0, op1, eng="dve"):
        return self.op(eng, lambda: self._veng(eng).scalar_tensor_tensor(out=_ap(out), in0=_ap(in0), scalar=_ap(scalar), in1=_ap(in1), op0=op0, op1=op1), [in0, scalar, in1], [out])

    def copy(self, out, in_, eng="dve"):
        if eng == "act":
            return self.op("act", lambda: self.nc.scalar.copy(out=_ap(out), in_=_ap(in_)), [in_], [out])
        return self.op(eng, lambda: self._veng(eng).tensor_copy(out=_ap(out), in_=_ap(in_)), [in_], [out])

    def memset(self, out, val, eng="dve"):
        return self.op(eng, lambda: self._veng(eng).memset(_ap(out), val), [], [out])

    def scan(self, out, d0, d1, initial, op0, op1, eng="dve"):
        return self.op(eng, lambda: self._veng(eng).tensor_tensor_scan(out=_ap(out), data0=_ap(d0), data1=_ap(d1), initial=_ap(initial), op0=op0, op1=op1), [d0, d1, initial], [out])

    def reduce(self, out, in_, op, axis=AX.X, eng="dve"):
        return self.op(eng, lambda: self._veng(eng).tensor_reduce(out=_ap(out), in_=_ap(in_), axis=axis, op=op), [in_], [out])


DEPTH = 4
ALPHA = (2 * DEPTH) ** 0.25
EPS = 1e-5
EPS_LN = EPS / (ALPHA * ALPHA)
NT = 1024
W_IN = 8192
C_Q, C_K, C_V, C_RX, C_RG, C_HQ, C_HZF, C_HZB, C_HI, C_HG, C_MG = 0, 512, 1024, 1536, 2048, 2560, 3072, 3584, 4096, 4608, 5120


def build(L=DEPTH, n_exp=32, stages=("mix", "moe")):
    nc = bass.Bass("TRN2", target_bir_lowering=False)
    S = Sched(nc)

    def din(name, shape, dt=F32):
        return nc.dram_tensor(name, list(shape), dt, kind="ExternalInput").ap()

    def dout(name, shape, dt=F32):
        return nc.dram_tensor(name, list(shape), dt, kind="ExternalOutput").ap()

    x_d = din("x", [NT, 1024]); cond_d = din("cond", [128, 8])
    kctx_d = din("kctx", [L, 256, 512]); vctx_d = din("vctx", [L, 256, 512])
    rg0_d = din("rg0", [128, L * 8]); hg0_d = din("hg0", [L, 2, 4, 128, 128])
    amk_d = din("amk", [8, 1280]); amq_d = din("amq", [8, 1024])
    ropec_d = din("ropec", [128, NT]); ropes_d = din("ropes", [128, NT]); perm_d = din("perm", [128, 128])
    cmask_d = din("cmask", [128, 3 * NT]); smask_d = din("smask", [128, 2 * NT]); rmask_d = din("rmask", [128, 2 * NT])
    hcm_d = din("hcm", [128, 64]); cb_d = din("cb", [128, 256]); ident_d = din("ident", [128, 128]); sel_d = din("sel", [32, 32 * 128])
    w_ada_d = din("w_ada", [L, 1024, 6144]); b_ada_d = din("b_ada", [128, L * 48]); w_in_d = din("w_in", [L, 1024, W_IN])
    dal_d = din("dal", [128, L * 256]); subln_d = din("subln", [128, L])
    convw_d = din("convw", [128, L * 16]); convb_d = din("convb", [128, L * 4]); gw_d = din("gw", [L, 2, 2, 8, 64, 64])
    gb_d = din("gb", [128, L * 16]); rlam_d = din("rlam", [128, L * 8]); hlb_d = din("hlb", [128, 32]); hnorm_d = din("hnorm", [128, L])
    w_br_d = din("w_br", [L, 3, 512, 1024]); w_out_d = din("w_out", [L, 1024, 1024])
    lnp_d = din("lnp", [128, 4 * L * 8])
    rw_d = din("rw", [L, 1024, 32]); rb_d = din("rb", [128, L * 32])
    NE = 32 if "moe" in stages else 1
    if "mix" in stages:
        stages = tuple(stages) + ("att", "rg", "hg")
    w1_d = din("w1d", [L, NE, 1024, 2048]); b1g_d = din("b1g", [128, L * 256]); b1l_d = din("b1l", [128, L * 256])
    w2_d = din("w2", [L, NE, 1024, 1024]); b2_d = din("b2", [L, NE, 1024])

    y_o = dout("y", [NT, 1024]); k_o = dout("ok", [L, NT, 512]); v_o = dout("ov", [L, NT, 512])
    rg_o = dout("org", [L, 32, 128]); hg_o = dout("ohg", [L, 4, 2, 4, 128, 128])

    es_all = ExitStack()

    uid = [0]

    def sb(es, name, shape, dt=F32):
        uid[0] += 1
        name = f"{name}_{uid[0]}"
        return Buf(es.enter_context(nc.sbuf_tensor(name, list(shape), dt)), name)

    def psb(es, name, shape, dt=F32):
        return Buf(es.enter_context(nc.psum_tensor(name, list(shape), dt)), name)

    def barrier():
        for e in ("pe", "act", "dve", "pool", "sp"):
            S.wait_all(e)

    P = es_all
    ps = [psb(P, f"ps{i}", [128, 512]) for i in range(7)]
    psb16 = psb(P, "psb16", [128, 1024], BF16)
    X = [sb(P, f"x{c}", [128, NT]) for c in range(8)]
    U = [sb(P, f"u{c}", [128, NT], BF16) for c in range(8)]
    ident = sb(P, "ident", [128, 128]); identb = sb(P, "identb", [128, 128], BF16)
    ones = sb(P, "ones", [128, 128]); onesb = sb(P, "onesb", [128, 128], BF16)
    mod = sb(P, "mod", [128, L * 48])
    lnp = sb(P, "lnp", [128, 4 * L * 8])
    small = {}
    for nm, dd, w in (("subln", subln_d, L), ("convw", convw_d, L * 16), ("convb", convb_d, L * 4), ("gb", gb_d, L * 16),
                      ("rlam", rlam_d, L * 8), ("hlb", hlb_d, 32), ("hnorm", hnorm_d, L), ("rg0", rg0_d, L * 8),
                      ("hcm", hcm_d, 64), ("rb", rb_d, L * 32), ("b1g", b1g_d, L * 256), ("b1l", b1l_d, L * 256),
                      ("cond", cond_d, 8)):
        small[nm] = sb(P, nm, [128, w])
        S.dma("sp", small[nm][:], dd[:, :])
    S.dma("sp", ident[:], ident_d[:, :]); S.dma("sp", lnp[:], lnp_d[:, :])
    S.ts(small["b1l"][:], small["b1l"][:], 1.0, ALU.add)
    S.copy(identb[:], ident[:])
    S.memset(ones[:], 1.0); S.memset(onesb[:], 1.0)
    lam_neg = sb(P, "lam_neg", [128, L])
    nsp = sb(P, "nsp", [128, L * 8])
    lbv = sb(P, "lbv", [128, 32]); oml = sb(P, "oml", [128, 32]); noml = sb(P, "noml", [128, 32])
    subw = sb(P, "subw", [128, L])
    hcmb = small["hcm"]

    with ExitStack() as E0:
        xt = sb(E0, "xt", [128, 8, 1024])
        S.dma("sp", xt[:], x_d.rearrange("(tb p) f -> p tb f", p=128))
        wst = [sb(E0, f"wst{i}", [128, 6144]) for i in range(2)]
        scond = sb(E0, "scond", [128, 8])
        S.act(scond[:], small["cond"][:], AF.Silu)
        badat = sb(E0, "badat", [128, L * 48]); S.dma("sp", badat[:], b_ada_d[:, :])
        for l in range(L):
            for kc in range(8):
                wt = wst[(l * 8 + kc) % 2]
                S.dma("sp", wt[:], w_ada_d[l, kc * 128:(kc + 1) * 128, :])
                for j in range(48):
                    S.mm(ps[0][:, j:j + 1], wt[:, j * 128:(j + 1) * 128], scond[:, kc:kc + 1], start=True, stop=True, signal=(j == 47))
                S.tt(mod[:, l * 48:(l + 1) * 48], ps[0][:, 0:48], (badat if kc == 0 else mod)[:, l * 48:(l + 1) * 48], ALU.add)
        for l in range(L):
            b = l * 48
            S.ts(mod[:, b + 8:b + 16], mod[:, b + 8:b + 16], 1.0, ALU.add)
            S.ts(mod[:, b + 32:b + 40], mod[:, b + 32:b + 40], 1.0, ALU.add)
            S.ts(mod[:, b + 16:b + 24], mod[:, b + 16:b + 24], 1.0 / ALPHA, ALU.mult)
            S.ts(mod[:, b + 40:b + 48], mod[:, b + 40:b + 48], 1.0 / ALPHA, ALU.mult)
        for c in range(8):
            for hf in range(2):
                for i in range(4):
                    tb = hf * 4 + i
                    S.transpose(ps[1 + hf][:, i * 128:(i + 1) * 128], xt[:, tb, c * 128:(c + 1) * 128], ident[:])
                S.copy(X[c][:, hf * 512:(hf + 1) * 512], ps[1 + hf][:], eng=("dve" if hf == 0 else "act"))
        dal = sb(E0, "dal", [128, L * 256]); S.dma("sp", dal[:], dal_d[:, :])
        t2 = sb(E0, "t2s", [128, L * 2 * 64]); t3 = sb(E0, "t3s", [128, L * 2])
        dv = dal.t[:, :].rearrange("p (l a d) -> p l a d", l=L, a=4)
        for l in range(L):
            for a in range(2):
                S.tt(t2[:, (l * 2 + a) * 64:(l * 2 + a + 1) * 64], dal[:, l * 256 + (2 * a) * 64:l * 256 + (2 * a + 1) * 64],
                     dal[:, l * 256 + (2 * a + 1) * 64:l * 256 + (2 * a + 2) * 64], ALU.mult)
                S.reduce(t3[:, l * 2 + a:l * 2 + a + 1], t2[:, (l * 2 + a) * 64:(l * 2 + a + 1) * 64], ALU.add)
        S.act(t3[:], t3[:], AF.Exp)
        for l in range(L):
            li = 0.8 - 0.6 * math.exp(-0.3 * l)
            S.stt(lam_neg[:, l:l + 1], t3[:, 2 * l + 1:2 * l + 2], -li, t3[:, 2 * l:2 * l + 1], ALU.add, ALU.subtract)
            S.ts(subw[:, l:l + 1], small["subln"][:, l:l + 1], 1.0 - li, ALU.mult)
        S.act(nsp[:], small["rlam"][:], AF.Exp, scale=-1.0)
        S.act(nsp[:], nsp[:], AF.Ln, bias=1.0)
        S.ts(nsp[:], nsp[:], -8.0, ALU.mult)
        eh = sb(E0, "eh", [128, 32]); sh_ = sb(E0, "sh_", [128, 8])
        S.act(eh[:], small["hlb"][:], AF.Exp)
        S.tt(sh_[:], eh[:, 0:8], eh[:, 8:16], ALU.add)
        S.tt(sh_[:], sh_[:], eh[:, 16:24], ALU.add)
        S.tt(sh_[:], sh_[:], eh[:, 24:32], ALU.add)
        S.op("dve", lambda: nc.vector.reciprocal(out=sh_.t[:], in_=sh_.t[:]), [sh_[:]], [sh_[:]])
        for l in range(4):
            S.tt(eh[:, l * 8:(l + 1) * 8], eh[:, l * 8:(l + 1) * 8], sh_[:], ALU.mult)
        S.memset(lbv[:, 0:8], 0.0)
        S.copy(lbv[:, 8:16], eh[:, 8:16])
        S.tt(lbv[:, 16:24], lbv[:, 8:16], eh[:, 16:24], ALU.add)
        S.tt(lbv[:, 24:32], lbv[:, 16:24], eh[:, 24:32], ALU.add)
        S.ts(oml[:], lbv[:], -1.0, ALU.mult, 1.0, ALU.add)
        S.ts(noml[:], oml[:], -1.0, ALU.mult)
        barrier()

    def layer_norm(l, which, E):
        gcol = (2 * which) * L * 8 + l * 8
        bcol = (2 * which + 1) * L * 8 + l * 8
        sq = [sb(E, f"lnsq{which}_{i}", [128, 512]) for i in range(2)]
        mean = sb(E, f"lnmean{which}", [128, 512]); rstd = sb(E, f"lnrstd{which}", [128, 512]); tmp = sb(E, f"lntmp{which}", [128, 512])
        for th in range(2):
            sl = slice(th * 512, (th + 1) * 512)
            for c in range(8):
                S.mm(ps[0][:], ones[:], X[c][:, sl], start=(c == 0), stop=(c == 7))
            for c in range(8):
                S.act(sq[c % 2][:], X[c][:, sl], AF.Square)
                S.mm(ps[1][:], ones[:], sq[c % 2][:], start=(c == 0), stop=(c == 7), signal=True)
            S.ts(mean[:], ps[0][:], 1.0 / 1024, ALU.mult)
            S.tt(tmp[:], mean[:], mean[:], ALU.mult)
            S.stt(rstd[:], ps[1][:], 1.0 / 1024, tmp[:], ALU.mult, ALU.subtract)
            S.act(rstd[:], rstd[:], AF.Ln, bias=EPS_LN)
            S.act(rstd[:], rstd[:], AF.Exp, scale=-0.5)
            for c in range(8):
                S.tt(X[c][:, sl], X[c][:, sl], mean[:], ALU.subtract)
                S.tt(X[c][:, sl], X[c][:, sl], rstd[:], ALU.mult)
                S.ts(X[c][:, sl], X[c][:, sl], lnp[:, gcol + c:gcol + c + 1], ALU.mult, lnp[:, bcol + c:bcol + c + 1], ALU.add)

    def modulate(l, second):
        b = l * 48 + (24 if second else 0)
        for c in range(8):
            S.ts(U[c][:], X[c][:], mod[:, b + 8 + c:b + 9 + c], ALU.mult, mod[:, b + c:b + c + 1], ALU.add)

    wq_rr = [0]

    def load_w_cols(wbufs, src_rows_ap, c0, ncols):
        wb = wbufs[wq_rr[0] % len(wbufs)]
        wq_rr[0] += 1
        S.dma("pool", wb[:, :, 0:ncols], src_rows_ap.rearrange("(kc p) c -> p kc c", p=128)[:, :, c0:c0 + ncols])
        return wb

    def proj_fm(wb, oc, th, out_ps, nk=8, rhs=None):
        rhs = rhs or U
        for kc in range(nk):
            S.mm(out_ps, wb[:, kc, oc * 128:(oc + 1) * 128], rhs[kc][:, th * 512:(th + 1) * 512], start=(kc == 0), stop=(kc == nk - 1))

    def proj_tm(wb, tb, out_ps, ncols=512):
        for kc in range(8):
            S.mm(out_ps, U[kc][:, tb * 128:(tb + 1) * 128], wb[:, kc, 0:ncols], start=(kc == 0), stop=(kc == 7))

    for l in range(L):
        li = 0.8 - 0.6 * math.exp(-0.3 * l)
        modulate(l, False)
        with ExitStack() as EM:
            BR = [[sb(EM, f"br{n}_{c}", [128, NT], BF16) for c in range(4)] for n in range(3)]
            wbufs = [sb(EM, f"wcol{i}", [128, 8, 512], BF16) for i in range(2)]
            if True:
                with ExitStack() as EA:
                  if "att" in stages:
                    Q = [[sb(EA, f"q{h}_{m}", [128, NT], BF16) for m in range(2)] for h in range(4)]
                    K = [[sb(EA, f"k{h}_{m}", [128, 1280], BF16) for m in range(2)] for h in range(4)]
                    for h in range(4 if "noM" not in stages else 0):
                        for m in range(2):
                            oth = slice(64, 128) if m == 0 else slice(0, 64)
                            mrow = slice(64, 72) if m == 0 else slice(0, 8)
                            S.memset(Q[h][m][oth, :], 0.0); S.memset(K[h][m][oth, :], 0.0)
                            S.dma("pool", Q[h][m][mrow, :], amq_d[:, :]); S.dma("pool", K[h][m][mrow, :], amk_d[:, :])
                    V = [sb(EA, f"v{t}", [128, 512], BF16) for t in range(10)]
                    rc = sb(EA, "ropec", [128, NT]); rs = sb(EA, "ropes", [128, NT]); perm = sb(EA, "perm", [128, 128], BF16)
                    S.dma("sp", rc[:], ropec_d[:, :]); S.dma("sp", rs[:], ropes_d[:, :]); S.dma("pool", perm[:], perm_d[:, :])
                    qb = [sb(EA, f"qb{i}", [128, 512], BF16) for i in range(2)]
                    t1 = [sb(EA, f"at1{i}", [128, 512]) for i in range(2)]
                    t2 = [sb(EA, f"at2{i}", [128, 512]) for i in range(2)]
                    stg = [sb(EA, f"stg{i}", [128, 512]) for i in range(2)]
                    for which, c0, DST in (((0, C_Q, Q), (1, C_K, K)) if "noA1" not in stages else ()):
                        wb = load_w_cols(wbufs, w_in_d[l], c0, 512)
                        for h in range(4):
                            for th in range(2):
                                i = th
                                sl = slice(th * 512, (th + 1) * 512)
                                proj_fm(wb, h, th, ps[th][:])
                                if "noR" in stages:
                                    S.copy(t1[i][:], ps[th][:], eng="act")
                                    S.memset(t2[i][:], 0.0)
                                else:
                                    S.copy(qb[i][:], ps[th][:], eng="act")
                                    S.op("dve", lambda: nc.vector.tensor_tensor(out=t1[i].t[:], in0=ps[th].t[:], in1=rc.t[:, sl], op=ALU.mult),
                                         [ps[th][:], rc[:, sl], qb[i][:]], [t1[i][:]])
                                    if "R1" in stages:
                                        S.memset(t2[i][:], 0.0)
                                    else:
                                        S.mm(ps[2 + th][:], perm[:], qb[i][:])
                                        S.tt(t2[i][:], ps[2 + th][:], rs[:, sl], ALU.mult)
                                S.tt(DST[h][0][0:64, sl], t1[i][0:64, :], t2[i][0:64, :], ALU.add)
                                S.tt(DST[h][1][64:128, sl], t1[i][64:128, :], t2[i][64:128, :], ALU.add)
                    wbk = load_w_cols(wbufs, w_in_d[l], C_K, 512)
                    NA2 = 8 if "noA2" not in stages else 0
                    for tb in range(NA2):
                        proj_tm(wbk, tb, ps[tb % 2][:])
                        S.copy(stg[tb % 2][:], ps[tb % 2][:], eng="act")
                        S.dma("sp", k_o[l, tb * 128:(tb + 1) * 128, :], stg[tb % 2][:])
                    wbv = load_w_cols(wbufs, w_in_d[l], C_V, 512)
                    for tb in range(NA2):
                        proj_tm(wbv, tb, ps[tb % 2][:])
                        S.copy(stg[tb % 2][:], ps[tb % 2][:], eng="act")
                        S.copy(V[tb][:], stg[tb % 2][:])
                        S.dma("sp", v_o[l, tb * 128:(tb + 1) * 128, :], stg[tb % 2][:])
                    NA3 = 2 if "noA3" not in stages else 0
                    for blk in range(NA3):
                        S.dma("sp", stg[blk][:], kctx_d[l, blk * 128:(blk + 1) * 128, :])
                        for h in range(4):
                            S.transpose(ps[2][:, h * 128:(h + 1) * 128], stg[blk][:, h * 128:(h + 1) * 128], ident[:])
                        for h in range(4):
                            S.copy(K[h][0][0:64, 1024 + blk * 128:1024 + (blk + 1) * 128], ps[2][0:64, h * 128:(h + 1) * 128])
                            S.copy(K[h][1][64:128, 1024 + blk * 128:1024 + (blk + 1) * 128], ps[2][64:128, h * 128:(h + 1) * 128])
                    for blk in range(NA3):
                        S.dma("pool", V[8 + blk][:], vctx_d[l, blk * 128:(blk + 1) * 128, :])
                    pT = [sb(EA, f"pT{i}", [128, 512], BF16) for i in range(4)]
                    o_a = sb(EA, "o_a", [128, 512]); o_b = sb(EA, "o_b", [128, 512]); rcp = sb(EA, "rcp", [128, 512]); sqa = sb(EA, "sqa", [128, 512])
                    pi = 0
                    for h in range(4 if "noattcore" not in stages else 0):
                        for qh in range(2):
                            qs = slice(qh * 512, (qh + 1) * 512)
                            items = [(m, kb) for m in range(2) for kb in range(10)]

                            def emit_score(idx):
                                m, kb = items[idx]
                                k_ = pi + idx
                                sc = ps[4 + (k_ % 3)]
                                S.mm(sc[:], K[h][m][:, kb * 128:(kb + 1) * 128], Q[h][m][:, qs])
                                p_ = pT[k_ % 4]
                                S.act(p_[:], sc[:], AF.Exp, scale=0.125)
                                return p_

                            p_next = emit_score(0)
                            for idx, (m, kb) in enumerate(items):
                                p_ = p_next
                                if idx + 1 < len(items):
                                    p_next = emit_score(idx + 1)
                                S.mm(ps[2 * m][:], V[kb][:, h * 128:(h + 1) * 128], p_[:], start=(kb == 0), stop=(kb == 9), signal=True)
                                S.mm(ps[2 * m + 1][:], onesb[:], p_[:], start=(kb == 0), stop=(kb == 9), signal=True)
                            pi += len(items)
                            S.op("dve", lambda: nc.vector.reciprocal(out=rcp.t[:], in_=ps[1].t[:]), [ps[1][:]], [rcp[:]])
                            S.tt(o_a[:], ps[0][:], rcp[:], ALU.mult)
                            S.op("dve", lambda: nc.vector.reciprocal(out=rcp.t[:], in_=ps[3].t[:]), [ps[3][:]], [rcp[:]])
                            S.tt(o_b[:], ps[2][:], rcp[:], ALU.mult)
                            S.stt(o_a[:], o_b[:], lam_neg[:, l:l + 1], o_a[:], ALU.mult, ALU.add)
                            S.act(sqa[:], o_a[:], AF.Square)
                            S.mm(ps[1][:], ones[:], sqa[:])
                            S.act(rcp[:], ps[1][:], AF.Ln, scale=1.0 / 128, bias=EPS)
                            S.act(rcp[:], rcp[:], AF.Exp, scale=-0.5)
                            S.tt(o_a[:], o_a[:], rcp[:], ALU.mult)
                            S.ts(BR[0][h][:, qs], o_a[:], subw[:, l:l + 1], ALU.mult)
                    barrier()
                with ExitStack() as EB:
                  if "rg" in stages:
                    cm = sb(EB, "cmaskt", [128, 3 * NT], BF16); S.dma("pool", cm[:], cmask_d[:, :])
                    sm = sb(EB, "smaskt", [128, 2 * NT], BF16); S.dma("pool", sm[:], smask_d[:, :])
                    gwt = sb(EB, "gwt", [128, 16 * 128], BF16)
                    S.memset(gwt[:], 0.0)
                    for d in range(2):
                        for g in range(2):
                            for n in range(8):
                                c, half = n // 2, n % 2
                                col = ((d * 2 + g) * 4 + c) * 128 + half * 64
                                S.dma("pool", gwt[half * 64:(half + 1) * 64, col:col + 64], gw_d[l, d, g, n, :, :])
                    rx = sb(EB, "rx", [128, NT]); xr = sb(EB, "xr", [128, NT]); xrb = sb(EB, "xrb", [128, NT], BF16)
                    tmp = sb(EB, "rtmp", [128, NT]); rr = sb(EB, "rr", [128, NT]); ii = sb(EB, "ii", [128, NT])
                    aa = sb(EB, "aa", [128, NT]); bb = sb(EB, "bb", [128, NT]); hh = [sb(EB, f"hh{d}", [128, NT]) for d in range(2)]
                    gg = sb(EB, "gg", [128, NT]); ge = sb(EB, "ge", [128, NT])
                    rgfin = sb(EB, "rgfin", [128, 32])
                    wbx = load_w_cols(wbufs, w_in_d[l], C_RX, 512)
                    wbg = load_w_cols(wbufs, w_in_d[l], C_RG, 512)
                    rgv = rgfin.t[:, :].rearrange("p (s d c) -> p s d c", s=4, d=2)
                    for c in range(4):
                        for th in range(2):
                            proj_fm(wbx, c, th, ps[th][:])
                            S.copy(rx[:, th * 512:(th + 1) * 512], ps[th][:], eng="act")
                        cw = lambda tap: small["convw"][:, l * 16 + tap * 4 + c:l * 16 + tap * 4 + c + 1]
                        S.ts(xr[:], rx[:], cw(2), ALU.mult, small["convb"][:, l * 4 + c:l * 4 + c + 1], ALU.add)
                        S.tt(tmp[:, 2:NT], rx[:, 0:NT - 2], cm[:, 2:NT], ALU.mult)
                        S.stt(xr[:, 2:NT], tmp[:, 2:NT], cw(0), xr[:, 2:NT], ALU.mult, ALU.add)
                        S.tt(tmp[:, 1:NT], rx[:, 0:NT - 1], cm[:, NT + 1:2 * NT], ALU.mult)
                        S.stt(xr[:, 1:NT], tmp[:, 1:NT], cw(1), xr[:, 1:NT], ALU.mult, ALU.add)
                        S.tt(tmp[:, 0:NT - 1], rx[:, 1:NT], cm[:, 2 * NT:3 * NT - 1], ALU.mult)
                        S.stt(xr[:, 0:NT - 1], tmp[:, 0:NT - 1], cw(3), xr[:, 0:NT - 1], ALU.mult, ALU.add)
                        S.copy(xrb[:], xr[:], eng="act")
                        for d in range(2):
                            for g, dst in ((0, rr), (1, ii)):
                                col = ((d * 2 + g) * 4 + c) * 128
                                bcol = l * 16 + (d * 2 + g) * 4 + c
                                for th in range(2):
                                    S.mm(ps[2 + th][:], gwt[:, col:col + 128], xrb[:, th * 512:(th + 1) * 512])
                                    S.act(dst[:, th * 512:(th + 1) * 512], ps[2 + th][:], AF.Sigmoid, bias=small["gb"][:, bcol:bcol + 1])
                            ncol = l * 8 + d * 4 + c
                            S.act(aa[:], rr[:], AF.Exp, scale=nsp[:, ncol:ncol + 1])
                            S.tt(bb[:], aa[:], aa[:], ALU.mult)
                            S.act(bb[:], bb[:], AF.Sqrt, scale=-1.0, bias=1.0)
                            S.tt(bb[:], bb[:], ii[:], ALU.mult)
                            S.tt(bb[:], bb[:], xr[:], ALU.mult)
                            S.tt(aa[:], aa[:], sm[:, d * NT:(d + 1) * NT], ALU.mult)
                            h0 = small["rg0"][:, ncol:ncol + 1]
                            if d == 0:
                                S.scan(hh[0][:], aa[:], bb[:], h0, ALU.mult, ALU.add)
                                S.copy(View(rgfin, rgv[:, :, 0, c]), hh[0][:, 255:NT:256])
                            else:
                                S.scan(hh[1][:, ::-1], aa[:, ::-1], bb[:, ::-1], h0, ALU.mult, ALU.add)
                                S.copy(View(rgfin, rgv[:, :, 1, c]), hh[1][:, 0:NT:256])
                        S.tt(hh[0][:], hh[0][:], hh[1][:], ALU.add)
                        for th in range(2):
                            proj_fm(wbg, c, th, ps[th][:])
                            S.copy(gg[:, th * 512:(th + 1) * 512], ps[th][:], eng="act")
                        S.tt(ge[:], gg[:], gg[:], ALU.mult)
                        S.ts(ge[:], ge[:], 0.044715, ALU.mult, 1.0, ALU.add)
                        S.tt(ge[:], ge[:], gg[:], ALU.mult)
                        S.act(ge[:], ge[:], AF.Sigmoid, scale=2.0 * math.sqrt(2.0 / math.pi))
                        S.tt(ge[:], ge[:], gg[:], ALU.mult)
                        S.tt(BR[1][c][:], hh[0][:], ge[:], ALU.mult)
                    S.transpose(ps[4][0:32, 0:128], rgfin[:], ident[:])
                    rgo = sb(EB, "rgo", [32, 128]); S.copy(rgo[:], ps[4][0:32, 0:128])
                    S.dma("sp", rg_o[l, :, :], rgo[:])
                    barrier()
                with ExitStack() as EC:
                  if "hg" in stages:
                    rm = sb(EC, "rmaskt", [128, 2 * NT], BF16); S.dma("pool", rm[:], rmask_d[:, :])
                    cbm = sb(EC, "cbm", [128, 256], BF16); S.dma("pool", cbm[:], cb_d[:, :])
                    HI = [sb(EC, f"hi{t}", [128, 512], BF16) for t in range(8)]
                    HIc = [sb(EC, f"hic{n}", [32, 512], BF16) for n in range(32)]
                    wbi = wbufs[0]
                    S.dma("pool", wbi[:, :, 0:512], w_in_d[l].rearrange("(kc p) c -> p kc c", p=128)[:, :, C_HI:C_HI + 512])
                    for tb in range(8):
                        proj_tm(wbi, tb, ps[tb % 2][:])
                        S.copy(HI[tb][:], ps[tb % 2][:], eng=("act" if tb % 2 else "dve"))
                    for n in range(32):
                        for kc in range(8):
                            S.mm(ps[2 + n % 2][0:32, :], U[kc][:, n * 32:(n + 1) * 32], wbi[:, kc, 0:512], start=(kc == 0), stop=(kc == 7))
                        S.copy(HIc[n][:], ps[2 + n % 2][0:32, :], eng=("act" if n % 2 else "dve"))
                    wh = [Buf(wbufs[1].t[:, :, i * 128:(i + 1) * 128], f"wh{i}") for i in range(4)]
                    qh = sb(EC, "qh", [128, NT]); osum = sb(EC, "osum", [128, NT]); sg = sb(EC, "sg", [128, NT])
                    gl = sb(EC, "gl", [128, NT]); kk = sb(EC, "kk", [128, NT]); bc = sb(EC, "bc", [128, NT]); ex = sb(EC, "ex", [128, NT])
                    kt = sb(EC, "kt", [128, NT], BF16); kh = sb(EC, "kh", [128, NT], BF16)
                    qt = [sb(EC, f"qt{d}", [128, NT], BF16) for d in range(2)]
                    Dv = [sb(EC, f"Dv{d}", [128, 32]) for d in range(2)]; Dcm = [sb(EC, f"Dcm{d}", [128, 32]) for d in range(2)]
                    KHc = [sb(EC, f"khc{d}", [32, 32 * 128], BF16) for d in range(2)]
                    AT = [sb(EC, f"AT{d}", [128, 8 * 128], BF16) for d in range(2)]
                    cbm4 = sb(EC, "cbm4", [128, 2 * 512], BF16)
                    for d in range(2):
                        for r4 in range(4):
                            S.copy(cbm4[:, d * 512 + r4 * 128:d * 512 + (r4 + 1) * 128], cbm[:, d * 128:(d + 1) * 128])
                    S32 = [sb(EC, f"S32{d}", [128, 128]) for d in range(2)]; Sb = [sb(EC, f"Sb{d}", [128, 128], BF16) for d in range(2)]
                    for h in range(4):
                        for i, c0 in enumerate((C_HQ, C_HZF, C_HZB, C_HG)):
                            S.dma("pool", wh[i][:], w_in_d[l].rearrange("(kc p) c -> p kc c", p=128)[:, :, c0 + h * 128:c0 + (h + 1) * 128])
                        for th in range(2):
                            proj_fm(wh[0], 0, th, ps[6][:])
                            S.act(qh[:, th * 512:(th + 1) * 512], ps[6][:], AF.Silu)
                        S.memset(osum[:], 0.0)
                        for d in range(2):
                            lc = l * 8 + d * 4 + h
                            for th in range(2):
                                proj_fm(wh[1 + d], 0, th, ps[6][:])
                                S.act(sg[:, th * 512:(th + 1) * 512], ps[6][:], AF.Sigmoid)
                            S.ts(gl[:], sg[:], oml[:, lc:lc + 1], ALU.mult, lbv[:, lc:lc + 1], ALU.add)
                            S.act(gl[:], gl[:], AF.Ln)
                            S.ts(kk[:], sg[:], noml[:, lc:lc + 1], ALU.mult, oml[:, lc:lc + 1], ALU.add)
                            if d == 0:
                                S.scan(bc[:], rm[:, 0:NT], gl[:], 0.0, ALU.mult, ALU.add)
                                S.act(Dv[d][:], bc[:, 31:NT:32], AF.Exp)
                            else:
                                S.scan(bc[:, ::-1], rm[:, 2 * NT - 1:NT - 1:-1], gl[:, ::-1], 0.0, ALU.mult, ALU.add)
                                S.act(Dv[d][:], bc[:, 0:NT:32], AF.Exp)
                            S.act(ex[:], bc[:], AF.Exp)
                            S.tt(qt[d][:], qh[:], ex[:], ALU.mult)
                            S.act(ex[:], bc[:], AF.Exp, scale=-1.0)
                            S.tt(ex[:], kk[:], ex[:], ALU.mult)
                            S.copy(kt[:], ex[:], eng="act")
                            for n in range(32):
                                S.ts(kh[:, n * 32:(n + 1) * 32], ex[:, n * 32:(n + 1) * 32], Dv[d][:, n:n + 1], ALU.mult)
                            S.tt(Dcm[d][:], Dv[d][:], hcmb[:, d * 32:(d + 1) * 32], ALU.mult)
                            for g in range(4):
                                for n8 in range(8):
                                    n = g * 8 + n8
                                    S.transpose(psb16[0:32, n8 * 128:(n8 + 1) * 128], kh[:, n * 32:(n + 1) * 32], identb[:])
                                S.copy(KHc[d][:, g * 1024:(g + 1) * 1024], psb16[0:32, :], eng="act")
                            for g in range(2):
                                pa = ps[6]
                                for t4 in range(4):
                                    tb = g * 4 + t4
                                    S.mm(pa[:, t4 * 128:(t4 + 1) * 128], kt[:, tb * 128:(tb + 1) * 128], qt[d][:, tb * 128:(tb + 1) * 128])
                                S.tt(AT[d][:, g * 512:(g + 1) * 512], pa[:], cbm4[:, d * 512:(d + 1) * 512], ALU.mult)
                            S.dma("sp", S32[d][:], hg0_d[l, d, h, :, :])
                            first = 0 if d == 0 else 31
                            S.ts(Sb[d][:], S32[d][:], hcmb[:, d * 32 + first:d * 32 + first + 1], ALU.mult)
                        def chunk_of(d, step):
                            return step if d == 0 else 31 - step

                        def emit_kv(d, step):
                            n = chunk_of(d, step)
                            S.mm(ps[2 + 2 * d + step % 2][:, 0:128], KHc[d][:, n * 128:(n + 1) * 128], HIc[n][:, h * 128:(h + 1) * 128])

                        for d in range(2):
                            emit_kv(d, 0)
                        for step in range(32):
                            for d in range(2):
                                po = ps[d]
                                n = chunk_of(d, step)
                                tb, j = n // 4, n % 4
                                bs = slice((tb % 4) * 128, (tb % 4 + 1) * 128)
                                blk_first = (j == 0) if d == 0 else (j == 3)
                                blk_last = (j == 3) if d == 0 else (j == 0)
                                if step + 1 < 32:
                                    emit_kv(d, step + 1)
                                if blk_first:
                                    S.mm(po[:, bs], HI[tb][:, h * 128:(h + 1) * 128], AT[d][:, tb * 128:(tb + 1) * 128], start=True, stop=False, signal=True)
                                S.mm(po[:, (tb % 4) * 128 + j * 32:(tb % 4) * 128 + (j + 1) * 32], Sb[d][:], qt[d][:, n * 32:(n + 1) * 32],
                                     start=False, stop=blk_last, signal=True)
                                pk = ps[2 + 2 * d + step % 2]
                                S.stt(S32[d][:], S32[d][:], Dcm[d][:, n:n + 1], pk[:, 0:128], ALU.mult, ALU.add)
                                seq_end = (n % 8 == 7) if d == 0 else (n % 8 == 0)
                                if seq_end:
                                    S.dma("sp", hg_o[l, n // 8, d, h, :, :], S32[d][:])
                                nn = n + 1 if d == 0 else n - 1
                                if 0 <= nn < 32:
                                    S.ts(Sb[d][:], S32[d][:], hcmb[:, d * 32 + nn:d * 32 + nn + 1], ALU.mult)
                                if blk_last:
                                    ts_ = slice(tb * 128, (tb + 1) * 128)
                                    S.tt(osum[:, ts_], osum[:, ts_], po[:, bs], ALU.add)
                        for th in range(2):
                            sl = slice(th * 512, (th + 1) * 512)
                            proj_fm(wh[3], 0, th, ps[6][:])
                            S.act(sg[:, sl], ps[6][:], AF.Silu)
                            S.act(gl[:, 0:512], osum[:, sl], AF.Square)
                            S.mm(ps[6][:], ones[:], gl[:, 0:512])
                            S.act(gl[:, 512:1024], ps[6][:], AF.Ln, scale=1.0 / 128, bias=EPS)
                            S.act(gl[:, 512:1024], gl[:, 512:1024], AF.Exp, scale=-0.5)
                            S.tt(gl[:, 512:1024], gl[:, 512:1024], osum[:, sl], ALU.mult)
                            S.ts(gl[:, 512:1024], gl[:, 512:1024], small["hnorm"][:, l:l + 1], ALU.mult)
                            S.tt(BR[2][h][:, sl], gl[:, 512:1024], sg[:, sl], ALU.mult)
                    barrier()
            with ExitStack() as ED:
                wbr = sb(ED, "wbr", [128, 12, 1024], BF16)
                S.dma("pool", wbr[:], w_br_d[l].rearrange("n (kc p) c -> p (n kc) c", p=128))
                macc = [sb(ED, f"macc{c}", [128, NT]) for c in range(8)]
                gsb = sb(ED, "gsb", [128, 512]); gp = sb(ED, "gp", [128, 512])
                for n in range(3):
                    for og in range(2):
                        wb = load_w_cols(wbufs, w_in_d[l], C_MG + n * 1024 + og * 512, 512)
                        for oi in range(4):
                            oc = og * 4 + oi
                            for th in range(2):
                                sl = slice(th * 512, (th + 1) * 512)
                                proj_fm(wb, oi, th, ps[th][:])
                                S.act(gsb[:], ps[th][:], AF.Sigmoid)
                                for kc in range(4):
                                    S.mm(ps[2 + th][:], wbr[:, n * 4 + kc, oc * 128:(oc + 1) * 128], BR[n][kc][:, sl], start=(kc == 0), stop=(kc == 3))
                                if n == 0:
                                    S.tt(macc[oc][:, sl], gsb[:], ps[2 + th][:], ALU.mult)
                                else:
                                    S.tt(gp[:], gsb[:], ps[2 + th][:], ALU.mult)
                                    S.tt(macc[oc][:, sl], macc[oc][:, sl], gp[:], ALU.add)
                mb = [sb(ED, f"mb{c}", [128, NT], BF16) for c in range(8)]
                for c in range(8):
                    S.copy(mb[c][:], macc[c][:], eng=("act" if c % 2 else "dve"))
                for og in range(2):
                    wb = load_w_cols(wbufs, w_out_d[l], og * 512, 512)
                    for oi in range(4):
                        oc = og * 4 + oi
                        for th in range(2):
                            sl = slice(th * 512, (th + 1) * 512)
                            proj_fm(wb, oi, th, ps[th][:], rhs=mb)
                            S.stt(X[oc][:, sl], ps[th][:], mod[:, l * 48 + 16 + oc:l * 48 + 17 + oc], X[oc][:, sl], ALU.mult, ALU.add)
                layer_norm(l, 0, ED)
                barrier()
        barrier()
        modulate(l, True)
        with ExitStack() as EE:
            if "moe" in stages:
                lg = sb(EE, "lg", [128, 256]); gate = sb(EE, "gate", [128, 256]); m8 = sb(EE, "m8", [128, 8]); nm = sb(EE, "nm", [128, 1])
                msk = sb(EE, "msk", [128, 32]); den = sb(EE, "den", [128, 1])
                gT = sb(EE, "gT", [32, NT])
                rbt = sb(EE, "rbt", [128, 256])
                for tb in range(8):
                    S.copy(rbt[:, tb * 32:(tb + 1) * 32], small["rb"][:, l * 32:(l + 1) * 32])
                ER = ExitStack()
                u2f = [sb(ER, f"u2f{i}", [128, NT]) for i in range(2)]
                rwt = sb(ER, "rwt", [128, 8, 32]); S.dma("sp", rwt[:], rw_d[l].rearrange("(kc p) e -> p kc e", p=128))
                b = l * 48 + 24
                for c in range(8):
                    uf = u2f[c % 2]
                    S.ts(uf[:], X[c][:], mod[:, b + 8 + c:b + 9 + c], ALU.mult, mod[:, b + c:b + c + 1], ALU.add)
                    for tb in range(8):
                        S.mm(ps[0][:, tb * 32:(tb + 1) * 32], uf[:, tb * 128:(tb + 1) * 128], rwt[:, c, :], start=True, stop=True, signal=(tb == 7))
                    S.tt(lg[:], ps[0][:, 0:256], (rbt if c == 0 else lg)[:], ALU.add)
                for tb in range(8):
                    lt = lg[:, tb * 32:(tb + 1) * 32]
                    S.op("dve", lambda: nc.vector.max(out=m8.t[:], in_=lg.t[:, tb * 32:(tb + 1) * 32]), [lt], [m8[:]])
                    S.ts(nm[:], m8[:, 0:1], -1.0, ALU.mult)
                    S.ts(msk[:], lt, m8[:, 3:4], ALU.is_ge)
                    gt_ = gate[:, tb * 32:(tb + 1) * 32]
                    S.act(gt_, lt, AF.Exp, bias=nm[:])
                    S.tt(gt_, gt_, msk[:], ALU.mult)
                    S.reduce(den[:], gt_, ALU.add)
                    S.op("dve", lambda: nc.vector.reciprocal(out=den.t[:], in_=den.t[:]), [den[:]], [den[:]])
                    S.ts(gt_, gt_, den[:], ALU.mult)
                    if tb < 4:
                        S.transpose(ps[1][0:32, tb * 128:(tb + 1) * 128], gt_, ident[:])
                S.copy(gT[:, 0:512], ps[1][0:32, 0:512]);
                for tb in range(4, 8):
                    pass
                for tb in range(4, 8):
                    S.transpose(ps[2][0:32, (tb - 4) * 128:(tb - 3) * 128], gate[:, tb * 32:(tb + 1) * 32], ident[:])
                S.copy(gT[:, 512:1024], ps[2][0:32, 0:512])
                barrier()
                ER.close()
                b2t = sb(EE, "b2t", [32, 1024]); S.dma("sp", b2t[:], b2_d[l, :, :])
                selt = sb(EE, "selt", [32, 32 * 128], BF16); S.dma("pool", selt[:], sel_d[:, :])
                gTb = sb(EE, "gTb", [32, NT], BF16); S.copy(gTb[:], gT[:])
                g2c = l * 48 + 40
                for dc in range(8):
                    for th in range(2):
                        sl = slice(th * 512, (th + 1) * 512)
                        S.mm(ps[3 + th][:], b2t[:, dc * 128:(dc + 1) * 128], gT[:, sl])
                        S.stt(X[dc][:, sl], ps[3 + th][:], mod[:, g2c + dc:g2c + dc + 1], X[dc][:, sl], ALU.mult, ALU.add)
                ring = [sb(EE, f"wring{i}", [128, 8, 1024], BF16) for i in range(4)]
                ACTB = [[sb(EE, f"actb{i}_{j}", [128, NT], BF16) for j in range(8)] for i in range(2)]
                gbc = [sb(EE, "gbc0", [128, NT])] * 2
                NR = 3
                Gb = [sb(EE, f"Gb{i}", [128, 512], BF16) for i in range(NR)]; Lb = [sb(EE, f"Lb{i}", [128, 512], BF16) for i in range(NR)]
                sgm = [sb(EE, f"sgm{i}", [128, 512], BF16) for i in range(NR)]
                t1m = [sb(EE, f"t1m{i}", [128, 512], BF16) for i in range(NR)]; t2m = [sb(EE, f"t2m{i}", [128, 512], BF16) for i in range(NR)]
                rr_ = [0]

                def load_piece(src):
                    wb = ring[rr_[0] % 4]; rr_[0] += 1
                    S.dma("pool", wb[:], src)
                    return wb

                def pieces(e):
                    v1 = w1_d[l, e].rearrange("(kc p) c -> p kc c", p=128)
                    return (load_piece(v1[:, :, 0:1024]), load_piece(v1[:, :, 1024:2048]),
                            load_piece(w2_d[l, e].rearrange("(kc p) c -> p kc c", p=128)))

                nxt = pieces(0)
                it = 0
                for e in range(n_exp):
                    wg, wl, w2b = nxt
                    ab = ACTB[e % 2]
                    for th in range(2):
                        S.mm(ps[5][:], selt[:, e * 128:(e + 1) * 128], gTb[:, th * 512:(th + 1) * 512])
                        S.copy(gbc[e % 2][:, th * 512:(th + 1) * 512], ps[5][:], eng="act")
                    bcol = (l * 32 + e) * 8
                    for j in range(8):
                        for th in range(2):
                            sl = slice(th * 512, (th + 1) * 512)
                            i = it % 2; r = it % NR; it += 1
                            proj_fm(wg, j, th, ps[2 * i][:])
                            proj_fm(wl, j, th, ps[2 * i + 1][:])
                            S.act(Gb[r][:], ps[2 * i][:], AF.Identity, bias=small["b1g"][:, bcol + j:bcol + j + 1])
                            S.act(Lb[r][:], ps[2 * i + 1][:], AF.Identity, bias=small["b1l"][:, bcol + j:bcol + j + 1])
                            S.ts(Gb[r][:], Gb[r][:], 7.0, ALU.min)
                            S.act(sgm[r][:], Gb[r][:], AF.Sigmoid, scale=1.702)
                            S.ts(Lb[r][:], Lb[r][:], 8.0, ALU.min, -6.0, ALU.max)
                            S.tt(t2m[r][:], Lb[r][:], gbc[e % 2][:, sl], ALU.mult, eng="pool")
                            S.tt(t1m[r][:], Gb[r][:], sgm[r][:], ALU.mult, eng="pool")
                            S.tt(ab[j][:, sl], t1m[r][:], t2m[r][:], ALU.mult)
                    if e + 1 < n_exp:
                        nxt = pieces(e + 1)
                    for dc in range(8):
                        for th in range(2):
                            sl = slice(th * 512, (th + 1) * 512)
                            i = it % 2; it += 1
                            proj_fm(w2b, dc, th, ps[2 * i][:], rhs=ab)
                            S.stt(X[dc][:, sl], ps[2 * i][:], mod[:, g2c + dc:g2c + dc + 1], X[dc][:, sl], ALU.mult, ALU.add)
            layer_norm(l, 1, EE)
            barrier()

    with ExitStack() as EF:
        yo = [sb(EF, f"yo{i}", [128, 1024]) for i in range(2)]
        for tb in range(8):
            for hf in range(2):
                for i in range(4):
                    c = hf * 4 + i
                    S.transpose(ps[hf][:, i * 128:(i + 1) * 128], X[c][:, tb * 128:(tb + 1) * 128], ident[:])
                S.copy(yo[tb % 2][:, hf * 512:(hf + 1) * 512], ps[hf][:], eng=("act" if hf else "dve"))
            S.dma("sp", y_o[tb * 128:(tb + 1) * 128, :], yo[tb % 2][:])
        barrier()
    es_all.close()
    return nc


def _cols(a, L):
    a = np.asarray(a, np.float32)
    lead = a.shape[:-1]
    n = a.shape[-1] // 128
    a = a.reshape(*lead, n, 128)
    a = np.moveaxis(a, -1, 0)
    return np.ascontiguousarray(a.reshape(128, -1))


def _structural(role):
    t = np.arange(NT)
    BIG = 32768.0
    amk = np.zeros((8, 1280), np.float32); amq = np.zeros((8, 1024), np.float32)
    if role == 1:
        amk[0, :] = -BIG; amq[0, :] = 1.0
        for g in range(4):
            amk[1 + g, g * 256:(g + 1) * 256] = BIG
            amq[1 + g, g * 256:(g + 1) * 256] = 1.0
    cos = np.ones((128, NT), np.float32); sin = np.zeros((128, NT), np.float32)
    perm = np.zeros((128, 128), np.float32)
    inv = 10000.0 ** (-np.arange(0, 32, 2, dtype=np.float32) / 32)
    row = (t // 64).astype(np.float32); col = (t % 64).astype(np.float32)
    for p in range(128):
        d = p % 64
        pos = row if d < 32 else col
        dd = d % 32
        j = dd % 16
        first = dd < 16
        partner = p + 16 if first else p - 16
        perm[partner, p] = 1.0
        if role == 0:
            ang = pos * inv[j]
            cos[p] = np.cos(ang)
            sin[p] = -np.sin(ang) if first else np.sin(ang)
    seg = (t % 256) if role == 1 else t
    seglen = 256 if role == 1 else NT
    cm = np.ones((3, NT), np.float32)
    cm[0, seg < 2] = 0; cm[1, seg < 1] = 0; cm[2, seg == seglen - 1] = 0
    smk = np.ones((2, NT), np.float32)
    if role == 1:
        smk[0, seg == 0] = 0; smk[1, seg == seglen - 1] = 0
    rmk = np.ones((2, NT), np.float32)
    rmk[0, t % 32 == 0] = 0; rmk[1, t % 32 == 31] = 0
    hcm = np.ones((2, 32), np.float32)
    if role == 1:
        hcm[0, np.arange(32) % 8 == 0] = 0; hcm[1, np.arange(32) % 8 == 7] = 0
    s_ = np.arange(128)[:, None]; t_ = np.arange(128)[None, :]
    same = (s_ // 32) == (t_ // 32)
    cb = np.concatenate([(same & (s_ <= t_)).astype(np.float32), (same & (s_ >= t_)).astype(np.float32)], axis=1)
    rep = lambda a: np.ascontiguousarray(np.broadcast_to(a.reshape(1, -1), (128, a.size)))
    return dict(amk=amk, amq=amq, ropec=cos, ropes=sin, perm=perm, cmask=rep(cm), smask=rep(smk), rmask=rep(rmk),
                hcm=rep(hcm), cb=np.ascontiguousarray(cb))


def prep_shared(inp, L=DEPTH):
    f = lambda k: np.asarray(inp[k], np.float32)
    sh = {}
    sh["w_ada"] = f("w_ada")[:L]; sh["w_in"] = f("w_in")[:L]; sh["w_br"] = f("w_branch")[:L]; sh["w_out"] = f("w_out")[:L]
    sh["rw"] = f("router_w")[:L]; sh["w2"] = f("w2")[:L]; sh["b2"] = f("b2")[:L]; sh["gw"] = f("rg_gate_w")[:L]
    w1 = f("w1")[:L]
    sh["w1d"] = np.ascontiguousarray(w1.reshape(L, 32, 1024, 1024, 2).transpose(0, 1, 2, 4, 3)).reshape(L, 32, 1024, 2048)
    b1 = f("b1")[:L].reshape(L, 32, 1024, 2)
    sh["b1g"] = _cols(b1[..., 0], L); sh["b1l"] = _cols(b1[..., 1], L)
    sh["b_ada"] = _cols(f("b_ada")[:L], L)
    sh["dal"] = np.ascontiguousarray(np.broadcast_to(f("da_lambda")[:L].reshape(1, -1), (128, L * 256)))
    sh["subln"] = _cols(f("da_subln")[:L], L); sh["hnorm"] = _cols(f("hg_norm")[:L], L)
    sh["convw"] = _cols(f("rg_conv_w")[:L], L); sh["convb"] = _cols(f("rg_conv_b")[:L], L)
    sh["gb"] = _cols(f("rg_gate_b")[:L], L); sh["rlam"] = _cols(f("rg_lambda")[:L], L)
    sh["hlb"] = _cols(f("hg_lb"), 4)
    sh["lnp"] = _cols(np.stack([f("ln1_g")[:L], f("ln1_b")[:L], f("ln2_g")[:L], f("ln2_b")[:L]]), L)
    sh["rb"] = np.ascontiguousarray(np.broadcast_to(f("router_b")[:L].reshape(1, -1), (128, L * 32)))
    sh["ident"] = np.eye(128, dtype=np.float32)
    sel = np.zeros((32, 32, 128), np.float32)
    for e in range(32):
        sel[e, e, :] = 1.0
    sh["sel"] = sel.reshape(32, 32 * 128)
    return sh


def prep_core(inp, c, L=DEPTH):
    f = lambda k: np.asarray(inp[k], np.float32)
    d = {}
    if c < 4:
        d["x"] = f("x_sample")[c]
        d["cond"] = _cols(f("c")[c], 1)
        d["kctx"] = f("cache_attn_k")[c, :L].reshape(L, 256, 512)
        d["vctx"] = f("cache_attn_v")[c, :L].reshape(L, 256, 512)
        d["rg0"] = _cols(f("state_rglru")[c, :L], L)
        d["hg0"] = f("state_hgrn")[c, :L]
        d.update(_structural(0))
    else:
        i = c - 4
        d["x"] = f("x_prompt")[4 * i:4 * i + 4].reshape(NT, 1024)
        d["cond"] = _cols(f("c_ctx"), 1)
        d["kctx"] = np.zeros((L, 256, 512), np.float32); d["vctx"] = np.zeros((L, 256, 512), np.float32)
        d["rg0"] = np.zeros((128, L * 8), np.float32); d["hg0"] = np.zeros((L, 2, 4, 128, 128), np.float32)
        d.update(_structural(1))
    return {k: np.ascontiguousarray(v, dtype=np.float32) for k, v in d.items()}


_NC_CACHE = {}


def kernel(**inputs):
    L = DEPTH
    if L not in _NC_CACHE:
        _NC_CACHE[L] = build(L)
    nc = _NC_CACHE[L]
    sh = prep_shared(inputs, L)
    in_maps = []
    for c in range(8):
        m = dict(sh)
        m.update(prep_core(inputs, c, L))
        in_maps.append(m)
    res = run_bass_kernel_spmd(nc, in_maps, core_ids=list(range(8))).results
    y_sample = np.stack([res[c]["y"] for c in range(4)]).astype(np.float32)
    y_prompt = np.concatenate([res[c]["y"].reshape(4, 256, 1024) for c in range(4, 8)]).astype(np.float32)
    ks, vs, rgs, hgs = [], [], [], []
    for c in range(4, 8):
        r = res[c]
        ks.append(r["ok"].reshape(L, 4, 256, 4, 2, 64).transpose(1, 0, 2, 3, 4, 5))
        vs.append(r["ov"].reshape(L, 4, 256, 4, 128).transpose(1, 0, 2, 3, 4))
        rgs.append(r["org"].reshape(L, 4, 2, 4, 128).transpose(1, 0, 2, 3, 4).reshape(4, L, 2, 512))
        hgs.append(r["ohg"].transpose(1, 0, 2, 3, 4, 5))
    cat = lambda xs: np.ascontiguousarray(np.concatenate(xs, axis=0), dtype=np.float32)
    return (y_prompt, y_sample, cat(ks), cat(vs), cat(rgs), cat(hgs))
```
